# Optimizing a Trainium2 kernel written in Bass

```python
import jax, jax.numpy as jnp
from jax import lax
import numpy as np

D_MODEL = 2048
BATCH = 2
SEQ = 8192
DEPTH = 4

A_HEADS = 8
A_HEAD_DIM = 128
A_WIDTH = A_HEADS * A_HEAD_DIM
MOBA_BLOCK = 256
MOBA_TOPK = 3
MOBA_QBLOCK = 64
R_HEADS = 4
R_QK_DIM = 256
R_V_DIM = 512
R_QK_WIDTH = R_HEADS * R_QK_DIM
R_V_WIDTH = R_HEADS * R_V_DIM
R_CHUNK = 256
_FF_RAW = -(-8 * D_MODEL // 3)
D_FF = -(-_FF_RAW // 256) * 256
EPS = 1e-6
SEQ_ALIGN = 256

kernel_name = "moba_retention_gated_hybrid"


def _split_points():
    sizes = [A_WIDTH] * 3 + [R_QK_WIDTH] * 2 + [R_V_WIDTH] * 2 + [D_MODEL] * 2
    pts, acc = [], 0
    for s in sizes[:-1]:
        acc += s
        pts.append(acc)
    return pts, acc + sizes[-1]


def rms_norm(x, g):
    xf = x.astype(jnp.float32)
    y = xf * lax.rsqrt(jnp.mean(xf * xf, axis=-1, keepdims=True) + EPS)
    return (y * g.astype(jnp.float32)).astype(x.dtype)


def head_group_norm(y):
    yf = y.astype(jnp.float32)
    mu = jnp.mean(yf, axis=-1, keepdims=True)
    var = jnp.mean(jnp.square(yf - mu), axis=-1, keepdims=True)
    return ((yf - mu) * lax.rsqrt(var + EPS)).astype(y.dtype)


def alibi_slopes(n):
    start = 2.0 ** (-8.0 / n)
    return jnp.array([start ** (i + 1) for i in range(n)], dtype=jnp.float32)


def moba_attention(q, k, v):
    B, H, S, d = q.shape
    nb = S // MOBA_BLOCK
    topk = min(MOBA_TOPK, nb)
    kb = k.reshape(B, H, nb, MOBA_BLOCK, d)
    vb = v.reshape(B, H, nb, MOBA_BLOCK, d)
    kmean = jnp.mean(kb.astype(jnp.float32), axis=3).astype(k.dtype)
    slopes = alibi_slopes(H)[None, :, None, None]
    scale = d ** -0.5
    blk_ids = jnp.arange(nb)
    offs = jnp.arange(MOBA_BLOCK)
    gather = jax.vmap(jax.vmap(lambda blocks, idx: blocks[idx]))

    def one_qblock(c):
        start = c * MOBA_QBLOCK
        qc = lax.dynamic_slice_in_dim(q, start, MOBA_QBLOCK, axis=2)
        t = start + jnp.arange(MOBA_QBLOCK)
        cur = start // MOBA_BLOCK
        gate = jnp.einsum('bhqd,bhnd->bhqn', qc, kmean).astype(jnp.float32)
        gate = jnp.where(blk_ids < cur, gate, -jnp.inf)
        _, sel = lax.top_k(gate, topk)
        valid = sel < cur
        kg = gather(kb, sel)
        vg = gather(vb, sel)
        s_past = jnp.einsum('bhqd,bhqjsd->bhqjs', qc, kg).astype(jnp.float32) * scale
        kpos_past = sel[..., None] * MOBA_BLOCK + offs
        s_past = s_past - slopes[..., None] * (t[:, None, None] - kpos_past).astype(jnp.float32)
        s_past = jnp.where(valid[..., None], s_past, -jnp.inf).reshape(B, H, MOBA_QBLOCK, topk * MOBA_BLOCK)
        k_own = lax.dynamic_slice_in_dim(kb, cur, 1, axis=2)[:, :, 0]
        v_own = lax.dynamic_slice_in_dim(vb, cur, 1, axis=2)[:, :, 0]
        kpos_own = cur * MOBA_BLOCK + offs
        rel = (t[:, None] - kpos_own[None, :])
        s_own = jnp.einsum('bhqd,bhsd->bhqs', qc, k_own).astype(jnp.float32) * scale
        s_own = s_own - slopes * rel.astype(jnp.float32)
        s_own = jnp.where(rel >= 0, s_own, -jnp.inf)
        p = jax.nn.softmax(jnp.concatenate([s_past, s_own], axis=-1), axis=-1).astype(v.dtype)
        p_past = p[..., :topk * MOBA_BLOCK].reshape(B, H, MOBA_QBLOCK, topk, MOBA_BLOCK)
        p_own = p[..., topk * MOBA_BLOCK:]
        return (jnp.einsum('bhqjs,bhqjsd->bhqd', p_past, vg)
                + jnp.einsum('bhqs,bhsd->bhqd', p_own, v_own))

    out = lax.map(one_qblock, jnp.arange(S // MOBA_QBLOCK))
    return out.transpose(1, 2, 0, 3, 4).reshape(B, H, S, d)


def retention(q, k, v):
    B, H, S, dk = q.shape
    dv = v.shape[-1]
    C = R_CHUNK
    n = S // C
    dt = v.dtype
    log_g = jnp.log(1.0 - jnp.exp2(-5.0 - jnp.arange(H, dtype=jnp.float32)))
    i = jnp.arange(C, dtype=jnp.float32)
    diff = i[:, None] - i[None, :]
    decay_mask = jnp.where(diff >= 0, jnp.exp(log_g[:, None, None] * jnp.maximum(diff, 0.0)), 0.0).astype(dt)
    qc = q.reshape(B, H, n, C, dk)
    kc = (k * (dk ** -0.5)).reshape(B, H, n, C, dk)
    vc = v.reshape(B, H, n, C, dv)
    inner = jnp.einsum('bhnid,bhnjd->bhnij', qc, kc) * decay_mask[:, None]
    inner_out = jnp.einsum('bhnij,bhnje->bhnie', inner, vc)
    k_dec = jnp.exp(log_g[:, None] * (C - 1.0 - i)).astype(dt)
    chunk_kv = jnp.einsum('bhnjd,bhnje->nbhde', kc * k_dec[:, None, :, None], vc)
    chunk_decay = jnp.exp(log_g * C).astype(dt)[None, :, None, None]

    def step(state, kv):
        return state * chunk_decay + kv, state

    _, states = lax.scan(step, jnp.zeros((B, H, dk, dv), dt), chunk_kv)
    q_dec = jnp.exp(log_g[:, None] * (i + 1.0)).astype(dt)
    cross = jnp.einsum('bhnid,nbhde->bhnie', qc * q_dec[:, None, :, None], states)
    return (inner_out + cross).reshape(B, H, S, dv)


def hybrid_layer(x, w_in, w_o_attn, w_o_ret, w_out, q_gain, k_gain, g_mix, g_ffn, w_ffn_in, w_ffn_out):
    B, S, _ = x.shape
    pts, _ = _split_points()
    h = rms_norm(x, g_mix)
    proj = h @ w_in
    qa, ka, va, qr, kr, vr, gr, gate_a, gate_r = jnp.split(proj, pts, axis=-1)

    def heads(t, nh):
        return t.reshape(B, S, nh, -1).transpose(0, 2, 1, 3)

    oa = moba_attention(rms_norm(heads(qa, A_HEADS), q_gain),
                        rms_norm(heads(ka, A_HEADS), k_gain),
                        heads(va, A_HEADS))
    oa = oa.transpose(0, 2, 1, 3).reshape(B, S, A_WIDTH)
    orr = retention(heads(qr, R_HEADS), heads(kr, R_HEADS), heads(vr, R_HEADS))
    orr = head_group_norm(orr).transpose(0, 2, 1, 3).reshape(B, S, R_V_WIDTH) * jax.nn.silu(gr)
    merged = jax.nn.sigmoid(gate_a) * (oa @ w_o_attn) + jax.nn.sigmoid(gate_r) * (orr @ w_o_ret)
    x = x + merged @ w_out
    h2 = rms_norm(x, g_ffn)
    g, u = jnp.split(h2 @ w_ffn_in, 2, axis=-1)
    return x + (jax.nn.silu(g) * u) @ w_ffn_out


def setup_inputs(seed: int = 0) -> dict:
    key = jax.random.key(seed)
    ks = jax.random.split(key, 12)
    _, in_cols = _split_points()
    f32 = jnp.float32

    def w(k, shape, fan_in):
        return jax.random.normal(k, shape, f32) * (fan_in ** -0.5)

    def gain(k, shape):
        return 1.0 + 0.02 * jax.random.normal(k, shape, f32)

    return {
        "x": jax.random.normal(ks[0], (BATCH, SEQ, D_MODEL), f32),
        "w_in": w(ks[1], (DEPTH, D_MODEL, in_cols), D_MODEL),
        "w_o_attn": w(ks[2], (DEPTH, A_WIDTH, D_MODEL), A_WIDTH),
        "w_o_ret": w(ks[3], (DEPTH, R_V_WIDTH, D_MODEL), R_V_WIDTH),
        "w_out": w(ks[4], (DEPTH, D_MODEL, D_MODEL), D_MODEL),
        "q_norm": gain(ks[5], (DEPTH, A_HEAD_DIM)),
        "k_norm": gain(ks[6], (DEPTH, A_HEAD_DIM)),
        "norm_mix": gain(ks[7], (DEPTH, D_MODEL)),
        "norm_ffn": gain(ks[8], (DEPTH, D_MODEL)),
        "w_ffn_in": w(ks[9], (DEPTH, D_MODEL, 2 * D_FF), D_MODEL),
        "w_ffn_out": w(ks[10], (DEPTH, D_FF, D_MODEL), D_FF),
    }


def reference(x, w_in, w_o_attn, w_o_ret, w_out, q_norm, k_norm, norm_mix, norm_ffn, w_ffn_in, w_ffn_out):
    S = x.shape[1]
    s_pad = -(-S // SEQ_ALIGN) * SEQ_ALIGN
    h = jnp.pad(x, ((0, 0), (0, s_pad - S), (0, 0)))
    for l in range(DEPTH):
        h = hybrid_layer(h, w_in[l], w_o_attn[l], w_o_ret[l], w_out[l], q_norm[l], k_norm[l],
                         norm_mix[l], norm_ffn[l], w_ffn_in[l], w_ffn_out[l])
    return h[:, :S]
```

```python
import numpy as np
import ml_dtypes
from contextlib import ExitStack

import concourse.bass as bass
import concourse.mybir as mybir
from concourse.bass_utils import run_bass_kernel_spmd

F32 = mybir.dt.float32
BF16 = mybir.dt.bfloat16
AF = mybir.ActivationFunctionType
ALU = mybir.AluOpType
AX = mybir.AxisListType
NPBF = ml_dtypes.bfloat16

D = 2048
S_LEN = 8192
NB = 32
T = 2048
NT = 8
DEPTH = 4
DFF = 5632
EPS = 1e-6
NEG = -30000.0
DBG = {}
IN_COLS = 13312

ENGS = ("pe", "act", "dve", "pool", "sp")
SEM_EPOCH = 20000


class Ev:
    __slots__ = ("eng", "iid", "sem", "val", "buf")

    def __init__(self, eng, iid):
        self.eng, self.iid, self.sem, self.val, self.buf = eng, iid, None, None, None


class Buf:
    __slots__ = ("name", "w", "r", "dsem", "dcnt", "excl", "dq")

    def __init__(self, name, excl=False):
        self.name, self.w, self.r, self.dsem, self.dcnt, self.excl, self.dq = name, None, [], None, 0, excl, None


class Sched:
    def __init__(self, nc, need):
        self.nc = nc
        self.dry = nc is None
        self.need = need
        self.iid = 0
        self.stack = ExitStack()
        if not self.dry:
            self.eng = {"pe": nc.tensor, "act": nc.scalar, "dve": nc.vector, "pool": nc.gpsimd, "sp": nc.sync}
        self.sig = {e: [None, 0, 0] for e in ENGS}
        self.waited = {e: {} for e in ENGS}
        self.last = {e: None for e in ENGS}
        self.live_bufs = []
        self.sem_pool = {}
        self.nsem = 0

    def buf(self, name, excl=False):
        b = Buf(name, excl)
        self.live_bufs.append(b)
        return b

    def bufs(self, name, n, excl=False):
        return [self.buf(f"{name}{i}", excl) for i in range(n)]

    def pbufs(self, name, n):
        return self.bufs(name, n, True)

    def _new_sem(self, name):
        self.nsem += 1
        if self.dry:
            return ("sem", name, self.nsem)
        return self.stack.enter_context(self.nc.semaphore(f"{name}_{self.nsem}"))

    def _dma_sem(self, b, q):
        if b.dsem is None:
            b.dq = q
            pool = self.sem_pool.setdefault(q, [])
            if pool:
                b.dsem, b.dcnt = pool.pop()
            else:
                b.dsem, b.dcnt = self._new_sem("d" + q), 0
        assert b.dq == q, (b.name, b.dq, q)
        return b.dsem

    def _wait(self, eng, sem, val):
        key = id(sem) if not isinstance(sem, tuple) else sem
        if self.waited[eng].get(key, -1) >= val:
            return
        self.waited[eng][key] = val
        if not self.dry:
            self.eng[eng].wait_ge(sem, val)

    def _dep(self, eng, d, raw):
        if d is None:
            return
        if d.eng == "dma":
            self._wait(eng, d.buf.dsem, 16 * d.buf.dcnt)
            return
        if d.eng == eng and eng in ("pe", "sp"):
            return
        if self.dry:
            self.need.add(d.iid)
            return
        assert d.sem is not None, "dependency on non-signalling instruction"
        self._wait(eng, d.sem, d.val)

    def _collect(self, eng, reads, writes):
        for b in reads:
            self._dep(eng, b.w, True)
            if b.excl:
                for r in b.r:
                    self._dep(eng, r, False)
        for b in writes:
            self._dep(eng, b.w, False)
            for r in b.r:
                self._dep(eng, r, False)

    def _commit(self, ev, reads, writes):
        for b in reads:
            if b.excl:
                b.w = ev
                b.r = []
            else:
                b.r.append(ev)
        for b in writes:
            b.w = ev
            b.r = []

    def op(self, eng, fn, reads=(), writes=()):
        iid = self.iid
        self.iid += 1
        ev = Ev(eng, iid)
        self._collect(eng, reads, writes)
        if not self.dry:
            ins = fn(self.eng[eng])
            if iid in self.need:
                sg = self.sig[eng]
                if sg[0] is None or sg[1] >= SEM_EPOCH:
                    sg[0], sg[1] = self._new_sem(eng), 0
                sg[1] += 1
                ev.sem, ev.val = sg[0], sg[1]
                ins.then_inc(sg[0], 1)
        self.last[eng] = ev
        self._commit(ev, reads, writes)
        return ev

    def dma(self, q, out_fn, in_fn, sb, reads=(), writes=(), **kw):
        iid = self.iid
        self.iid += 1
        ev = Ev("dma", iid)
        ev.buf = sb
        self._collect(q, reads, writes)
        sem = self._dma_sem(sb, q)
        sb.dcnt += 1
        if not self.dry:
            self.eng[q].dma_start(out=out_fn(), in_=in_fn(), **kw).then_inc(sem, 16)
        self._commit(ev, reads, writes)
        return ev

    def barrier(self, final=False):
        lasts = dict(self.last)
        dbufs = [b for b in self.live_bufs if b.dsem is not None]
        for e in (("sp",) if final else ENGS):
            for f in ENGS:
                d = lasts[f]
                if d is None or f == e or f == "sp":
                    continue
                if self.dry:
                    self.need.add(d.iid)
                else:
                    self._wait(e, d.sem, d.val)
            for b in dbufs:
                self._wait(e, b.dsem, 16 * b.dcnt)
        for b in dbufs:
            self.sem_pool[b.dq].append((b.dsem, b.dcnt))
        self.live_bufs = []


class Phase:
    def __init__(self, S):
        self.S = S
        self.stack = ExitStack()

    def __enter__(self):
        return self

    def __exit__(self, *a):
        self.S.barrier()
        self.stack.close()
        return False

    def sbuf(self, name, shape, dt):
        if self.S.dry:
            return None
        return self.stack.enter_context(self.S.nc.sbuf_tensor("sb_" + name, list(shape), dt))

    def psum(self, name, shape, dt):
        if self.S.dry:
            return None
        return self.stack.enter_context(self.S.nc.psum_tensor("ps_" + name, list(shape), dt))


def block_of(i, j):
    return 4 * i + (j if i % 2 == 0 else 3 - j)


def log_g():
    return np.log(1.0 - np.exp2(-5.0 - np.arange(4, dtype=np.float64)))


def alibi_slopes():
    return np.array([0.5 ** (i + 1) for i in range(8)], dtype=np.float64)


class Gemm:
    def __init__(self, S, ph, KC, name="w", nslots=3):
        self.S, self.KC, self.ns = S, KC, nslots
        self.wb = [ph.sbuf(f"{name}b{i}", [128, KC, 512], BF16) for i in range(nslots)]
        self.Bw = S.bufs(f"B{name}", nslots)
        self.n = 0

    def load(self, w_ap_fn, c0, ncols=512, k0=0):
        S = self.S
        sl = self.n % self.ns
        self.n += 1
        KC = self.KC
        S.dma("pool",
              lambda: self.wb[sl][:, :, 0:ncols],
              lambda: w_ap_fn()[k0 * 128:(k0 + KC) * 128, c0:c0 + ncols].rearrange("(kc p) n -> p kc n", p=128),
              self.Bw[sl], writes=[self.Bw[sl]])
        return sl


def mm_group(S, out_fn, lhs_fns, rhs_fns, reads, Bout):
    n = len(lhs_fns)
    for i in range(n):
        S.op("pe", (lambda e, i=i: e.matmul(out_fn(), lhsT=lhs_fns[i](), rhs=rhs_fns[i](),
                                            start=(i == 0), stop=(i == n - 1))),
             reads=reads, writes=[Bout])


def norm_to_hT(S, ph, x_fn, gm, Bgm, ident, Bident, hT, BhT, pT, BpT, tag, epsc, Bepsc):
    xt = [ph.sbuf(f"{tag}xt{i}", [128, D], F32) for i in range(2)]
    xs = [ph.sbuf(f"{tag}xs{i}", [128, D], BF16) for i in range(2)]
    junk = ph.sbuf(f"{tag}junk", [128, D], BF16)
    ss = [ph.sbuf(f"{tag}ss{i}", [128, 1], F32) for i in range(2)]
    rs = [ph.sbuf(f"{tag}rs{i}", [128, 1], F32) for i in range(2)]
    Bxt, Bxs, Bss, Brs = S.bufs(tag + "Bxt", 2), S.bufs(tag + "Bxs", 2), S.bufs(tag + "Bss", 2), S.bufs(tag + "Brs", 2)
    Bjunk = S.buf(tag + "Bjunk")
    for tt in range(T // 128):
        k = tt % 2
        S.dma("sp", lambda k=k: xt[k][:], lambda tt=tt: x_fn()[tt * 128:(tt + 1) * 128, :], Bxt[k], writes=[Bxt[k]])
        S.op("dve", lambda e, k=k: e.memset(ss[k][:], 0.0), writes=[Bss[k]])
        S.op("act", lambda e, k=k: e.activation(out=junk[:], in_=xt[k][:], func=AF.Square, accum_out=ss[k][:]),
             reads=[Bxt[k], Bss[k]], writes=[Bjunk, Bss[k]])
        S.op("act", lambda e, k=k: e.activation(out=rs[k][:], in_=ss[k][:], func=AF.Sqrt, scale=1.0 / D, bias=epsc[:]),
             reads=[Bss[k], Bepsc], writes=[Brs[k]])
        S.op("dve", lambda e, k=k: e.reciprocal(out=rs[k][:], in_=rs[k][:]), reads=[Brs[k]], writes=[Brs[k]])
        S.op("act", lambda e, k=k: e.activation(out=xs[k][:], in_=xt[k][:], func=AF.Copy, scale=rs[k][:]),
             reads=[Bxt[k], Brs[k]], writes=[Bxs[k]])
        for half in range(2):
            for kk in range(8):
                kc = half * 8 + kk
                S.op("pe", lambda e, k=k, kk=kk, kc=kc, half=half: e.transpose(
                    out=pT[half][:, kk, :], in_=xs[k][:, kc * 128:(kc + 1) * 128], identity=ident[:]),
                     reads=[Bxs[k], Bident], writes=[BpT[half]])
            S.op("dve", lambda e, half=half, tt=tt: e.tensor_tensor(
                out=hT[:, half * 8:(half + 1) * 8, tt * 128:(tt + 1) * 128], in0=pT[half][:],
                in1=gm[:, half * 8:(half + 1) * 8].unsqueeze(2).to_broadcast([128, 8, 128]), op=ALU.mult),
                 reads=[BpT[half], Bgm], writes=[BhT])


def gen_p1(S, dr):
    with Phase(S) as ph:
        hT = ph.sbuf("hT", [128, 16, T], BF16)
        BhT = S.buf("BhT")
        ident = ph.sbuf("ident", [128, 128], BF16)
        ones = ph.sbuf("ones", [128, 128], BF16)
        gm = ph.sbuf("gm", [128, 16], F32)
        qg = ph.sbuf("qg", [128, 2], F32)
        qdec = ph.sbuf("qdec", [128, 4, 512], F32)
        kdec = ph.sbuf("kdec", [128, 2, 1024], F32)
        Bident, Bones, Bgm, Bqg, Bqdec, Bkdec = (S.buf(n) for n in ("Bident", "Bones", "Bgm", "Bqg", "Bqdec", "Bkdec"))
        S.dma("sp", lambda: ident[:], lambda: dr["ident"](), Bident, writes=[Bident])
        S.dma("sp", lambda: ones[:], lambda: dr["ones"](), Bones, writes=[Bones])
        S.dma("sp", lambda: gm[:], lambda: dr["gmix"](), Bgm, writes=[Bgm])
        S.dma("sp", lambda: qg[:], lambda: dr["qkg"](), Bqg, writes=[Bqg])
        S.dma("sp", lambda: qdec[:], lambda: dr["qdec"](), Bqdec, writes=[Bqdec])
        S.dma("sp", lambda: kdec[:], lambda: dr["kdec"](), Bkdec, writes=[Bkdec])
        epsc = ph.sbuf("epsc", [128, 1], F32)
        Bepsc = S.buf("Bepsc")
        S.op("dve", lambda e: e.memset(epsc[:], EPS), writes=[Bepsc])
        S.op("dve", lambda e: e.tensor_scalar(out=qg[:, 0:1], in0=qg[:, 0:1], scalar1=float(128 ** -0.5), scalar2=None,
                                              op0=ALU.mult), reads=[Bqg], writes=[Bqg])

        pT = [ph.psum(f"pT{i}", [128, 8, 128], BF16) for i in range(2)]
        BpT = S.pbufs("BpT", 2)
        acc = [ph.psum(f"acc{i}", [128, 512], F32) for i in range(4)]
        Bacc = S.pbufs("Bacc", 4)
        ssp = [ph.psum(f"ssp{i}", [128, 512], F32) for i in range(2)]
        Bssp = S.pbufs("Bssp", 2)

        G = Gemm(S, ph, 16)
        nblk = IN_COLS // 512
        slots = {}
        for b in range(2):
            slots[b] = G.load(dr["w_in"], b * 512)

        norm_to_hT(S, ph, dr["x"], gm, Bgm, ident, Bident, hT, BhT, pT, BpT, "n1", epsc, Bepsc)

        stg = [ph.sbuf(f"stg{i}", [128, T], BF16) for i in range(3)]
        Bstg = S.bufs("Bstg", 3)
        stt = [ph.sbuf(f"stt{i}", [128, 512], BF16) for i in range(4)]
        Bstt = S.bufs("Bstt", 4)
        sq = [ph.sbuf(f"sq{i}", [128, 512], BF16) for i in range(2)]
        Bsq = S.bufs("Bsq", 2)
        rr = [ph.sbuf(f"rr{i}", [128, 512], F32) for i in range(2)]
        Brr = S.bufs("Brr", 2)
        cnt = {"acc": 0, "stg": 0, "stt": 0, "sq": 0}

        def fm_block(sl, sub, kind, out_rows):
            outs = out_rows if isinstance(out_rows, list) else [out_rows]
            sgs = []
            for _ in outs:
                sgs.append(cnt["stg"] % 3)
                cnt["stg"] += 1
            for tb in range(4):
                a = cnt["acc"] % 4
                cnt["acc"] += 1
                mm_group(S, lambda a=a: acc[a][:],
                         [(lambda kc=kc: G.wb[sl][:, kc, sub * 128:(sub + 1) * 128]) for kc in range(16)],
                         [(lambda kc=kc, tb=tb: hT[:, kc, tb * 512:(tb + 1) * 512]) for kc in range(16)],
                         [G.Bw[sl], BhT], Bacc[a])
                cols = slice(tb * 512, (tb + 1) * 512)
                if kind in ("qn", "kn"):
                    q = cnt["sq"] % 2
                    cnt["sq"] += 1
                    gc = 0 if kind == "qn" else 1
                    S.op("act", lambda e, a=a, q=q: e.activation(out=sq[q][:], in_=acc[a][:], func=AF.Square),
                         reads=[Bacc[a]], writes=[Bsq[q]])
                    S.op("pe", lambda e, q=q: e.matmul(ssp[q][:], lhsT=ones[:], rhs=sq[q][:], start=True, stop=True),
                         reads=[Bones, Bsq[q]], writes=[Bssp[q]])
                    S.op("act", lambda e, q=q: e.activation(out=rr[q][:], in_=ssp[q][:], func=AF.Sqrt, scale=1.0 / 128, bias=epsc[:]),
                         reads=[Bssp[q], Bepsc], writes=[Brr[q]])
                    S.op("dve", lambda e, q=q: e.reciprocal(out=rr[q][:], in_=rr[q][:]), reads=[Brr[q]], writes=[Brr[q]])
                    S.op("dve", lambda e, a=a, q=q, gc=gc, cols=cols, sg=sgs[0]: e.scalar_tensor_tensor(
                        out=stg[sg][:, cols], in0=acc[a][:], scalar=qg[:, gc:gc + 1], in1=rr[q][:],
                        op0=ALU.mult, op1=ALU.mult), reads=[Bacc[a], Brr[q], Bqg], writes=[Bstg[sgs[0]]])
                elif kind.startswith("qr"):
                    h = int(kind[2])
                    S.op("act", lambda e, a=a, cols=cols, sg=sgs[0]: e.activation(out=stg[sg][:, cols], in_=acc[a][:], func=AF.Copy),
                         reads=[Bacc[a]], writes=[Bstg[sgs[0]]])
                    S.op("dve", lambda e, a=a, cols=cols, sg=sgs[1], h=h: e.tensor_tensor(
                        out=stg[sg][:, cols], in0=acc[a][:], in1=qdec[:, h, :], op=ALU.mult),
                         reads=[Bacc[a], Bqdec], writes=[Bstg[sgs[1]]])
                elif kind == "kr":
                    S.op("act", lambda e, a=a, cols=cols, sg=sgs[0]: e.activation(out=stg[sg][:, cols], in_=acc[a][:], func=AF.Copy, scale=1.0 / 16),
                         reads=[Bacc[a]], writes=[Bstg[sgs[0]]])
                elif kind == "silu":
                    S.op("act", lambda e, a=a, cols=cols, sg=sgs[0]: e.activation(out=stg[sg][:, cols], in_=acc[a][:], func=AF.Silu),
                         reads=[Bacc[a]], writes=[Bstg[sgs[0]]])
                elif kind == "sig":
                    S.op("act", lambda e, a=a, cols=cols, sg=sgs[0]: e.activation(out=stg[sg][:, cols], in_=acc[a][:], func=AF.Sigmoid),
                         reads=[Bacc[a]], writes=[Bstg[sgs[0]]])
                else:
                    raise ValueError(kind)
            for sg, o in zip(sgs, outs):
                S.dma("sp", o, lambda sg=sg: stg[sg][:], Bstg[sg], reads=[Bstg[sg]])

        def tm_block(sl, kind, out_fn):
            for tt in range(T // 128):
                a = cnt["acc"] % 4
                cnt["acc"] += 1
                mm_group(S, lambda a=a: acc[a][:],
                         [(lambda kc=kc, tt=tt: hT[:, kc, tt * 128:(tt + 1) * 128]) for kc in range(16)],
                         [(lambda kc=kc: G.wb[sl][:, kc, :]) for kc in range(16)],
                         [G.Bw[sl], BhT], Bacc[a])
                st = cnt["stt"] % 4
                cnt["stt"] += 1
                if kind == "v":
                    S.op("act", lambda e, a=a, st=st: e.activation(out=stt[st][:], in_=acc[a][:], func=AF.Copy),
                         reads=[Bacc[a]], writes=[Bstt[st]])
                else:
                    c0 = kind[1]
                    S.op("dve", lambda e, a=a, st=st, tt=tt, c0=c0: e.tensor_tensor(
                        out=stt[st][:], in0=acc[a][:], in1=kdec[:, tt % 2, c0:c0 + 512], op=ALU.mult),
                         reads=[Bacc[a], Bkdec], writes=[Bstt[st]])
                S.dma("sp", (lambda tt=tt: out_fn(tt)), lambda st=st: stt[st][:], Bstt[st], reads=[Bstt[st]])

        for b in DBG.get('blocks', range(nblk)):
            if b + 2 < nblk and 'blocks' not in DBG:
                slots[b + 2] = G.load(dr["w_in"], (b + 2) * 512)
            if 'blocks' in DBG:
                slots[b] = G.load(dr["w_in"], b * 512)
            sl = slots[b]
            c0 = b * 512
            if c0 < 1024:
                for sub in range(4):
                    hh = (c0 // 128) + sub
                    fm_block(sl, sub, "qn", (lambda hh=hh: dr["qaT"]()[hh * 128:(hh + 1) * 128, :]))
            elif c0 < 2048:
                for sub in range(4):
                    hh = ((c0 - 1024) // 128) + sub
                    fm_block(sl, sub, "kn", (lambda hh=hh: dr["kaT"]()[hh * 128:(hh + 1) * 128, :]))
            elif c0 < 3072:
                cc = c0 - 2048
                tm_block(sl, "v", (lambda tt, cc=cc: dr["va"]()[tt * 128:(tt + 1) * 128, cc:cc + 512]))
            elif c0 < 4096:
                for sub in range(4):
                    f = (c0 - 3072) // 128 + sub
                    fm_block(sl, sub, f"qr{f // 2}", [(lambda f=f: dr["qrT"]()[f * 128:(f + 1) * 128, :]),
                                                       (lambda f=f: dr["qdT"]()[f * 128:(f + 1) * 128, :])])
            elif c0 < 5120:
                for sub in range(4):
                    f = (c0 - 4096) // 128 + sub
                    fm_block(sl, sub, "kr", (lambda f=f: dr["krT"]()[f * 128:(f + 1) * 128, :]))
                cc = c0 - 4096
                tm_block(sl, ("kd", cc), (lambda tt, cc=cc: dr["kd"]()[tt * 128:(tt + 1) * 128, cc:cc + 512]))
            elif c0 < 7168:
                cc = c0 - 5120
                tm_block(sl, "v", (lambda tt, cc=cc: dr["vr"]()[tt * 128:(tt + 1) * 128, cc:cc + 512]))
            elif c0 < 9216:
                for sub in range(4):
                    f = (c0 - 7168) // 128 + sub
                    fm_block(sl, sub, "silu", (lambda f=f: dr["sgT"]()[f * 128:(f + 1) * 128, :]))
            elif c0 < 11264:
                for sub in range(4):
                    f = (c0 - 9216) // 128 + sub
                    fm_block(sl, sub, "sig", (lambda f=f: dr["gaT"]()[f * 128:(f + 1) * 128, :]))
            else:
                for sub in range(4):
                    f = (c0 - 11264) // 128 + sub
                    fm_block(sl, sub, "sig", (lambda f=f: dr["ggT"]()[f * 128:(f + 1) * 128, :]))


P1_OUTS = {"qaT": [1024, T], "kaT": [1024, T], "va": [T, 1024], "qrT": [1024, T], "qdT": [1024, T],
           "krT": [1024, T], "kd": [T, 1024], "vr": [T, 2048], "sgT": [2048, T], "gaT": [2048, T], "ggT": [2048, T]}
P1_INS = {"x": ([T, D], F32), "w_in": ([D, IN_COLS], F32), "ident": ([128, 128], BF16), "ones": ([128, 128], BF16),
          "gmix": ([128, 16], F32), "qkg": ([128, 2], F32), "qdec": ([128, 4, 512], F32), "kdec": ([128, 2, 1024], F32)}


def build_program(gen, ins, outs, scratch=None):
    need = set()
    scratch = scratch or {}
    S0 = Sched(None, need)
    gen(S0, {k: (lambda: None) for k in list(ins) + list(outs) + list(scratch)})
    S0.barrier(final=True)
    nc = bass.Bass("TRN2", target_bir_lowering=False)
    aps = {}
    for k, (shape, dt) in ins.items():
        aps[k] = nc.dram_tensor(k, list(shape), dt, kind="ExternalInput").ap()
    for k, shape in outs.items():
        dt = BF16
        if isinstance(shape, tuple):
            shape, dt = shape
        aps[k] = nc.dram_tensor(k, list(shape), dt, kind="ExternalOutput").ap()
    for k, (shape, dt) in scratch.items():
        aps[k] = nc.dram_tensor(k, list(shape), dt).ap()
    S = Sched(nc, need)
    with S.stack:
        gen(S, {k: (lambda k=k: aps[k]) for k in aps})
        S.barrier(final=True)
    assert S.iid == S0.iid, (S.iid, S0.iid)
    return nc, S


def p1_consts():
    lg = log_g()
    i = np.arange(256, dtype=np.float64)
    qd = np.exp(lg[:, None] * (i[None, :] + 1.0))
    qdec = np.tile(np.concatenate([qd, qd], axis=1)[None], (128, 1, 1)).astype(np.float32)
    p = np.arange(128)
    kdec = np.zeros((128, 2, 1024), np.float32)
    for s in range(2):
        for h in range(4):
            kdec[:, s, h * 256:(h + 1) * 256] = (np.exp(lg[h] * (255.0 - (s * 128 + p))) / 16.0)[:, None]
    return {"ident": np.eye(128, dtype=np.float32).astype(NPBF), "ones": np.ones((128, 128), np.float32).astype(NPBF),
            "qdec": qdec, "kdec": kdec}


P2_INS = {
    "x": ([T, D], F32),
    "qaT": ([1024, T], BF16), "kaT_all": ([1024, S_LEN], BF16), "vaug_all": ([8, 128, 64, 129], BF16),
    "kaT_loc": ([1024, T], BF16), "vaug_loc": ([8, 128, 16, 129], BF16),
    "qrT": ([1024, T], BF16), "qdT": ([1024, T], BF16), "krT": ([1024, T], BF16),
    "kd_all": ([32, 128, 2, 1024], BF16), "vr_all": ([32, 128, 2, 2048], BF16), "vr_loc": ([8, 128, 2, 2048], BF16),
    "sgT": ([2048, T], BF16), "gaT": ([2048, T], BF16), "ggT": ([2048, T], BF16),
    "w_o_attn": ([1024, D], F32), "w_o_ret": ([2048, D], F32), "w_out": ([D, D], F32),
    "w_ffn_in": ([D, 2 * DFF], F32), "w_ffn_out": ([DFF, D], F32), "gffn": ([128, 16], F32),
    "ident": ([128, 128], BF16), "ones": ([128, 128], BF16),
    "validb": ([128, 8, 32], F32), "tab2": ([128, 8, 8, 32], F32), "alibk": ([128, 8, 2], F32),
    "aq": ([64, 8, 256], BF16), "selm": ([64, 32, 128], BF16), "cm": ([128, 8, 2, 256], BF16),
    "dm": ([128, 4, 2, 256], F32), "wsel": ([128, 32], F32), "cdec": ([128, 4], F32),
}
P2_OUTS = {"xo": ([T, D], F32)}
P2_SCRATCH = {"oaT_d": ([1024, T], BF16), "m1T_d": ([2048, T], BF16), "orrT_d": ([2048, T], BF16),
              "mgT_d": ([2048, T], BF16), "x1_d": ([T, D], F32), "aT_d": ([DFF, T], BF16)}


def p2_attention(S, dr):
    with Phase(S) as ph:
        def cst(name, shape, dt):
            t = ph.sbuf("c_" + name, shape, dt)
            b = S.buf("Bc_" + name)
            S.dma("sp", lambda: t[:], lambda: dr[name](), b, writes=[b])
            return t, b
        ident, Bident = cst("ident", [128, 128], BF16)
        validb, Bvalidb = cst("validb", [128, 8, 32], F32)
        tab2, Btab2 = cst("tab2", [128, 8, 8, 32], F32)
        alibk, Balibk = cst("alibk", [128, 8, 2], F32)
        aq, Baq = cst("aq", [64, 8, 256], BF16)
        selm, Bselm = cst("selm", [64, 32, 128], BF16)
        cm, Bcm = cst("cm", [128, 8, 2, 256], BF16)

        KT = [ph.sbuf(f"KT{i}", [128, S_LEN], BF16) for i in range(2)]
        VA = [ph.sbuf(f"VA{i}", [128, 64, 129], BF16) for i in range(2)]
        QT = [ph.sbuf(f"QT{i}", [128, T], BF16) for i in range(2)]
        KL = [ph.sbuf(f"KL{i}", [128, T], BF16) for i in range(2)]
        VL = [ph.sbuf(f"VL{i}", [128, 16, 129], BF16) for i in range(2)]
        BKT, BVA, BQT, BKL, BVL = (S.bufs(n, 2) for n in ("BKT", "BVA", "BQT", "BKL", "BVL"))
        oaT = ph.sbuf("oaT", [128, 8, T], BF16)
        BoaT = S.bufs("BoaT", 8)
        km = ph.sbuf("km", [128, 32], F32)
        kmh = ph.sbuf("kmh", [128, 32], BF16)
        kmhf = ph.sbuf("kmhf", [128, 32], F32)
        kml = ph.sbuf("kml", [128, 32], BF16)
        Bkm, Bkmh, Bkmhf, Bkml = (S.buf(n) for n in ("Bkm", "Bkmh", "Bkmhf", "Bkml"))
        M = [ph.sbuf(f"M{i}", [64, 256], BF16) for i in range(2)]
        BM = S.bufs("BM", 2)
        g2 = ph.sbuf("g2", [128, 2, 32], F32)
        mx8 = ph.sbuf("mx8", [128, 2, 8], F32)
        msk = ph.sbuf("msk", [128, 2, 32], F32)
        mrow = ph.sbuf("mrow", [128, 2, 32], BF16)
        Bg2, Bmx8, Bmsk, Bmrow = (S.buf(n) for n in ("Bg2", "Bmx8", "Bmsk", "Bmrow"))
        PT = [ph.sbuf(f"PT{i}", [128, 256], BF16) for i in range(4)]
        BPT = S.bufs("BPT", 4)
        rden = ph.sbuf("rden", [128, 2], F32)
        Brden = S.buf("Brden")
        on = [ph.sbuf(f"on{i}", [128, 128], BF16) for i in range(2)]
        Bon = S.bufs("Bon", 2)

        sT = [ph.psum(f"sT{i}", [128, 512], F32) for i in range(3)]
        BsT = S.pbufs("BsT", 3)
        Ops = [ph.psum(f"O{i}", [128, 512], F32) for i in range(2)]
        BO = S.pbufs("BO", 2)
        gp = ph.psum("gp", [128, 2, 32], F32)
        Bgp = S.pbufs("Bgp", 1)[0]
        mT = ph.psum("mT", [32, 256], BF16)
        BmT = S.pbufs("BmT", 1)[0]
        oT = ph.psum("oT", [128, 2, 128], BF16)
        BoT = S.pbufs("BoT", 1)[0]

        def load_head(h):
            k = h % 2
            S.dma("sp", lambda: KT[k][:], lambda: dr["kaT_all"]()[h * 128:(h + 1) * 128, :], BKT[k], writes=[BKT[k]])
            S.dma("sp", lambda: VA[k][:], lambda: dr["vaug_all"]()[h], BVA[k], writes=[BVA[k]])
            S.dma("sp", lambda: QT[k][:], lambda: dr["qaT"]()[h * 128:(h + 1) * 128, :], BQT[k], writes=[BQT[k]])
            S.dma("sp", lambda: KL[k][:], lambda: dr["kaT_loc"]()[h * 128:(h + 1) * 128, :], BKL[k], writes=[BKL[k]])
            S.dma("sp", lambda: VL[k][:], lambda: dr["vaug_loc"]()[h], BVL[k], writes=[BVL[k]])

        load_head(0)
        cnt = {"s": 0, "p": 0, "on": 0}
        for h in range(8):
            k = h % 2
            if h + 1 < 8:
                load_head(h + 1)
            S.op("dve", lambda e, k=k: e.tensor_reduce(out=km[:], in_=KT[k][:].rearrange("p (n s) -> p n s", s=256),
                                                       axis=AX.X, op=ALU.add), reads=[BKT[k]], writes=[Bkm])
            S.op("dve", lambda e: e.tensor_scalar(out=km[:], in0=km[:], scalar1=1.0 / 256, scalar2=None, op0=ALU.mult),
                 reads=[Bkm], writes=[Bkm])
            S.op("dve", lambda e: e.tensor_copy(out=kmh[:], in_=km[:]), reads=[Bkm], writes=[Bkmh])
            S.op("dve", lambda e: e.tensor_copy(out=kmhf[:], in_=kmh[:]), reads=[Bkmh], writes=[Bkmhf])
            S.op("dve", lambda e: e.tensor_tensor(out=kml[:], in0=km[:], in1=kmhf[:], op=ALU.subtract),
                 reads=[Bkm, Bkmhf], writes=[Bkml])
            for par in range(2):
                S.op("dve", lambda e, par=par, h=h: e.tensor_copy(out=M[par][32:64, :], in_=aq[32:64, h, :]),
                     reads=[Baq], writes=[BM[par]])
            for i in range(NT):
                par = i % 2
                qcols = slice(i * 256, (i + 1) * 256)
                for s in range(2):
                    qs = slice(i * 256 + s * 128, i * 256 + (s + 1) * 128)
                    S.op("pe", lambda e, s=s, qs=qs, k=k: e.matmul(gp[:, s, :], lhsT=QT[k][:, qs], rhs=kmh[:], start=True, stop=False),
                         reads=[BQT[k], Bkmh], writes=[Bgp])
                    S.op("pe", lambda e, s=s, qs=qs, k=k: e.matmul(gp[:, s, :], lhsT=QT[k][:, qs], rhs=kml[:], start=False, stop=True),
                         reads=[BQT[k], Bkml], writes=[Bgp])
                S.op("dve", lambda e, i=i: e.tensor_tensor(out=g2[:], in0=gp[:], in1=validb[:, i:i + 1, :].to_broadcast([128, 2, 32]),
                                                          op=ALU.add), reads=[Bgp, Bvalidb], writes=[Bg2])
                for s in range(2):
                    S.op("dve", lambda e, s=s: e.max(out=mx8[:, s, :], in_=g2[:, s, :]), reads=[Bg2], writes=[Bmx8])
                for s in range(2):
                    S.op("dve", lambda e, s=s: e.tensor_scalar(out=msk[:, s, :], in0=g2[:, s, :], scalar1=mx8[:, s, 2:3], scalar2=30000.0,
                                                              op0=ALU.is_ge, op1=ALU.mult), reads=[Bg2, Bmx8], writes=[Bmsk])
                S.op("dve", lambda e, i=i, h=h: e.tensor_tensor(out=mrow[:], in0=msk[:], in1=tab2[:, i, h:h + 1, :].to_broadcast([128, 2, 32]),
                                                              op=ALU.add), reads=[Bmsk, Btab2], writes=[Bmrow])
                for s in range(2):
                    S.op("pe", lambda e, s=s: e.transpose(out=mT[:, s * 128:(s + 1) * 128], in_=mrow[:, s, :], identity=ident[:]),
                         reads=[Bmrow, Bident], writes=[BmT])
                S.op("dve", lambda e, par=par: e.tensor_copy(out=M[par][0:32, :], in_=mT[:]), reads=[BmT], writes=[BM[par]])
                nblk = 4 * i + 3
                steps = [(n, c) for n in range(nblk) for c in range(2)] + [("own", 0), ("own", 1)]
                first = [True, True]
                for (n, c) in steps:
                    r = cnt["s"] % 3
                    cnt["s"] += 1
                    p = cnt["p"] % 4
                    cnt["p"] += 1
                    if n == "own":
                        ks = slice(i * 256 + c * 128, i * 256 + (c + 1) * 128)
                        S.op("pe", lambda e, r=r, ks=ks, k=k, qcols=qcols: e.matmul(sT[r][:, 0:256], lhsT=KL[k][:, ks], rhs=QT[k][:, qcols], start=True, stop=False),
                             reads=[BKL[k], BQT[k]], writes=[BsT[r]])
                        S.op("pe", lambda e, r=r, c=c, h=h: e.matmul(sT[r][:, 0:256], lhsT=ident[:], rhs=cm[:, h, c, :], start=False, stop=True),
                             reads=[Bident, Bcm], writes=[BsT[r]])
                    else:
                        ks = slice((2 * n + c) * 128, (2 * n + c + 1) * 128)
                        S.op("pe", lambda e, r=r, ks=ks, k=k, qcols=qcols: e.matmul(sT[r][:, 0:256], lhsT=KT[k][:, ks], rhs=QT[k][:, qcols], start=True, stop=False),
                             reads=[BKT[k], BQT[k]], writes=[BsT[r]])
                        S.op("pe", lambda e, r=r, n=n, par=par: e.matmul(sT[r][:, 0:256], lhsT=selm[:, n, :], rhs=M[par][:], start=False, stop=True),
                             reads=[Bselm, BM[par]], writes=[BsT[r]])
                    S.op("act", lambda e, r=r, p=p, h=h, c=c: e.activation(out=PT[p][:], in_=sT[r][:, 0:256], func=AF.Exp, bias=alibk[:, h, c:c + 1]),
                         reads=[BsT[r], Balibk], writes=[BPT[p]])
                    for s in range(2):
                        if n == "own" and c == 1 and s == 0:
                            continue
                        last = (n == "own") and (c == 1 or (c == 0 and s == 0))
                        if n == "own":
                            rhs_fn = (lambda k=k, i=i, c=c: VL[k][:, 2 * i + c, :])
                            rb = BVL[k]
                        else:
                            rhs_fn = (lambda k=k, n=n, c=c: VA[k][:, 2 * n + c, :])
                            rb = BVA[k]
                        S.op("pe", lambda e, s=s, p=p, rhs_fn=rhs_fn, st=first[s], last=last: e.matmul(
                            Ops[s][:, 0:129], lhsT=PT[p][:, s * 128:(s + 1) * 128], rhs=rhs_fn(), start=st, stop=last),
                             reads=[BPT[p], rb], writes=[BO[s]])
                        first[s] = False
                for s in range(2):
                    o = cnt["on"] % 2
                    cnt["on"] += 1
                    S.op("dve", lambda e, s=s: e.reciprocal(out=rden[:, s:s + 1], in_=Ops[s][:, 128:129]), reads=[BO[s]], writes=[Brden])
                    S.op("dve", lambda e, s=s, o=o: e.tensor_scalar(out=on[o][:], in0=Ops[s][:, 0:128], scalar1=rden[:, s:s + 1], scalar2=None,
                                                                   op0=ALU.mult), reads=[BO[s], Brden], writes=[Bon[o]])
                    S.op("pe", lambda e, s=s, o=o: e.transpose(out=oT[:, s, :], in_=on[o][:], identity=ident[:]),
                         reads=[Bon[o], Bident], writes=[BoT])
                S.op("act", lambda e, h=h, qcols=qcols: e.activation(out=oaT[:, h, qcols], in_=oT[:].rearrange("p s q -> p (s q)"), func=AF.Copy),
                     reads=[BoT], writes=[BoaT[h]])
            S.dma("sp", lambda h=h: dr["oaT_d"]()[h * 128:(h + 1) * 128, :], lambda h=h: oaT[:, h, :], BoaT[h], reads=[BoaT[h]])


def p2_oattn(S, dr):
    with Phase(S) as ph:
        oaT = ph.sbuf("oaT2", [128, 8, T], BF16)
        BoaT = S.buf("BoaT2")
        S.dma("sp", lambda: oaT[:], lambda: dr["oaT_d"]().rearrange("(kc p) t -> p kc t", p=128), BoaT, writes=[BoaT])
        G = Gemm(S, ph, 8, "wa")
        acc = [ph.psum(f"acca{i}", [128, 512], F32) for i in range(4)]
        Bacc = S.pbufs("Bacca", 4)
        gt = [ph.sbuf(f"gta{i}", [128, T], BF16) for i in range(2)]
        Bgt = S.bufs("Bgta", 2)
        stg = [ph.sbuf(f"stga{i}", [128, T], BF16) for i in range(2)]
        Bstg = S.bufs("Bstga", 2)
        slots = {0: G.load(dr["w_o_attn"], 0), 1: G.load(dr["w_o_attn"], 512)}
        na = 0
        for b in range(4):
            if b + 2 < 4:
                slots[b + 2] = G.load(dr["w_o_attn"], (b + 2) * 512)
            sl = slots[b]
            for sub in range(4):
                fc = b * 4 + sub
                g = fc % 2
                S.dma("sp", lambda g=g: gt[g][:], lambda fc=fc: dr["gaT"]()[fc * 128:(fc + 1) * 128, :], Bgt[g], writes=[Bgt[g]])
                for tb in range(4):
                    a = na % 4
                    na += 1
                    cols = slice(tb * 512, (tb + 1) * 512)
                    mm_group(S, lambda a=a: acc[a][:],
                             [(lambda kc=kc, sl=sl, sub=sub: G.wb[sl][:, kc, sub * 128:(sub + 1) * 128]) for kc in range(8)],
                             [(lambda kc=kc, cols=cols: oaT[:, kc, cols]) for kc in range(8)],
                             [G.Bw[sl], BoaT], Bacc[a])
                    S.op("dve", lambda e, a=a, g=g, cols=cols: e.tensor_tensor(out=stg[g][:, cols], in0=acc[a][:], in1=gt[g][:, cols], op=ALU.mult),
                         reads=[Bacc[a], Bgt[g]], writes=[Bstg[g]])
                S.dma("sp", lambda fc=fc: dr["m1T_d"]()[fc * 128:(fc + 1) * 128, :], lambda g=g: stg[g][:], Bstg[g], reads=[Bstg[g]])


def p2_retention(S, dr):
    with Phase(S) as ph:
        def cst(name, shape, dt):
            t = ph.sbuf("c_" + name, shape, dt)
            b = S.buf("Bc_" + name)
            S.dma("sp", lambda: t[:], lambda: dr[name](), b, writes=[b])
            return t, b
        ones, Bones = cst("ones", [128, 128], BF16)
        dm, Bdm = cst("dm", [128, 4, 2, 256], F32)
        wsel, Bwsel = cst("wsel", [128, 32], F32)
        cdec, Bcdec = cst("cdec", [128, 4], F32)
        epsc = ph.sbuf("epsc", [128, 1], F32)
        Bepsc = S.buf("Bepsc")
        S.op("dve", lambda e: e.memset(epsc[:], EPS), writes=[Bepsc])

        St = ph.sbuf("St", [128, 4, 2, 512], F32)
        Sacc = ph.sbuf("Sacc", [128, 4, 2, 512], F32)
        Ssel = ph.sbuf("Ssel", [128, 4, 2, 512], BF16)
        BSt, BSacc, BSsel = S.buf("BSt"), S.buf("BSacc"), S.buf("BSsel")
        S.op("dve", lambda e: e.memset(St[:], 0.0), writes=[BSt])
        kdc = [ph.sbuf(f"kdc{i}", [128, 2, 1024], BF16) for i in range(2)]
        vrc = [ph.sbuf(f"vrc{i}", [128, 2, 2048], BF16) for i in range(2)]
        Bkdc, Bvrc = S.bufs("Bkdc", 2), S.bufs("Bvrc", 2)
        qT = [ph.sbuf(f"rq{i}", [128, 8, 256], BF16) for i in range(2)]
        qdT = [ph.sbuf(f"rqd{i}", [128, 8, 256], BF16) for i in range(2)]
        kT = [ph.sbuf(f"rk{i}", [128, 8, 256], BF16) for i in range(2)]
        vl = [ph.sbuf(f"rvl{i}", [128, 2, 2048], BF16) for i in range(2)]
        sg = [ph.sbuf(f"rsg{i}", [128, 16, 256], BF16) for i in range(2)]
        BqT, BqdT, BkT, Bvl, Bsg = (S.bufs(n, 2) for n in ("BrqT", "BrqdT", "BrkT", "Brvl", "Brsg"))
        innm = [ph.sbuf(f"innm{i}", [128, 2, 256], BF16) for i in range(2)]
        Binnm = S.bufs("Binnm", 2)
        Y = [ph.sbuf(f"Y{i}", [128, 4, 256], F32) for i in range(2)]
        Yb = [ph.sbuf(f"Yb{i}", [128, 4, 256], BF16) for i in range(2)]
        Ysq = [ph.sbuf(f"Ysq{i}", [128, 4, 256], BF16) for i in range(2)]
        BY, BYb, BYsq = S.bufs("BY", 2), S.bufs("BYb", 2), S.bufs("BYsq", 2)
        mu = ph.sbuf("mu", [128, 256], F32)
        msq = ph.sbuf("msq", [128, 256], F32)
        var = ph.sbuf("var", [128, 256], F32)
        Bmu, Bmsq, Bvar = S.buf("Bmu"), S.buf("Bmsq"), S.buf("Bvar")
        t1 = ph.sbuf("t1", [128, 4, 256], F32)
        Bt1 = S.buf("Bt1")
        ost = [ph.sbuf(f"ost{i}", [128, 4, 256], BF16) for i in range(2)]
        Bost = S.bufs("Bost", 2)

        inn = [ph.psum(f"inn{i}", [128, 2, 256], F32) for i in range(2)]
        Binn = S.pbufs("Binn", 2)
        yps = [ph.psum(f"yps{i}", [128, 2, 256], F32) for i in range(2)]
        Byps = S.pbufs("Byps", 2)
        stp = ph.psum("stp", [128, 2, 256], F32)
        Bstp = S.pbufs("Bstp", 1)[0]
        kvp = [ph.psum(f"kvp{i}", [128, 512], F32) for i in range(2)]
        Bkvp = S.pbufs("Bkvp", 2)

        def load_chunk(n):
            k = n % 2
            S.dma("sp", lambda: kdc[k][:], lambda: dr["kd_all"]()[n], Bkdc[k], writes=[Bkdc[k]])
            S.dma("sp", lambda: vrc[k][:], lambda: dr["vr_all"]()[n], Bvrc[k], writes=[Bvrc[k]])

        def load_tile(i):
            k = i % 2
            cs = slice(i * 256, (i + 1) * 256)
            for dst, Bd, nm in ((qT, BqT, "qrT"), (qdT, BqdT, "qdT"), (kT, BkT, "krT")):
                S.dma("sp", lambda dst=dst: dst[k][:], lambda nm=nm: dr[nm]()[:, cs].rearrange("(fc p) t -> p fc t", p=128), Bd[k], writes=[Bd[k]])
            S.dma("sp", lambda: vl[k][:], lambda: dr["vr_loc"]()[i], Bvl[k], writes=[Bvl[k]])
            S.dma("sp", lambda: sg[k][:], lambda: dr["sgT"]()[:, cs].rearrange("(fc p) t -> p fc t", p=128), Bsg[k], writes=[Bsg[k]])

        load_chunk(0)
        load_tile(0)
        cnt = {"hh": 0}
        for n in range(NB):
            i, m = divmod(n, 4)
            if n + 1 < NB - 0 and n + 1 <= 30:
                load_chunk(n + 1)
            w_ap = (lambda n=n: wsel[:, n:n + 1])
            if m == 0:
                S.op("dve", lambda e, w_ap=w_ap: e.tensor_scalar(out=Sacc[:], in0=St[:], scalar1=w_ap(), scalar2=None, op0=ALU.mult),
                     reads=[BSt, Bwsel], writes=[BSacc])
            elif m < 3:
                S.op("dve", lambda e, w_ap=w_ap: e.scalar_tensor_tensor(out=Sacc[:], in0=St[:], scalar=w_ap(), in1=Sacc[:], op0=ALU.mult, op1=ALU.add),
                     reads=[BSt, Bwsel, BSacc], writes=[BSacc])
            else:
                S.op("dve", lambda e, w_ap=w_ap: e.scalar_tensor_tensor(out=Ssel[:], in0=St[:], scalar=w_ap(), in1=Sacc[:], op0=ALU.mult, op1=ALU.add),
                     reads=[BSt, Bwsel, BSacc], writes=[BSsel])
            if m == 3:
                if i + 1 < NT:
                    load_tile(i + 1)
                k = i % 2
                for h in range(4):
                    hh = cnt["hh"] % 2
                    cnt["hh"] += 1
                    for js in range(2):
                        for dc in range(2):
                            S.op("pe", lambda e, hh=hh, js=js, dc=dc, h=h, k=k: e.matmul(
                                inn[hh][:, js, :], lhsT=kT[k][:, h * 2 + dc, js * 128:(js + 1) * 128], rhs=qT[k][:, h * 2 + dc, :],
                                start=(dc == 0), stop=(dc == 1)), reads=[BkT[k], BqT[k]], writes=[Binn[hh]])
                    S.op("dve", lambda e, hh=hh, h=h: e.tensor_tensor(out=innm[hh][:], in0=inn[hh][:], in1=dm[:, h, :, :], op=ALU.mult),
                         reads=[Binn[hh], Bdm], writes=[Binnm[hh]])
                    for ec in range(4):
                        pb, pe_ = divmod(ec, 2)
                        lhs = [(lambda js=js, ec=ec, h=h, k=k: vl[k][:, js, h * 512 + ec * 128:h * 512 + (ec + 1) * 128]) for js in range(2)] + \
                              [(lambda dc=dc, ec=ec, h=h: Ssel[:, h, dc, ec * 128:(ec + 1) * 128]) for dc in range(2)]
                        rhs = [(lambda js=js, hh=hh: innm[hh][:, js, :]) for js in range(2)] + \
                              [(lambda dc=dc, h=h, k=k: qdT[k][:, h * 2 + dc, :]) for dc in range(2)]
                        mm_group(S, (lambda pb=pb, pe_=pe_: yps[pb][:, pe_, :]), lhs, rhs, [Bvl[k], Binnm[hh], BSsel, BqdT[k]], Byps[pb])
                    for pb in range(2):
                        S.op("act", lambda e, pb=pb, hh=hh: e.activation(out=Y[hh][:, pb * 2:(pb + 1) * 2, :], in_=yps[pb][:], func=AF.Copy),
                             reads=[Byps[pb]], writes=[BY[hh]])
                    S.op("dve", lambda e, hh=hh: e.tensor_copy(out=Yb[hh][:], in_=Y[hh][:]), reads=[BY[hh]], writes=[BYb[hh]])
                    S.op("act", lambda e, hh=hh: e.activation(out=Ysq[hh][:], in_=Y[hh][:], func=AF.Square), reads=[BY[hh]], writes=[BYsq[hh]])
                    mm_group(S, lambda: stp[:, 0, :], [(lambda: ones[:])] * 4, [(lambda ec=ec, hh=hh: Yb[hh][:, ec, :]) for ec in range(4)],
                             [Bones, BYb[hh]], Bstp)
                    mm_group(S, lambda: stp[:, 1, :], [(lambda: ones[:])] * 4, [(lambda ec=ec, hh=hh: Ysq[hh][:, ec, :]) for ec in range(4)],
                             [Bones, BYsq[hh]], Bstp)
                    S.op("dve", lambda e: e.tensor_scalar(out=mu[:], in0=stp[:, 0, :], scalar1=1.0 / 512, scalar2=None, op0=ALU.mult),
                         reads=[Bstp], writes=[Bmu])
                    S.op("dve", lambda e: e.tensor_tensor(out=msq[:], in0=mu[:], in1=mu[:], op=ALU.mult), reads=[Bmu], writes=[Bmsq])
                    S.op("dve", lambda e: e.scalar_tensor_tensor(out=var[:], in0=stp[:, 1, :], scalar=1.0 / 512, in1=msq[:], op0=ALU.mult, op1=ALU.subtract),
                         reads=[Bstp, Bmsq], writes=[Bvar])
                    S.op("act", lambda e: e.activation(out=var[:], in_=var[:], func=AF.Sqrt, bias=epsc[:]), reads=[Bvar, Bepsc], writes=[Bvar])
                    S.op("dve", lambda e: e.reciprocal(out=var[:], in_=var[:]), reads=[Bvar], writes=[Bvar])
                    S.op("dve", lambda e, hh=hh: e.tensor_tensor(out=t1[:], in0=Y[hh][:], in1=mu[:].unsqueeze(1).to_broadcast([128, 4, 256]), op=ALU.subtract),
                         reads=[BY[hh], Bmu], writes=[Bt1])
                    S.op("dve", lambda e: e.tensor_tensor(out=t1[:], in0=t1[:], in1=var[:].unsqueeze(1).to_broadcast([128, 4, 256]), op=ALU.mult),
                         reads=[Bt1, Bvar], writes=[Bt1])
                    S.op("dve", lambda e, hh=hh, h=h, k=k: e.tensor_tensor(out=ost[hh][:], in0=t1[:], in1=sg[k][:, h * 4:(h + 1) * 4, :], op=ALU.mult),
                         reads=[Bt1, Bsg[k]], writes=[Bost[hh]])
                    S.dma("sp", lambda h=h, i=i: dr["orrT_d"]()[h * 512:(h + 1) * 512, i * 256:(i + 1) * 256].rearrange("(ec p) t -> p ec t", p=128),
                          lambda hh=hh: ost[hh][:], Bost[hh], reads=[Bost[hh]])
            if n <= 30:
                kk = n % 2
                for h in range(4):
                    for dc in range(2):
                        mm_group(S, (lambda dc=dc: kvp[dc][:]),
                                 [(lambda js=js, h=h, dc=dc, kk=kk: kdc[kk][:, js, h * 256 + dc * 128:h * 256 + (dc + 1) * 128]) for js in range(2)],
                                 [(lambda js=js, h=h, kk=kk: vrc[kk][:, js, h * 512:(h + 1) * 512]) for js in range(2)],
                                 [Bkdc[kk], Bvrc[kk]], Bkvp[dc])
                        S.op("dve", lambda e, h=h, dc=dc: e.scalar_tensor_tensor(out=St[:, h, dc, :], in0=St[:, h, dc, :], scalar=cdec[:, h:h + 1],
                                                                                in1=kvp[dc][:], op0=ALU.mult, op1=ALU.add),
                             reads=[BSt, Bcdec, Bkvp[dc]], writes=[BSt])


def p2_oret(S, dr):
    with Phase(S) as ph:
        aT = ph.sbuf("orrT", [128, 16, T], BF16)
        BaT = S.buf("BorrT")
        S.dma("sp", lambda: aT[:], lambda: dr["orrT_d"]().rearrange("(kc p) t -> p kc t", p=128), BaT, writes=[BaT])
        G = Gemm(S, ph, 16, "wr")
        acc = [ph.psum(f"accr{i}", [128, 512], F32) for i in range(4)]
        Bacc = S.pbufs("Baccr", 4)
        gt = [ph.sbuf(f"gtr{i}", [128, T], BF16) for i in range(2)]
        m1 = [ph.sbuf(f"m1r{i}", [128, T], BF16) for i in range(2)]
        Bgt, Bm1 = S.bufs("Bgtr", 2), S.bufs("Bm1r", 2)
        tmp = [ph.sbuf(f"tmpr{i}", [128, 512], F32) for i in range(2)]
        Btmp = S.bufs("Btmpr", 2)
        stg = [ph.sbuf(f"stgr{i}", [128, T], BF16) for i in range(2)]
        Bstg = S.bufs("Bstgr", 2)
        slots = {0: G.load(dr["w_o_ret"], 0), 1: G.load(dr["w_o_ret"], 512)}
        na = 0
        for b in range(4):
            if b + 2 < 4:
                slots[b + 2] = G.load(dr["w_o_ret"], (b + 2) * 512)
            sl = slots[b]
            for sub in range(4):
                fc = b * 4 + sub
                g = fc % 2
                S.dma("sp", lambda g=g: gt[g][:], lambda fc=fc: dr["ggT"]()[fc * 128:(fc + 1) * 128, :], Bgt[g], writes=[Bgt[g]])
                S.dma("sp", lambda g=g: m1[g][:], lambda fc=fc: dr["m1T_d"]()[fc * 128:(fc + 1) * 128, :], Bm1[g], writes=[Bm1[g]])
                for tb in range(4):
                    a = na % 4
                    tq = na % 2
                    na += 1
                    cols = slice(tb * 512, (tb + 1) * 512)
                    mm_group(S, lambda a=a: acc[a][:],
                             [(lambda kc=kc, sl=sl, sub=sub: G.wb[sl][:, kc, sub * 128:(sub + 1) * 128]) for kc in range(16)],
                             [(lambda kc=kc, cols=cols: aT[:, kc, cols]) for kc in range(16)],
                             [G.Bw[sl], BaT], Bacc[a])
                    S.op("dve", lambda e, a=a, g=g, cols=cols, tq=tq: e.tensor_tensor(out=tmp[tq][:], in0=acc[a][:], in1=gt[g][:, cols], op=ALU.mult),
                         reads=[Bacc[a], Bgt[g]], writes=[Btmp[tq]])
                    S.op("dve", lambda e, g=g, cols=cols, tq=tq: e.tensor_tensor(out=stg[g][:, cols], in0=tmp[tq][:], in1=m1[g][:, cols], op=ALU.add),
                         reads=[Btmp[tq], Bm1[g]], writes=[Bstg[g]])
                S.dma("sp", lambda fc=fc: dr["mgT_d"]()[fc * 128:(fc + 1) * 128, :], lambda g=g: stg[g][:], Bstg[g], reads=[Bstg[g]])


def p2_wout(S, dr):
    with Phase(S) as ph:
        aT = ph.sbuf("mgT", [128, 16, T], BF16)
        BaT = S.buf("BmgT")
        S.dma("sp", lambda: aT[:], lambda: dr["mgT_d"]().rearrange("(kc p) t -> p kc t", p=128), BaT, writes=[BaT])
        G = Gemm(S, ph, 16, "wo")
        acc = [ph.psum(f"acco{i}", [128, 512], F32) for i in range(4)]
        Bacc = S.pbufs("Bacco", 4)
        xt = [ph.sbuf(f"xto{i}", [128, 512], F32) for i in range(3)]
        Bxt = S.bufs("Bxto", 3)
        so = [ph.sbuf(f"soo{i}", [128, 512], F32) for i in range(3)]
        Bso = S.bufs("Bsoo", 3)
        slots = {0: G.load(dr["w_out"], 0), 1: G.load(dr["w_out"], 512)}
        na = 0
        for b in range(4):
            if b + 2 < 4:
                slots[b + 2] = G.load(dr["w_out"], (b + 2) * 512)
            sl = slots[b]
            cs = slice(b * 512, (b + 1) * 512)
            for tt in range(T // 128):
                a = na % 4
                q = na % 3
                na += 1
                rows = slice(tt * 128, (tt + 1) * 128)
                S.dma("sp", lambda q=q: xt[q][:], lambda rows=rows, cs=cs: dr["x"]()[rows, cs], Bxt[q], writes=[Bxt[q]])
                mm_group(S, lambda a=a: acc[a][:],
                         [(lambda kc=kc, rows=rows: aT[:, kc, rows]) for kc in range(16)],
                         [(lambda kc=kc, sl=sl: G.wb[sl][:, kc, :]) for kc in range(16)],
                         [G.Bw[sl], BaT], Bacc[a])
                S.op("dve", lambda e, a=a, q=q: e.tensor_tensor(out=so[q][:], in0=acc[a][:], in1=xt[q][:], op=ALU.add),
                     reads=[Bacc[a], Bxt[q]], writes=[Bso[q]])
                S.dma("sp", lambda rows=rows, cs=cs: dr["x1_d"]()[rows, cs], lambda q=q: so[q][:], Bso[q], reads=[Bso[q]])


def p2_ffn_in(S, dr):
    with Phase(S) as ph:
        hT = ph.sbuf("h2T", [128, 16, T], BF16)
        BhT = S.buf("Bh2T")
        ident = ph.sbuf("identf", [128, 128], BF16)
        gm = ph.sbuf("gm2", [128, 16], F32)
        Bident, Bgm = S.buf("Bidentf"), S.buf("Bgm2")
        S.dma("sp", lambda: ident[:], lambda: dr["ident"](), Bident, writes=[Bident])
        S.dma("sp", lambda: gm[:], lambda: dr["gffn"](), Bgm, writes=[Bgm])
        epsc = ph.sbuf("epsc2", [128, 1], F32)
        Bepsc = S.buf("Bepsc2")
        S.op("dve", lambda e: e.memset(epsc[:], EPS), writes=[Bepsc])
        pT = [ph.psum(f"pTf{i}", [128, 8, 128], BF16) for i in range(2)]
        BpT = S.pbufs("BpTf", 2)
        G = Gemm(S, ph, 16, "wf", 4)
        order = []
        for b in range(11):
            order += [("g", b), ("u", b)]
        slots = {}

        def ld(idx):
            kind, b = order[idx]
            slots[idx] = G.load(dr["w_ffn_in"], b * 512 + (DFF if kind == "u" else 0))
        ld(0)
        ld(1)
        norm_to_hT(S, ph, dr["x1_d"], gm, Bgm, ident, Bident, hT, BhT, pT, BpT, "n2", epsc, Bepsc)
        accg = [ph.psum(f"accg{i}", [128, 512], F32) for i in range(3)]
        accu = [ph.psum(f"accu{i}", [128, 512], F32) for i in range(3)]
        Baccg, Baccu = S.pbufs("Baccg", 3), S.pbufs("Baccu", 3)
        sgt = [ph.sbuf(f"sgt{i}", [128, 512], F32) for i in range(3)]
        Bsgt = S.bufs("Bsgt", 3)
        stg = [ph.sbuf(f"stgf{i}", [128, T], BF16) for i in range(2)]
        Bstg = S.bufs("Bstgf", 2)
        na = 0
        nf = 0
        for b in range(11):
            if 2 * b + 2 < 22:
                ld(2 * b + 2)
            if 2 * b + 3 < 22:
                ld(2 * b + 3)
            slg, slu = slots[2 * b], slots[2 * b + 1]
            for sub in range(4):
                fb = b * 4 + sub
                g = nf % 2
                nf += 1
                for tb in range(4):
                    a = na % 3
                    na += 1
                    cols = slice(tb * 512, (tb + 1) * 512)
                    for (accx, Bx, slx) in ((accg, Baccg, slg), (accu, Baccu, slu)):
                        mm_group(S, lambda a=a, accx=accx: accx[a][:],
                                 [(lambda kc=kc, slx=slx, sub=sub: G.wb[slx][:, kc, sub * 128:(sub + 1) * 128]) for kc in range(16)],
                                 [(lambda kc=kc, cols=cols: hT[:, kc, cols]) for kc in range(16)],
                                 [G.Bw[slx], BhT], Bx[a])
                    S.op("act", lambda e, a=a: e.activation(out=sgt[a][:], in_=accg[a][:], func=AF.Silu), reads=[Baccg[a]], writes=[Bsgt[a]])
                    S.op("dve", lambda e, a=a, g=g, cols=cols: e.tensor_tensor(out=stg[g][:, cols], in0=accu[a][:], in1=sgt[a][:], op=ALU.mult),
                         reads=[Baccu[a], Bsgt[a]], writes=[Bstg[g]])
                S.dma("sp", lambda fb=fb: dr["aT_d"]()[fb * 128:(fb + 1) * 128, :], lambda g=g: stg[g][:], Bstg[g], reads=[Bstg[g]])


def p2_ffn_out(S, dr):
    KC = DFF // 128
    for th in range(2):
        with Phase(S) as ph:
            aT = ph.sbuf(f"aTh{th}", [128, KC, 1024], BF16)
            BaT = S.buf("BaTh")
            for q in range(4):
                S.dma("sp", lambda q=q: aT[:, q * 11:(q + 1) * 11, :],
                      lambda q=q: dr["aT_d"]()[q * 11 * 128:(q + 1) * 11 * 128, th * 1024:(th + 1) * 1024].rearrange("(kc p) t -> p kc t", p=128),
                      BaT, writes=[BaT])
            wb = [ph.sbuf(f"wfo{th}_{i}", [128, KC, 256], BF16) for i in range(3)]
            Bw = S.bufs("Bwfo", 3)

            def ld(cb):
                sl = cb % 3
                S.dma("pool", lambda: wb[sl][:], lambda: dr["w_ffn_out"]()[:, cb * 256:(cb + 1) * 256].rearrange("(kc p) n -> p kc n", p=128),
                      Bw[sl], writes=[Bw[sl]])
            ld(0)
            ld(1)
            acc = [ph.psum(f"accf{th}_{i}", [128, 512], F32) for i in range(4)]
            Bacc = S.pbufs("Baccf", 4)
            xt = [ph.sbuf(f"xtf{th}_{i}", [128, 256], F32) for i in range(3)]
            Bxt = S.bufs("Bxtf", 3)
            so = [ph.sbuf(f"sof{th}_{i}", [128, 256], F32) for i in range(3)]
            Bso = S.bufs("Bsof", 3)
            na = 0
            for cb in range(8):
                if cb + 2 < 8:
                    ld(cb + 2)
                sl = cb % 3
                cs = slice(cb * 256, (cb + 1) * 256)
                for tt in range(8):
                    a = na % 4
                    q = na % 3
                    na += 1
                    rows = slice(th * 1024 + tt * 128, th * 1024 + (tt + 1) * 128)
                    lrows = slice(tt * 128, (tt + 1) * 128)
                    S.dma("sp", lambda q=q: xt[q][:], lambda rows=rows, cs=cs: dr["x1_d"]()[rows, cs], Bxt[q], writes=[Bxt[q]])
                    mm_group(S, lambda a=a: acc[a][:, 0:256],
                             [(lambda kc=kc, lrows=lrows: aT[:, kc, lrows]) for kc in range(KC)],
                             [(lambda kc=kc, sl=sl: wb[sl][:, kc, :]) for kc in range(KC)],
                             [Bw[sl], BaT], Bacc[a])
                    S.op("dve", lambda e, a=a, q=q: e.tensor_tensor(out=so[q][:], in0=acc[a][:, 0:256], in1=xt[q][:], op=ALU.add),
                         reads=[Bacc[a], Bxt[q]], writes=[Bso[q]])
                    S.dma("sp", lambda rows=rows, cs=cs: dr["xo"]()[rows, cs], lambda q=q: so[q][:], Bso[q], reads=[Bso[q]])


def gen_p2(S, dr):
    for ph in DBG.get("p2_phases", ("attn", "oattn", "ret", "oret", "wout", "ffn_in", "ffn_out")):
        {"attn": p2_attention, "oattn": p2_oattn, "ret": p2_retention, "oret": p2_oret, "wout": p2_wout,
         "ffn_in": p2_ffn_in, "ffn_out": p2_ffn_out}[ph](S, dr)


def p2_consts(j):
    sl = alibi_slopes()
    lg = log_g()
    Bi = [block_of(i, j) for i in range(NT)]
    p = np.arange(128)
    validb = np.zeros((128, 8, 32), np.float32)
    tab2 = np.zeros((128, 8, 8, 32), np.float32)
    wsel = np.zeros((128, 32), np.float32)
    for i in range(NT):
        for n in range(32):
            ok = n < Bi[i]
            validb[:, i, n] = 0.0 if ok else -1e30
            for h in range(8):
                tab2[:, i, h, n] = (-sl[h] * 256.0 * (Bi[i] - n) - 30000.0) if ok else -60000.0
        wsel[:, Bi[i]] = 1.0
    alibk = np.zeros((128, 8, 2), np.float32)
    for h in range(8):
        for c in range(2):
            alibk[:, h, c] = sl[h] * (c * 128 + p)
    aq = np.zeros((64, 8, 256), np.float32)
    q = np.arange(256)
    for h in range(8):
        aq[32, h, :] = -sl[h] * q
    selm = np.zeros((64, 32, 128), np.float32)
    for n in range(32):
        selm[n, n, :] = 1.0
        selm[32, n, :] = 1.0
    cm = np.zeros((128, 8, 2, 256), np.float32)
    for h in range(8):
        for c in range(2):
            kk = (c * 128 + p)[:, None]
            cm[:, h, c, :] = np.where(kk <= q[None, :], -sl[h] * q[None, :], -30000.0)
    dm = np.zeros((128, 4, 2, 256), np.float32)
    for h in range(4):
        for js in range(2):
            jj = (js * 128 + p)[:, None].astype(np.float64)
            diff = q[None, :].astype(np.float64) - jj
            dm[:, h, js, :] = np.where(diff >= 0, np.exp(lg[h] * np.maximum(diff, 0.0)), 0.0)
    cdec = np.tile(np.exp(lg * 256.0)[None, :], (128, 1)).astype(np.float32)
    return {"ident": np.eye(128, dtype=np.float32).astype(NPBF), "ones": np.ones((128, 128), np.float32).astype(NPBF),
            "validb": validb, "tab2": tab2, "alibk": alibk, "aq": aq.astype(NPBF), "selm": selm.astype(NPBF),
            "cm": cm.astype(NPBF), "dm": dm, "wsel": wsel, "cdec": cdec}


def vaug_layout(va, nchunk):
    v = va.reshape(nchunk, 128, 8, 128).transpose(2, 1, 0, 3)
    out = np.ones((8, 128, nchunk, 129), dtype=va.dtype)
    out[..., :128] = v
    return out


def chunk_layout(a):
    nch = a.shape[0] // 256
    return np.ascontiguousarray(a.reshape(nch, 2, 128, a.shape[1]).transpose(0, 2, 1, 3))


def shard_tokens(x):
    out = []
    for c in range(8):
        b, j = divmod(c, 4)
        out.append(np.concatenate([x[b, block_of(i, j) * 256:(block_of(i, j) + 1) * 256] for i in range(NT)], 0))
    return out


def unshard_tokens(xs):
    out = np.empty((2, S_LEN, D), xs[0].dtype)
    for c in range(8):
        b, j = divmod(c, 4)
        for i in range(NT):
            B = block_of(i, j)
            out[b, B * 256:(B + 1) * 256] = xs[c][i * 256:(i + 1) * 256]
    return out


def p2_inputs_from_p1(p1res, xs, wl, consts):
    ins = []
    glob = {}
    for b in range(2):
        kaT = np.empty((1024, S_LEN), NPBF)
        va = np.empty((S_LEN, 1024), NPBF)
        kd = np.empty((S_LEN, 1024), NPBF)
        vr = np.empty((S_LEN, 2048), NPBF)
        for j in range(4):
            r = p1res[4 * b + j]
            for i in range(NT):
                B = block_of(i, j)
                g, l = slice(B * 256, (B + 1) * 256), slice(i * 256, (i + 1) * 256)
                kaT[:, g] = r["kaT"][:, l]
                va[g] = r["va"][l]
                kd[g] = r["kd"][l]
                vr[g] = r["vr"][l]
        glob[b] = {"kaT_all": kaT, "vaug_all": vaug_layout(va, 64), "kd_all": chunk_layout(kd), "vr_all": chunk_layout(vr)}
    for c in range(8):
        b, j = divmod(c, 4)
        r = p1res[c]
        d = dict(glob[b])
        d.update({"x": xs[c], "qaT": r["qaT"], "kaT_loc": r["kaT"], "vaug_loc": vaug_layout(np.asarray(r["va"]), 16),
                  "qrT": r["qrT"], "qdT": r["qdT"], "krT": r["krT"], "vr_loc": chunk_layout(np.asarray(r["vr"])),
                  "sgT": r["sgT"], "gaT": r["gaT"], "ggT": r["ggT"]})
        d.update(wl)
        d.update(consts[j])
        ins.append(d)
    return ins


_PROGS = {}


def _prog(name):
    if name not in _PROGS:
        if name == "p1":
            _PROGS[name] = build_program(gen_p1, P1_INS, P1_OUTS)[0]
        else:
            _PROGS[name] = build_program(gen_p2, P2_INS, P2_OUTS, P2_SCRATCH)[0]
    return _PROGS[name]


def _pk(v):
    return np.ascontiguousarray(np.asarray(v, np.float32).reshape(16, 128).T)


def kernel(x, w_in, w_o_attn, w_o_ret, w_out, q_norm, k_norm, norm_mix, norm_ffn, w_ffn_in, w_ffn_out):
    x = np.asarray(x, np.float32)
    cores = list(range(8))
    xs = shard_tokens(x)
    c1 = p1_consts()
    c2 = [p2_consts(j) for j in range(4)]
    p1, p2 = _prog("p1"), _prog("p2")
    for l in range(DEPTH):
        wl1 = {"w_in": np.ascontiguousarray(w_in[l], dtype=np.float32), "gmix": _pk(norm_mix[l]),
               "qkg": np.ascontiguousarray(np.stack([q_norm[l], k_norm[l]], 1), dtype=np.float32)}
        wl1.update(c1)
        ins1 = [dict(wl1, x=xs[c]) for c in cores]
        r1 = run_bass_kernel_spmd(p1, ins1, core_ids=cores).results
        wl2 = {"w_o_attn": np.ascontiguousarray(w_o_attn[l], dtype=np.float32), "w_o_ret": np.ascontiguousarray(w_o_ret[l], dtype=np.float32),
               "w_out": np.ascontiguousarray(w_out[l], dtype=np.float32), "w_ffn_in": np.ascontiguousarray(w_ffn_in[l], dtype=np.float32),
               "w_ffn_out": np.ascontiguousarray(w_ffn_out[l], dtype=np.float32), "gffn": _pk(norm_ffn[l])}
        ins2 = p2_inputs_from_p1(r1, xs, wl2, c2)
        r2 = run_bass_kernel_spmd(p2, ins2, core_ids=cores).results
        xs = [np.asarray(r2[c]["xo"], np.float32) for c in cores]
    return unshard_tokens(xs)
```

```python
import numpy as np
import ml_dtypes
from contextlib import ExitStack

import concourse.bass as bass
import concourse.mybir as mybir
from concourse.bass_utils import run_bass_kernel_spmd

F32 = mybir.dt.float32
BF16 = mybir.dt.bfloat16
AF = mybir.ActivationFunctionType
ALU = mybir.AluOpType
AX = mybir.AxisListType
NPBF = ml_dtypes.bfloat16

D = 2048
S_LEN = 8192
NB = 32
T = 2048
NT = 8
DEPTH = 4
DFF = 5632
EPS = 1e-6
NEG = -30000.0
DBG = {}
IN_COLS = 13312

ENGS = ("pe", "act", "dve", "pool", "sp")
SEM_EPOCH = 20000


class Ev:
    __slots__ = ("eng", "iid", "sem", "val", "buf")

    def __init__(self, eng, iid):
        self.eng, self.iid, self.sem, self.val, self.buf = eng, iid, None, None, None


class Buf:
    __slots__ = ("name", "w", "r", "dsem", "dcnt", "excl", "dq")

    def __init__(self, name, excl=False):
        self.name, self.w, self.r, self.dsem, self.dcnt, self.excl, self.dq = name, None, [], None, 0, excl, None


class Sched:
    def __init__(self, nc, need):
        self.nc = nc
        self.dry = nc is None
        self.need = need
        self.iid = 0
        self.stack = ExitStack()
        if not self.dry:
            self.eng = {"pe": nc.tensor, "act": nc.scalar, "dve": nc.vector, "pool": nc.gpsimd, "sp": nc.sync}
        self.sig = {e: [None, 0, 0] for e in ENGS}
        self.waited = {e: {} for e in ENGS}
        self.last = {e: None for e in ENGS}
        self.live_bufs = []
        self.sem_pool = {}
        self.nsem = 0

    def buf(self, name, excl=False):
        b = Buf(name, excl)
        self.live_bufs.append(b)
        return b

    def bufs(self, name, n, excl=False):
        return [self.buf(f"{name}{i}", excl) for i in range(n)]

    def pbufs(self, name, n):
        return self.bufs(name, n, True)

    def _new_sem(self, name):
        self.nsem += 1
        if self.dry:
            return ("sem", name, self.nsem)
        return self.stack.enter_context(self.nc.semaphore(f"{name}_{self.nsem}"))

    def _dma_sem(self, b, q):
        if b.dsem is None:
            b.dq = q
            pool = self.sem_pool.setdefault(q, [])
            if pool:
                b.dsem, b.dcnt = pool.pop()
            else:
                b.dsem, b.dcnt = self._new_sem("d" + q), 0
        assert b.dq == q, (b.name, b.dq, q)
        return b.dsem

    def _wait(self, eng, sem, val):
        key = id(sem) if not isinstance(sem, tuple) else sem
        if self.waited[eng].get(key, -1) >= val:
            return
        self.waited[eng][key] = val
        if not self.dry:
            self.eng[eng].wait_ge(sem, val)

    def _dep(self, eng, d, raw):
        if d is None:
            return
        if d.eng == "dma":
            self._wait(eng, d.buf.dsem, 16 * d.buf.dcnt)
            return
        if d.eng == eng and eng in ("pe", "sp"):
            return
        if self.dry:
            self.need.add(d.iid)
            return
        assert d.sem is not None, "dependency on non-signalling instruction"
        self._wait(eng, d.sem, d.val)

    def _collect(self, eng, reads, writes):
        for b in reads:
            self._dep(eng, b.w, True)
            if b.excl:
                for r in b.r:
                    self._dep(eng, r, False)
        for b in writes:
            self._dep(eng, b.w, False)
            for r in b.r:
                self._dep(eng, r, False)

    def _commit(self, ev, reads, writes):
        for b in reads:
            if b.excl:
                b.w = ev
                b.r = []
            else:
                b.r.append(ev)
        for b in writes:
            b.w = ev
            b.r = []

    def op(self, eng, fn, reads=(), writes=()):
        iid = self.iid
        self.iid += 1
        ev = Ev(eng, iid)
        self._collect(eng, reads, writes)
        if not self.dry:
            ins = fn(self.eng[eng])
            if iid in self.need:
                sg = self.sig[eng]
                if sg[0] is None or sg[1] >= SEM_EPOCH:
                    sg[0], sg[1] = self._new_sem(eng), 0
                sg[1] += 1
                ev.sem, ev.val = sg[0], sg[1]
                ins.then_inc(sg[0], 1)
        self.last[eng] = ev
        self._commit(ev, reads, writes)
        return ev

    def dma(self, q, out_fn, in_fn, sb, reads=(), writes=(), **kw):
        iid = self.iid
        self.iid += 1
        ev = Ev("dma", iid)
        ev.buf = sb
        self._collect(q, reads, writes)
        sem = self._dma_sem(sb, q)
        sb.dcnt += 1
        if not self.dry:
            self.eng[q].dma_start(out=out_fn(), in_=in_fn(), **kw).then_inc(sem, 16)
        self._commit(ev, reads, writes)
        return ev

    def barrier(self, final=False):
        lasts = dict(self.last)
        dbufs = [b for b in self.live_bufs if b.dsem is not None]
        for e in (("sp",) if final else ENGS):
            for f in ENGS:
                d = lasts[f]
                if d is None or f == e or f == "sp":
                    continue
                if self.dry:
                    self.need.add(d.iid)
                else:
                    self._wait(e, d.sem, d.val)
            for b in dbufs:
                self._wait(e, b.dsem, 16 * b.dcnt)
        for b in dbufs:
            self.sem_pool[b.dq].append((b.dsem, b.dcnt))
        self.live_bufs = []


class Phase:
    def __init__(self, S):
        self.S = S
        self.stack = ExitStack()

    def __enter__(self):
        return self

    def __exit__(self, *a):
        self.S.barrier()
        self.stack.close()
        return False

    def sbuf(self, name, shape, dt):
        if self.S.dry:
            return None
        return self.stack.enter_context(self.S.nc.sbuf_tensor("sb_" + name, list(shape), dt))

    def psum(self, name, shape, dt):
        if self.S.dry:
            return None
        return self.stack.enter_context(self.S.nc.psum_tensor("ps_" + name, list(shape), dt))


def block_of(i, j):
    return 4 * i + (j if i % 2 == 0 else 3 - j)


def log_g():
    return np.log(1.0 - np.exp2(-5.0 - np.arange(4, dtype=np.float64)))


def alibi_slopes():
    return np.array([0.5 ** (i + 1) for i in range(8)], dtype=np.float64)


class Gemm:
    def __init__(self, S, ph, KC, name="w", nslots=3):
        self.S, self.KC, self.ns = S, KC, nslots
        self.wb = [ph.sbuf(f"{name}b{i}", [128, KC, 512], BF16) for i in range(nslots)]
        self.Bw = S.bufs(f"B{name}", nslots)
        self.n = 0

    def load(self, w_ap_fn, c0, ncols=512, k0=0):
        S = self.S
        sl = self.n % self.ns
        self.n += 1
        KC = self.KC
        S.dma("pool",
              lambda: self.wb[sl][:, :, 0:ncols],
              lambda: w_ap_fn()[k0 * 128:(k0 + KC) * 128, c0:c0 + ncols].rearrange("(kc p) n -> p kc n", p=128),
              self.Bw[sl], writes=[self.Bw[sl]])
        return sl


def mm_group(S, out_fn, lhs_fns, rhs_fns, reads, Bout):
    n = len(lhs_fns)
    for i in range(n):
        S.op("pe", (lambda e, i=i: e.matmul(out_fn(), lhsT=lhs_fns[i](), rhs=rhs_fns[i](),
                                            start=(i == 0), stop=(i == n - 1))),
             reads=reads, writes=[Bout])


def norm_to_hT(S, ph, x_fn, gm, Bgm, ident, Bident, hT, BhT, pT, BpT, tag, epsc, Bepsc):
    xt = [ph.sbuf(f"{tag}xt{i}", [128, D], F32) for i in range(2)]
    xs = [ph.sbuf(f"{tag}xs{i}", [128, D], BF16) for i in range(2)]
    junk = ph.sbuf(f"{tag}junk", [128, D], BF16)
    ss = [ph.sbuf(f"{tag}ss{i}", [128, 1], F32) for i in range(2)]
    rs = [ph.sbuf(f"{tag}rs{i}", [128, 1], F32) for i in range(2)]
    Bxt, Bxs, Bss, Brs = S.bufs(tag + "Bxt", 2), S.bufs(tag + "Bxs", 2), S.bufs(tag + "Bss", 2), S.bufs(tag + "Brs", 2)
    Bjunk = S.buf(tag + "Bjunk")
    for tt in range(T // 128):
        k = tt % 2
        S.dma("sp", lambda k=k: xt[k][:], lambda tt=tt: x_fn()[tt * 128:(tt + 1) * 128, :], Bxt[k], writes=[Bxt[k]])
        S.op("dve", lambda e, k=k: e.memset(ss[k][:], 0.0), writes=[Bss[k]])
        S.op("act", lambda e, k=k: e.activation(out=junk[:], in_=xt[k][:], func=AF.Square, accum_out=ss[k][:]),
             reads=[Bxt[k], Bss[k]], writes=[Bjunk, Bss[k]])
        S.op("act", lambda e, k=k: e.activation(out=rs[k][:], in_=ss[k][:], func=AF.Sqrt, scale=1.0 / D, bias=epsc[:]),
             reads=[Bss[k], Bepsc], writes=[Brs[k]])
        S.op("dve", lambda e, k=k: e.reciprocal(out=rs[k][:], in_=rs[k][:]), reads=[Brs[k]], writes=[Brs[k]])
        S.op("act", lambda e, k=k: e.activation(out=xs[k][:], in_=xt[k][:], func=AF.Copy, scale=rs[k][:]),
             reads=[Bxt[k], Brs[k]], writes=[Bxs[k]])
        for half in range(2):
            for kk in range(8):
                kc = half * 8 + kk
                S.op("pe", lambda e, k=k, kk=kk, kc=kc, half=half: e.transpose(
                    out=pT[half][:, kk, :], in_=xs[k][:, kc * 128:(kc + 1) * 128], identity=ident[:]),
                     reads=[Bxs[k], Bident], writes=[BpT[half]])
            S.op("dve", lambda e, half=half, tt=tt: e.tensor_tensor(
                out=hT[:, half * 8:(half + 1) * 8, tt * 128:(tt + 1) * 128], in0=pT[half][:],
                in1=gm[:, half * 8:(half + 1) * 8].unsqueeze(2).to_broadcast([128, 8, 128]), op=ALU.mult),
                 reads=[BpT[half], Bgm], writes=[BhT])


def gen_p1(S, dr):
    with Phase(S) as ph:
        hT = ph.sbuf("hT", [128, 16, T], BF16)
        BhT = S.buf("BhT")
        ident = ph.sbuf("ident", [128, 128], BF16)
        ones = ph.sbuf("ones", [128, 128], BF16)
        gm = ph.sbuf("gm", [128, 16], F32)
        qg = ph.sbuf("qg", [128, 2], F32)
        qdec = ph.sbuf("qdec", [128, 4, 512], F32)
        kdec = ph.sbuf("kdec", [128, 2, 1024], F32)
        Bident, Bones, Bgm, Bqg, Bqdec, Bkdec = (S.buf(n) for n in ("Bident", "Bones", "Bgm", "Bqg", "Bqdec", "Bkdec"))
        S.dma("sp", lambda: ident[:], lambda: dr["ident"](), Bident, writes=[Bident])
        S.dma("sp", lambda: ones[:], lambda: dr["ones"](), Bones, writes=[Bones])
        S.dma("sp", lambda: gm[:], lambda: dr["gmix"](), Bgm, writes=[Bgm])
        S.dma("sp", lambda: qg[:], lambda: dr["qkg"](), Bqg, writes=[Bqg])
        S.dma("sp", lambda: qdec[:], lambda: dr["qdec"](), Bqdec, writes=[Bqdec])
        S.dma("sp", lambda: kdec[:], lambda: dr["kdec"](), Bkdec, writes=[Bkdec])
        epsc = ph.sbuf("epsc", [128, 1], F32)
        Bepsc = S.buf("Bepsc")
        S.op("dve", lambda e: e.memset(epsc[:], EPS), writes=[Bepsc])
        S.op("dve", lambda e: e.tensor_scalar(out=qg[:, 0:1], in0=qg[:, 0:1], scalar1=float(128 ** -0.5), scalar2=None,
                                              op0=ALU.mult), reads=[Bqg], writes=[Bqg])

        pT = [ph.psum(f"pT{i}", [128, 8, 128], BF16) for i in range(2)]
        BpT = S.pbufs("BpT", 2)
        acc = [ph.psum(f"acc{i}", [128, 512], F32) for i in range(4)]
        Bacc = S.pbufs("Bacc", 4)
        ssp = [ph.psum(f"ssp{i}", [128, 512], F32) for i in range(2)]
        Bssp = S.pbufs("Bssp", 2)

        G = Gemm(S, ph, 16)
        nblk = IN_COLS // 512
        slots = {}
        for b in range(2):
            slots[b] = G.load(dr["w_in"], b * 512)

        norm_to_hT(S, ph, dr["x"], gm, Bgm, ident, Bident, hT, BhT, pT, BpT, "n1", epsc, Bepsc)

        stg = [ph.sbuf(f"stg{i}", [128, T], BF16) for i in range(3)]
        Bstg = S.bufs("Bstg", 3)
        stt = [ph.sbuf(f"stt{i}", [128, 512], BF16) for i in range(4)]
        Bstt = S.bufs("Bstt", 4)
        sq = [ph.sbuf(f"sq{i}", [128, 512], BF16) for i in range(2)]
        Bsq = S.bufs("Bsq", 2)
        rr = [ph.sbuf(f"rr{i}", [128, 512], F32) for i in range(2)]
        Brr = S.bufs("Brr", 2)
        cnt = {"acc": 0, "stg": 0, "stt": 0, "sq": 0}

        def fm_block(sl, sub, kind, out_rows):
            outs = out_rows if isinstance(out_rows, list) else [out_rows]
            sgs = []
            for _ in outs:
                sgs.append(cnt["stg"] % 3)
                cnt["stg"] += 1
            for tb in range(4):
                a = cnt["acc"] % 4
                cnt["acc"] += 1
                mm_group(S, lambda a=a: acc[a][:],
                         [(lambda kc=kc: G.wb[sl][:, kc, sub * 128:(sub + 1) * 128]) for kc in range(16)],
                         [(lambda kc=kc, tb=tb: hT[:, kc, tb * 512:(tb + 1) * 512]) for kc in range(16)],
                         [G.Bw[sl], BhT], Bacc[a])
                cols = slice(tb * 512, (tb + 1) * 512)
                if kind in ("qn", "kn"):
                    q = cnt["sq"] % 2
                    cnt["sq"] += 1
                    gc = 0 if kind == "qn" else 1
                    S.op("act", lambda e, a=a, q=q: e.activation(out=sq[q][:], in_=acc[a][:], func=AF.Square),
                         reads=[Bacc[a]], writes=[Bsq[q]])
                    S.op("pe", lambda e, q=q: e.matmul(ssp[q][:], lhsT=ones[:], rhs=sq[q][:], start=True, stop=True),
                         reads=[Bones, Bsq[q]], writes=[Bssp[q]])
                    S.op("act", lambda e, q=q: e.activation(out=rr[q][:], in_=ssp[q][:], func=AF.Sqrt, scale=1.0 / 128, bias=epsc[:]),
                         reads=[Bssp[q], Bepsc], writes=[Brr[q]])
                    S.op("dve", lambda e, q=q: e.reciprocal(out=rr[q][:], in_=rr[q][:]), reads=[Brr[q]], writes=[Brr[q]])
                    S.op("dve", lambda e, a=a, q=q, gc=gc, cols=cols, sg=sgs[0]: e.scalar_tensor_tensor(
                        out=stg[sg][:, cols], in0=acc[a][:], scalar=qg[:, gc:gc + 1], in1=rr[q][:],
                        op0=ALU.mult, op1=ALU.mult), reads=[Bacc[a], Brr[q], Bqg], writes=[Bstg[sgs[0]]])
                elif kind.startswith("qr"):
                    h = int(kind[2])
                    S.op("act", lambda e, a=a, cols=cols, sg=sgs[0]: e.activation(out=stg[sg][:, cols], in_=acc[a][:], func=AF.Copy),
                         reads=[Bacc[a]], writes=[Bstg[sgs[0]]])
                    S.op("dve", lambda e, a=a, cols=cols, sg=sgs[1], h=h: e.tensor_tensor(
                        out=stg[sg][:, cols], in0=acc[a][:], in1=qdec[:, h, :], op=ALU.mult),
                         reads=[Bacc[a], Bqdec], writes=[Bstg[sgs[1]]])
                elif kind == "kr":
                    S.op("act", lambda e, a=a, cols=cols, sg=sgs[0]: e.activation(out=stg[sg][:, cols], in_=acc[a][:], func=AF.Copy, scale=1.0 / 16),
                         reads=[Bacc[a]], writes=[Bstg[sgs[0]]])
                elif kind == "silu":
                    S.op("act", lambda e, a=a, cols=cols, sg=sgs[0]: e.activation(out=stg[sg][:, cols], in_=acc[a][:], func=AF.Silu),
                         reads=[Bacc[a]], writes=[Bstg[sgs[0]]])
                elif kind == "sig":
                    S.op("act", lambda e, a=a, cols=cols, sg=sgs[0]: e.activation(out=stg[sg][:, cols], in_=acc[a][:], func=AF.Sigmoid),
                         reads=[Bacc[a]], writes=[Bstg[sgs[0]]])
                else:
                    raise ValueError(kind)
            for sg, o in zip(sgs, outs):
                S.dma("sp", o, lambda sg=sg: stg[sg][:], Bstg[sg], reads=[Bstg[sg]])

        def tm_block(sl, kind, out_fn):
            for tt in range(T // 128):
                a = cnt["acc"] % 4
                cnt["acc"] += 1
                mm_group(S, lambda a=a: acc[a][:],
                         [(lambda kc=kc, tt=tt: hT[:, kc, tt * 128:(tt + 1) * 128]) for kc in range(16)],
                         [(lambda kc=kc: G.wb[sl][:, kc, :]) for kc in range(16)],
                         [G.Bw[sl], BhT], Bacc[a])
                st = cnt["stt"] % 4
                cnt["stt"] += 1
                if kind == "v":
                    S.op("act", lambda e, a=a, st=st: e.activation(out=stt[st][:], in_=acc[a][:], func=AF.Copy),
                         reads=[Bacc[a]], writes=[Bstt[st]])
                else:
                    c0 = kind[1]
                    S.op("dve", lambda e, a=a, st=st, tt=tt, c0=c0: e.tensor_tensor(
                        out=stt[st][:], in0=acc[a][:], in1=kdec[:, tt % 2, c0:c0 + 512], op=ALU.mult),
                         reads=[Bacc[a], Bkdec], writes=[Bstt[st]])
                S.dma("sp", (lambda tt=tt: out_fn(tt)), lambda st=st: stt[st][:], Bstt[st], reads=[Bstt[st]])

        for b in DBG.get('blocks', range(nblk)):
            if b + 2 < nblk and 'blocks' not in DBG:
                slots[b + 2] = G.load(dr["w_in"], (b + 2) * 512)
            if 'blocks' in DBG:
                slots[b] = G.load(dr["w_in"], b * 512)
            sl = slots[b]
            c0 = b * 512
            if c0 < 1024:
                for sub in range(4):
                    hh = (c0 // 128) + sub
                    fm_block(sl, sub, "qn", (lambda hh=hh: dr["qaT"]()[hh * 128:(hh + 1) * 128, :]))
            elif c0 < 2048:
                for sub in range(4):
                    hh = ((c0 - 1024) // 128) + sub
                    fm_block(sl, sub, "kn", (lambda hh=hh: dr["kaT"]()[hh * 128:(hh + 1) * 128, :]))
            elif c0 < 3072:
                cc = c0 - 2048
                tm_block(sl, "v", (lambda tt, cc=cc: dr["va"]()[tt * 128:(tt + 1) * 128, cc:cc + 512]))
            elif c0 < 4096:
                for sub in range(4):
                    f = (c0 - 3072) // 128 + sub
                    fm_block(sl, sub, f"qr{f // 2}", [(lambda f=f: dr["qrT"]()[f * 128:(f + 1) * 128, :]),
                                                       (lambda f=f: dr["qdT"]()[f * 128:(f + 1) * 128, :])])
            elif c0 < 5120:
                for sub in range(4):
                    f = (c0 - 4096) // 128 + sub
                    fm_block(sl, sub, "kr", (lambda f=f: dr["krT"]()[f * 128:(f + 1) * 128, :]))
                cc = c0 - 4096
                tm_block(sl, ("kd", cc), (lambda tt, cc=cc: dr["kd"]()[tt * 128:(tt + 1) * 128, cc:cc + 512]))
            elif c0 < 7168:
                cc = c0 - 5120
                tm_block(sl, "v", (lambda tt, cc=cc: dr["vr"]()[tt * 128:(tt + 1) * 128, cc:cc + 512]))
            elif c0 < 9216:
                for sub in range(4):
                    f = (c0 - 7168) // 128 + sub
                    fm_block(sl, sub, "silu", (lambda f=f: dr["sgT"]()[f * 128:(f + 1) * 128, :]))
            elif c0 < 11264:
                for sub in range(4):
                    f = (c0 - 9216) // 128 + sub
                    fm_block(sl, sub, "sig", (lambda f=f: dr["gaT"]()[f * 128:(f + 1) * 128, :]))
            else:
                for sub in range(4):
                    f = (c0 - 11264) // 128 + sub
                    fm_block(sl, sub, "sig", (lambda f=f: dr["ggT"]()[f * 128:(f + 1) * 128, :]))


P1_OUTS = {"qaT": [1024, T], "kaT": [1024, T], "va": [T, 1024], "qrT": [1024, T], "qdT": [1024, T],
           "krT": [1024, T], "kd": [T, 1024], "vr": [T, 2048], "sgT": [2048, T], "gaT": [2048, T], "ggT": [2048, T]}
P1_INS = {"x": ([T, D], F32), "w_in": ([D, IN_COLS], F32), "ident": ([128, 128], BF16), "ones": ([128, 128], BF16),
          "gmix": ([128, 16], F32), "qkg": ([128, 2], F32), "qdec": ([128, 4, 512], F32), "kdec": ([128, 2, 1024], F32)}


def build_program(gen, ins, outs, scratch=None):
    need = set()
    scratch = scratch or {}
    S0 = Sched(None, need)
    gen(S0, {k: (lambda: None) for k in list(ins) + list(outs) + list(scratch)})
    S0.barrier(final=True)
    nc = bass.Bass("TRN2", target_bir_lowering=False)
    aps = {}
    for k, (shape, dt) in ins.items():
        aps[k] = nc.dram_tensor(k, list(shape), dt, kind="ExternalInput").ap()
    for k, shape in outs.items():
        dt = BF16
        if isinstance(shape, tuple):
            shape, dt = shape
        aps[k] = nc.dram_tensor(k, list(shape), dt, kind="ExternalOutput").ap()
    for k, (shape, dt) in scratch.items():
        aps[k] = nc.dram_tensor(k, list(shape), dt).ap()
    S = Sched(nc, need)
    with S.stack:
        gen(S, {k: (lambda k=k: aps[k]) for k in aps})
        S.barrier(final=True)
    assert S.iid == S0.iid, (S.iid, S0.iid)
    return nc, S


def p1_consts():
    lg = log_g()
    i = np.arange(256, dtype=np.float64)
    qd = np.exp(lg[:, None] * (i[None, :] + 1.0))
    qdec = np.tile(np.concatenate([qd, qd], axis=1)[None], (128, 1, 1)).astype(np.float32)
    p = np.arange(128)
    kdec = np.zeros((128, 2, 1024), np.float32)
    for s in range(2):
        for h in range(4):
            kdec[:, s, h * 256:(h + 1) * 256] = (np.exp(lg[h] * (255.0 - (s * 128 + p))) / 16.0)[:, None]
    return {"ident": np.eye(128, dtype=np.float32).astype(NPBF), "ones": np.ones((128, 128), np.float32).astype(NPBF),
            "qdec": qdec, "kdec": kdec}


P2_INS = {
    "x": ([T, D], F32),
    "qaT": ([1024, T], BF16), "kaT_all": ([1024, S_LEN], BF16), "vaug_all": ([8, 128, 64, 129], BF16),
    "kaT_loc": ([1024, T], BF16), "vaug_loc": ([8, 128, 16, 129], BF16),
    "qrT": ([1024, T], BF16), "qdT": ([1024, T], BF16), "krT": ([1024, T], BF16),
    "kd_all": ([32, 128, 2, 1024], BF16), "vr_all": ([32, 128, 2, 2048], BF16), "vr_loc": ([8, 128, 2, 2048], BF16),
    "sgT": ([2048, T], BF16), "gaT": ([2048, T], BF16), "ggT": ([2048, T], BF16),
    "w_o_attn": ([1024, D], F32), "w_o_ret": ([2048, D], F32), "w_out": ([D, D], F32),
    "w_ffn_in": ([D, 2 * DFF], F32), "w_ffn_out": ([DFF, D], F32), "gffn": ([128, 16], F32),
    "ident": ([128, 128], BF16), "ones": ([128, 128], BF16),
    "validb": ([128, 8, 32], F32), "tab2": ([128, 8, 8, 32], F32), "alibk": ([128, 8, 2], F32),
    "aq": ([64, 8, 256], BF16), "selm": ([64, 32, 128], BF16), "cm": ([128, 8, 2, 256], BF16),
    "dm": ([128, 4, 2, 256], F32), "wsel": ([128, 32], F32), "cdec": ([128, 4], F32),
}
P2_OUTS = {"xo": ([T, D], F32)}
P2_SCRATCH = {"oaT_d": ([1024, T], BF16), "m1T_d": ([2048, T], BF16), "orrT_d": ([2048, T], BF16),
              "mgT_d": ([2048, T], BF16), "x1_d": ([T, D], F32), "aT_d": ([DFF, T], BF16)}


def p2_attention(S, dr):
    with Phase(S) as ph:
        def cst(name, shape, dt):
            t = ph.sbuf("c_" + name, shape, dt)
            b = S.buf("Bc_" + name)
            S.dma("sp", lambda: t[:], lambda: dr[name](), b, writes=[b])
            return t, b
        ident, Bident = cst("ident", [128, 128], BF16)
        validb, Bvalidb = cst("validb", [128, 8, 32], F32)
        tab2, Btab2 = cst("tab2", [128, 8, 8, 32], F32)
        alibk, Balibk = cst("alibk", [128, 8, 2], F32)
        aq, Baq = cst("aq", [64, 8, 256], BF16)
        selm, Bselm = cst("selm", [64, 32, 128], BF16)
        cm, Bcm = cst("cm", [128, 8, 2, 256], BF16)

        KT = [ph.sbuf(f"KT{i}", [128, S_LEN], BF16) for i in range(2)]
        VA = [ph.sbuf(f"VA{i}", [128, 64, 129], BF16) for i in range(2)]
        QT = [ph.sbuf(f"QT{i}", [128, T], BF16) for i in range(2)]
        KL = [ph.sbuf(f"KL{i}", [128, T], BF16) for i in range(2)]
        VL = [ph.sbuf(f"VL{i}", [128, 16, 129], BF16) for i in range(2)]
        BKT, BVA, BQT, BKL, BVL = (S.bufs(n, 2) for n in ("BKT", "BVA", "BQT", "BKL", "BVL"))
        oaT = ph.sbuf("oaT", [128, 8, T], BF16)
        BoaT = S.bufs("BoaT", 8)
        km = ph.sbuf("km", [128, 32], F32)
        kmh = ph.sbuf("kmh", [128, 32], BF16)
        kmhf = ph.sbuf("kmhf", [128, 32], F32)
        kml = ph.sbuf("kml", [128, 32], BF16)
        Bkm, Bkmh, Bkmhf, Bkml = (S.buf(n) for n in ("Bkm", "Bkmh", "Bkmhf", "Bkml"))
        M = [ph.sbuf(f"M{i}", [64, 256], BF16) for i in range(2)]
        BM = S.bufs("BM", 2)
        g2 = ph.sbuf("g2", [128, 2, 32], F32)
        mx8 = ph.sbuf("mx8", [128, 2, 8], F32)
        msk = ph.sbuf("msk", [128, 2, 32], F32)
        mrow = ph.sbuf("mrow", [128, 2, 32], BF16)
        Bg2, Bmx8, Bmsk, Bmrow = (S.buf(n) for n in ("Bg2", "Bmx8", "Bmsk", "Bmrow"))
        PT = [ph.sbuf(f"PT{i}", [128, 256], BF16) for i in range(4)]
        BPT = S.bufs("BPT", 4)
        rden = ph.sbuf("rden", [128, 2], F32)
        Brden = S.buf("Brden")
        on = [ph.sbuf(f"on{i}", [128, 128], BF16) for i in range(2)]
        Bon = S.bufs("Bon", 2)

        sT = [ph.psum(f"sT{i}", [128, 512], F32) for i in range(3)]
        BsT = S.pbufs("BsT", 3)
        Ops = [ph.psum(f"O{i}", [128, 512], F32) for i in range(2)]
        BO = S.pbufs("BO", 2)
        gp = ph.psum("gp", [128, 2, 32], F32)
        Bgp = S.pbufs("Bgp", 1)[0]
        mT = ph.psum("mT", [32, 256], BF16)
        BmT = S.pbufs("BmT", 1)[0]
        oT = ph.psum("oT", [128, 2, 128], BF16)
        BoT = S.pbufs("BoT", 1)[0]

        def load_head(h):
            k = h % 2
            S.dma("sp", lambda: KT[k][:], lambda: dr["kaT_all"]()[h * 128:(h + 1) * 128, :], BKT[k], writes=[BKT[k]])
            S.dma("sp", lambda: VA[k][:], lambda: dr["vaug_all"]()[h], BVA[k], writes=[BVA[k]])
            S.dma("sp", lambda: QT[k][:], lambda: dr["qaT"]()[h * 128:(h + 1) * 128, :], BQT[k], writes=[BQT[k]])
            S.dma("sp", lambda: KL[k][:], lambda: dr["kaT_loc"]()[h * 128:(h + 1) * 128, :], BKL[k], writes=[BKL[k]])
            S.dma("sp", lambda: VL[k][:], lambda: dr["vaug_loc"]()[h], BVL[k], writes=[BVL[k]])

        load_head(0)
        cnt = {"s": 0, "p": 0, "on": 0}
        for h in range(8):
            k = h % 2
            if h + 1 < 8:
                load_head(h + 1)
            S.op("dve", lambda e, k=k: e.tensor_reduce(out=km[:], in_=KT[k][:].rearrange("p (n s) -> p n s", s=256),
                                                       axis=AX.X, op=ALU.add), reads=[BKT[k]], writes=[Bkm])
            S.op("dve", lambda e: e.tensor_scalar(out=km[:], in0=km[:], scalar1=1.0 / 256, scalar2=None, op0=ALU.mult),
                 reads=[Bkm], writes=[Bkm])
            S.op("dve", lambda e: e.tensor_copy(out=kmh[:], in_=km[:]), reads=[Bkm], writes=[Bkmh])
            S.op("dve", lambda e: e.tensor_copy(out=kmhf[:], in_=kmh[:]), reads=[Bkmh], writes=[Bkmhf])
            S.op("dve", lambda e: e.tensor_tensor(out=kml[:], in0=km[:], in1=kmhf[:], op=ALU.subtract),
                 reads=[Bkm, Bkmhf], writes=[Bkml])
            for par in range(2):
                S.op("dve", lambda e, par=par, h=h: e.tensor_copy(out=M[par][32:64, :], in_=aq[32:64, h, :]),
                     reads=[Baq], writes=[BM[par]])
            for i in range(NT):
                par = i % 2
                qcols = slice(i * 256, (i + 1) * 256)
                for s in range(2):
                    qs = slice(i * 256 + s * 128, i * 256 + (s + 1) * 128)
                    S.op("pe", lambda e, s=s, qs=qs, k=k: e.matmul(gp[:, s, :], lhsT=QT[k][:, qs], rhs=kmh[:], start=True, stop=False),
                         reads=[BQT[k], Bkmh], writes=[Bgp])
                    S.op("pe", lambda e, s=s, qs=qs, k=k: e.matmul(gp[:, s, :], lhsT=QT[k][:, qs], rhs=kml[:], start=False, stop=True),
                         reads=[BQT[k], Bkml], writes=[Bgp])
                S.op("dve", lambda e, i=i: e.tensor_tensor(out=g2[:], in0=gp[:], in1=validb[:, i:i + 1, :].to_broadcast([128, 2, 32]),
                                                          op=ALU.add), reads=[Bgp, Bvalidb], writes=[Bg2])
                for s in range(2):
                    S.op("dve", lambda e, s=s: e.max(out=mx8[:, s, :], in_=g2[:, s, :]), reads=[Bg2], writes=[Bmx8])
                for s in range(2):
                    S.op("dve", lambda e, s=s: e.tensor_scalar(out=msk[:, s, :], in0=g2[:, s, :], scalar1=mx8[:, s, 2:3], scalar2=30000.0,
                                                              op0=ALU.is_ge, op1=ALU.mult), reads=[Bg2, Bmx8], writes=[Bmsk])
                S.op("dve", lambda e, i=i, h=h: e.tensor_tensor(out=mrow[:], in0=msk[:], in1=tab2[:, i, h:h + 1, :].to_broadcast([128, 2, 32]),
                                                              op=ALU.add), reads=[Bmsk, Btab2], writes=[Bmrow])
                for s in range(2):
                    S.op("pe", lambda e, s=s: e.transpose(out=mT[:, s * 128:(s + 1) * 128], in_=mrow[:, s, :], identity=ident[:]),
                         reads=[Bmrow, Bident], writes=[BmT])
                S.op("dve", lambda e, par=par: e.tensor_copy(out=M[par][0:32, :], in_=mT[:]), reads=[BmT], writes=[BM[par]])
                nblk = 4 * i + 3
                steps = [(n, c) for n in range(nblk) for c in range(2)] + [("own", 0), ("own", 1)]
                first = [True, True]
                for (n, c) in steps:
                    r = cnt["s"] % 3
                    cnt["s"] += 1
                    p = cnt["p"] % 4
                    cnt["p"] += 1
                    if n == "own":
                        ks = slice(i * 256 + c * 128, i * 256 + (c + 1) * 128)
                        S.op("pe", lambda e, r=r, ks=ks, k=k, qcols=qcols: e.matmul(sT[r][:, 0:256], lhsT=KL[k][:, ks], rhs=QT[k][:, qcols], start=True, stop=False),
                             reads=[BKL[k], BQT[k]], writes=[BsT[r]])
                        S.op("pe", lambda e, r=r, c=c, h=h: e.matmul(sT[r][:, 0:256], lhsT=ident[:], rhs=cm[:, h, c, :], start=False, stop=True),
                             reads=[Bident, Bcm], writes=[BsT[r]])
                    else:
                        ks = slice((2 * n + c) * 128, (2 * n + c + 1) * 128)
                        S.op("pe", lambda e, r=r, ks=ks, k=k, qcols=qcols: e.matmul(sT[r][:, 0:256], lhsT=KT[k][:, ks], rhs=QT[k][:, qcols], start=True, stop=False),
                             reads=[BKT[k], BQT[k]], writes=[BsT[r]])
                        S.op("pe", lambda e, r=r, n=n, par=par: e.matmul(sT[r][:, 0:256], lhsT=selm[:, n, :], rhs=M[par][:], start=False, stop=True),
                             reads=[Bselm, BM[par]], writes=[BsT[r]])
                    S.op("act", lambda e, r=r, p=p, h=h, c=c: e.activation(out=PT[p][:], in_=sT[r][:, 0:256], func=AF.Exp, bias=alibk[:, h, c:c + 1]),
                         reads=[BsT[r], Balibk], writes=[BPT[p]])
                    for s in range(2):
                        if n == "own" and c == 1 and s == 0:
                            continue
                        last = (n == "own") and (c == 1 or (c == 0 and s == 0))
                        if n == "own":
                            rhs_fn = (lambda k=k, i=i, c=c: VL[k][:, 2 * i + c, :])
                            rb = BVL[k]
                        else:
                            rhs_fn = (lambda k=k, n=n, c=c: VA[k][:, 2 * n + c, :])
                            rb = BVA[k]
                        S.op("pe", lambda e, s=s, p=p, rhs_fn=rhs_fn, st=first[s], last=last: e.matmul(
                            Ops[s][:, 0:129], lhsT=PT[p][:, s * 128:(s + 1) * 128], rhs=rhs_fn(), start=st, stop=last),
                             reads=[BPT[p], rb], writes=[BO[s]])
                        first[s] = False
                for s in range(2):
                    o = cnt["on"] % 2
                    cnt["on"] += 1
                    S.op("dve", lambda e, s=s: e.reciprocal(out=rden[:, s:s + 1], in_=Ops[s][:, 128:129]), reads=[BO[s]], writes=[Brden])
                    S.op("dve", lambda e, s=s, o=o: e.tensor_scalar(out=on[o][:], in0=Ops[s][:, 0:128], scalar1=rden[:, s:s + 1], scalar2=None,
                                                                   op0=ALU.mult), reads=[BO[s], Brden], writes=[Bon[o]])
                    S.op("pe", lambda e, s=s, o=o: e.transpose(out=oT[:, s, :], in_=on[o][:], identity=ident[:]),
                         reads=[Bon[o], Bident], writes=[BoT])
                S.op("act", lambda e, h=h, qcols=qcols: e.activation(out=oaT[:, h, qcols], in_=oT[:].rearrange("p s q -> p (s q)"), func=AF.Copy),
                     reads=[BoT], writes=[BoaT[h]])
            S.dma("sp", lambda h=h: dr["oaT_d"]()[h * 128:(h + 1) * 128, :], lambda h=h: oaT[:, h, :], BoaT[h], reads=[BoaT[h]])


def p2_oattn(S, dr):
    with Phase(S) as ph:
        oaT = ph.sbuf("oaT2", [128, 8, T], BF16)
        BoaT = S.buf("BoaT2")
        S.dma("sp", lambda: oaT[:], lambda: dr["oaT_d"]().rearrange("(kc p) t -> p kc t", p=128), BoaT, writes=[BoaT])
        G = Gemm(S, ph, 8, "wa")
        acc = [ph.psum(f"acca{i}", [128, 512], F32) for i in range(4)]
        Bacc = S.pbufs("Bacca", 4)
        gt = [ph.sbuf(f"gta{i}", [128, T], BF16) for i in range(2)]
        Bgt = S.bufs("Bgta", 2)
        stg = [ph.sbuf(f"stga{i}", [128, T], BF16) for i in range(2)]
        Bstg = S.bufs("Bstga", 2)
        slots = {0: G.load(dr["w_o_attn"], 0), 1: G.load(dr["w_o_attn"], 512)}
        na = 0
        for b in range(4):
            if b + 2 < 4:
                slots[b + 2] = G.load(dr["w_o_attn"], (b + 2) * 512)
            sl = slots[b]
            for sub in range(4):
                fc = b * 4 + sub
                g = fc % 2
                S.dma("sp", lambda g=g: gt[g][:], lambda fc=fc: dr["gaT"]()[fc * 128:(fc + 1) * 128, :], Bgt[g], writes=[Bgt[g]])
                for tb in range(4):
                    a = na % 4
                    na += 1
                    cols = slice(tb * 512, (tb + 1) * 512)
                    mm_group(S, lambda a=a: acc[a][:],
                             [(lambda kc=kc, sl=sl, sub=sub: G.wb[sl][:, kc, sub * 128:(sub + 1) * 128]) for kc in range(8)],
                             [(lambda kc=kc, cols=cols: oaT[:, kc, cols]) for kc in range(8)],
                             [G.Bw[sl], BoaT], Bacc[a])
                    S.op("dve", lambda e, a=a, g=g, cols=cols: e.tensor_tensor(out=stg[g][:, cols], in0=acc[a][:], in1=gt[g][:, cols], op=ALU.mult),
                         reads=[Bacc[a], Bgt[g]], writes=[Bstg[g]])
                S.dma("sp", lambda fc=fc: dr["m1T_d"]()[fc * 128:(fc + 1) * 128, :], lambda g=g: stg[g][:], Bstg[g], reads=[Bstg[g]])


def p2_retention(S, dr):
    with Phase(S) as ph:
        def cst(name, shape, dt):
            t = ph.sbuf("c_" + name, shape, dt)
            b = S.buf("Bc_" + name)
            S.dma("sp", lambda: t[:], lambda: dr[name](), b, writes=[b])
            return t, b
        ones, Bones = cst("ones", [128, 128], BF16)
        dm, Bdm = cst("dm", [128, 4, 2, 256], F32)
        wsel, Bwsel = cst("wsel", [128, 32], F32)
        cdec, Bcdec = cst("cdec", [128, 4], F32)
        epsc = ph.sbuf("epsc", [128, 1], F32)
        Bepsc = S.buf("Bepsc")
        S.op("dve", lambda e: e.memset(epsc[:], EPS), writes=[Bepsc])

        St = ph.sbuf("St", [128, 4, 2, 512], F32)
        Sb = [ph.sbuf(f"Sb{i}", [128, 4, 2, 512], BF16) for i in range(8)]
        BSb = S.bufs("BSb", 8)
        BSt = S.buf("BSt")
        S.op("dve", lambda e: e.memset(St[:], 0.0), writes=[BSt])
        kdc = [ph.sbuf(f"kdc{i}", [128, 2, 1024], BF16) for i in range(2)]
        vrc = [ph.sbuf(f"vrc{i}", [128, 2, 2048], BF16) for i in range(2)]
        Bkdc, Bvrc = S.bufs("Bkdc", 2), S.bufs("Bvrc", 2)
        qT = [ph.sbuf(f"rq{i}", [128, 8, 256], BF16) for i in range(2)]
        qdT = [ph.sbuf(f"rqd{i}", [128, 8, 256], BF16) for i in range(2)]
        kT = [ph.sbuf(f"rk{i}", [128, 8, 256], BF16) for i in range(2)]
        vl = [ph.sbuf(f"rvl{i}", [128, 2, 2048], BF16) for i in range(2)]
        sg = [ph.sbuf(f"rsg{i}", [128, 16, 256], BF16) for i in range(2)]
        BqT, BqdT, BkT, Bvl, Bsg = (S.bufs(n, 2) for n in ("BrqT", "BrqdT", "BrkT", "Brvl", "Brsg"))
        innm = [ph.sbuf(f"innm{i}", [128, 2, 256], BF16) for i in range(2)]
        Binnm = S.bufs("Binnm", 2)
        Y = [ph.sbuf(f"Y{i}", [128, 4, 256], F32) for i in range(2)]
        Yb = [ph.sbuf(f"Yb{i}", [128, 4, 256], BF16) for i in range(2)]
        Ysq = [ph.sbuf(f"Ysq{i}", [128, 4, 256], BF16) for i in range(2)]
        BY, BYb, BYsq = S.bufs("BY", 2), S.bufs("BYb", 2), S.bufs("BYsq", 2)
        mu = ph.sbuf("mu", [128, 256], F32)
        msq = ph.sbuf("msq", [128, 256], F32)
        var = ph.sbuf("var", [128, 256], F32)
        Bmu, Bmsq, Bvar = S.buf("Bmu"), S.buf("Bmsq"), S.buf("Bvar")
        t1 = ph.sbuf("t1", [128, 4, 256], F32)
        Bt1 = S.buf("Bt1")
        ost = [ph.sbuf(f"ost{i}", [128, 4, 256], BF16) for i in range(2)]
        Bost = S.bufs("Bost", 2)

        inn = [ph.psum(f"inn{i}", [128, 2, 256], F32) for i in range(2)]
        Binn = S.pbufs("Binn", 2)
        yps = [ph.psum(f"yps{i}", [128, 2, 256], F32) for i in range(2)]
        Byps = S.pbufs("Byps", 2)
        stp = ph.psum("stp", [128, 2, 256], F32)
        Bstp = S.pbufs("Bstp", 1)[0]
        kvp = [ph.psum(f"kvp{i}", [128, 512], F32) for i in range(2)]
        Bkvp = S.pbufs("Bkvp", 2)

        def load_chunk(n):
            k = n % 2
            S.dma("sp", lambda: kdc[k][:], lambda: dr["kd_all"]()[n], Bkdc[k], writes=[Bkdc[k]])
            S.dma("sp", lambda: vrc[k][:], lambda: dr["vr_all"]()[n], Bvrc[k], writes=[Bvrc[k]])

        def load_tile(i):
            k = i % 2
            cs = slice(i * 256, (i + 1) * 256)
            for dst, Bd, nm in ((qT, BqT, "qrT"), (qdT, BqdT, "qdT"), (kT, BkT, "krT")):
                S.dma("sp", lambda dst=dst: dst[k][:], lambda nm=nm: dr[nm]()[:, cs].rearrange("(fc p) t -> p fc t", p=128), Bd[k], writes=[Bd[k]])
            S.dma("sp", lambda: vl[k][:], lambda: dr["vr_loc"]()[i], Bvl[k], writes=[Bvl[k]])
            S.dma("sp", lambda: sg[k][:], lambda: dr["sgT"]()[:, cs].rearrange("(fc p) t -> p fc t", p=128), Bsg[k], writes=[Bsg[k]])

        load_chunk(0)
        load_tile(0)
        cnt = {"hh": 0}
        for n in range(NB):
            i, m = divmod(n, 4)
            if n + 1 < NB - 0 and n + 1 <= 30:
                load_chunk(n + 1)
            sbi = (i % 2) * 4 + m
            S.op("act", lambda e, n=n, sbi=sbi: e.activation(out=Sb[sbi][:], in_=St[:], func=AF.Copy, scale=wsel[:, n:n + 1]),
                 reads=[BSt, Bwsel], writes=[BSb[sbi]])
            if m == 3:
                if i + 1 < NT:
                    load_tile(i + 1)
                k = i % 2
                for h in range(4):
                    hh = cnt["hh"] % 2
                    cnt["hh"] += 1
                    for js in range(2):
                        for dc in range(2):
                            S.op("pe", lambda e, hh=hh, js=js, dc=dc, h=h, k=k: e.matmul(
                                inn[hh][:, js, :], lhsT=kT[k][:, h * 2 + dc, js * 128:(js + 1) * 128], rhs=qT[k][:, h * 2 + dc, :],
                                start=(dc == 0), stop=(dc == 1)), reads=[BkT[k], BqT[k]], writes=[Binn[hh]])
                    S.op("dve", lambda e, hh=hh, h=h: e.tensor_tensor(out=innm[hh][:], in0=inn[hh][:], in1=dm[:, h, :, :], op=ALU.mult),
                         reads=[Binn[hh], Bdm], writes=[Binnm[hh]])
                    for ec in range(4):
                        pb, pe_ = divmod(ec, 2)
                        sb0 = (i % 2) * 4
                        lhs = [(lambda js=js, ec=ec, h=h, k=k: vl[k][:, js, h * 512 + ec * 128:h * 512 + (ec + 1) * 128]) for js in range(2)] + \
                              [(lambda dc=dc, ec=ec, h=h, mm=mm, sb0=sb0: Sb[sb0 + mm][:, h, dc, ec * 128:(ec + 1) * 128]) for mm in range(4) for dc in range(2)]
                        rhs = [(lambda js=js, hh=hh: innm[hh][:, js, :]) for js in range(2)] + \
                              [(lambda dc=dc, h=h, k=k: qdT[k][:, h * 2 + dc, :]) for mm in range(4) for dc in range(2)]
                        mm_group(S, (lambda pb=pb, pe_=pe_: yps[pb][:, pe_, :]), lhs, rhs,
                                 [Bvl[k], Binnm[hh], BqdT[k]] + BSb[sb0:sb0 + 4], Byps[pb])
                    for pb in range(2):
                        S.op("act", lambda e, pb=pb, hh=hh: e.activation(out=Y[hh][:, pb * 2:(pb + 1) * 2, :], in_=yps[pb][:], func=AF.Copy),
                             reads=[Byps[pb]], writes=[BY[hh]])
                    S.op("dve", lambda e, hh=hh: e.tensor_copy(out=Yb[hh][:], in_=Y[hh][:]), reads=[BY[hh]], writes=[BYb[hh]])
                    S.op("act", lambda e, hh=hh: e.activation(out=Ysq[hh][:], in_=Y[hh][:], func=AF.Square), reads=[BY[hh]], writes=[BYsq[hh]])
                    mm_group(S, lambda: stp[:, 0, :], [(lambda: ones[:])] * 4, [(lambda ec=ec, hh=hh: Yb[hh][:, ec, :]) for ec in range(4)],
                             [Bones, BYb[hh]], Bstp)
                    mm_group(S, lambda: stp[:, 1, :], [(lambda: ones[:])] * 4, [(lambda ec=ec, hh=hh: Ysq[hh][:, ec, :]) for ec in range(4)],
                             [Bones, BYsq[hh]], Bstp)
                    S.op("dve", lambda e: e.tensor_scalar(out=mu[:], in0=stp[:, 0, :], scalar1=1.0 / 512, scalar2=None, op0=ALU.mult),
                         reads=[Bstp], writes=[Bmu])
                    S.op("dve", lambda e: e.tensor_tensor(out=msq[:], in0=mu[:], in1=mu[:], op=ALU.mult), reads=[Bmu], writes=[Bmsq])
                    S.op("dve", lambda e: e.scalar_tensor_tensor(out=var[:], in0=stp[:, 1, :], scalar=1.0 / 512, in1=msq[:], op0=ALU.mult, op1=ALU.subtract),
                         reads=[Bstp, Bmsq], writes=[Bvar])
                    S.op("act", lambda e: e.activation(out=var[:], in_=var[:], func=AF.Sqrt, bias=epsc[:]), reads=[Bvar, Bepsc], writes=[Bvar])
                    S.op("dve", lambda e: e.reciprocal(out=var[:], in_=var[:]), reads=[Bvar], writes=[Bvar])
                    S.op("dve", lambda e, hh=hh: e.tensor_tensor(out=t1[:], in0=Y[hh][:], in1=mu[:].unsqueeze(1).to_broadcast([128, 4, 256]), op=ALU.subtract),
                         reads=[BY[hh], Bmu], writes=[Bt1])
                    S.op("dve", lambda e: e.tensor_tensor(out=t1[:], in0=t1[:], in1=var[:].unsqueeze(1).to_broadcast([128, 4, 256]), op=ALU.mult),
                         reads=[Bt1, Bvar], writes=[Bt1])
                    S.op("dve", lambda e, hh=hh, h=h, k=k: e.tensor_tensor(out=ost[hh][:], in0=t1[:], in1=sg[k][:, h * 4:(h + 1) * 4, :], op=ALU.mult),
                         reads=[Bt1, Bsg[k]], writes=[Bost[hh]])
                    S.dma("sp", lambda h=h, i=i: dr["orrT_d"]()[h * 512:(h + 1) * 512, i * 256:(i + 1) * 256].rearrange("(ec p) t -> p ec t", p=128),
                          lambda hh=hh: ost[hh][:], Bost[hh], reads=[Bost[hh]])
            if n <= 30:
                kk = n % 2
                for h in range(4):
                    for dc in range(2):
                        mm_group(S, (lambda dc=dc: kvp[dc][:]),
                                 [(lambda js=js, h=h, dc=dc, kk=kk: kdc[kk][:, js, h * 256 + dc * 128:h * 256 + (dc + 1) * 128]) for js in range(2)],
                                 [(lambda js=js, h=h, kk=kk: vrc[kk][:, js, h * 512:(h + 1) * 512]) for js in range(2)],
                                 [Bkdc[kk], Bvrc[kk]], Bkvp[dc])
                        S.op("dve", lambda e, h=h, dc=dc: e.scalar_tensor_tensor(out=St[:, h, dc, :], in0=St[:, h, dc, :], scalar=cdec[:, h:h + 1],
                                                                                in1=kvp[dc][:], op0=ALU.mult, op1=ALU.add),
                             reads=[BSt, Bcdec, Bkvp[dc]], writes=[BSt])


def p2_oret(S, dr):
    with Phase(S) as ph:
        aT = ph.sbuf("orrT", [128, 16, T], BF16)
        BaT = S.buf("BorrT")
        S.dma("sp", lambda: aT[:], lambda: dr["orrT_d"]().rearrange("(kc p) t -> p kc t", p=128), BaT, writes=[BaT])
        G = Gemm(S, ph, 16, "wr")
        acc = [ph.psum(f"accr{i}", [128, 512], F32) for i in range(4)]
        Bacc = S.pbufs("Baccr", 4)
        gt = [ph.sbuf(f"gtr{i}", [128, T], BF16) for i in range(2)]
        m1 = [ph.sbuf(f"m1r{i}", [128, T], BF16) for i in range(2)]
        Bgt, Bm1 = S.bufs("Bgtr", 2), S.bufs("Bm1r", 2)
        tmp = [ph.sbuf(f"tmpr{i}", [128, 512], F32) for i in range(2)]
        Btmp = S.bufs("Btmpr", 2)
        stg = [ph.sbuf(f"stgr{i}", [128, T], BF16) for i in range(2)]
        Bstg = S.bufs("Bstgr", 2)
        slots = {0: G.load(dr["w_o_ret"], 0), 1: G.load(dr["w_o_ret"], 512)}
        na = 0
        for b in range(4):
            if b + 2 < 4:
                slots[b + 2] = G.load(dr["w_o_ret"], (b + 2) * 512)
            sl = slots[b]
            for sub in range(4):
                fc = b * 4 + sub
                g = fc % 2
                S.dma("sp", lambda g=g: gt[g][:], lambda fc=fc: dr["ggT"]()[fc * 128:(fc + 1) * 128, :], Bgt[g], writes=[Bgt[g]])
                S.dma("sp", lambda g=g: m1[g][:], lambda fc=fc: dr["m1T_d"]()[fc * 128:(fc + 1) * 128, :], Bm1[g], writes=[Bm1[g]])
                for tb in range(4):
                    a = na % 4
                    tq = na % 2
                    na += 1
                    cols = slice(tb * 512, (tb + 1) * 512)
                    mm_group(S, lambda a=a: acc[a][:],
                             [(lambda kc=kc, sl=sl, sub=sub: G.wb[sl][:, kc, sub * 128:(sub + 1) * 128]) for kc in range(16)],
                             [(lambda kc=kc, cols=cols: aT[:, kc, cols]) for kc in range(16)],
                             [G.Bw[sl], BaT], Bacc[a])
                    S.op("dve", lambda e, a=a, g=g, cols=cols, tq=tq: e.tensor_tensor(out=tmp[tq][:], in0=acc[a][:], in1=gt[g][:, cols], op=ALU.mult),
                         reads=[Bacc[a], Bgt[g]], writes=[Btmp[tq]])
                    S.op("dve", lambda e, g=g, cols=cols, tq=tq: e.tensor_tensor(out=stg[g][:, cols], in0=tmp[tq][:], in1=m1[g][:, cols], op=ALU.add),
                         reads=[Btmp[tq], Bm1[g]], writes=[Bstg[g]])
                S.dma("sp", lambda fc=fc: dr["mgT_d"]()[fc * 128:(fc + 1) * 128, :], lambda g=g: stg[g][:], Bstg[g], reads=[Bstg[g]])


def p2_wout(S, dr):
    with Phase(S) as ph:
        aT = ph.sbuf("mgT", [128, 16, T], BF16)
        BaT = S.buf("BmgT")
        S.dma("sp", lambda: aT[:], lambda: dr["mgT_d"]().rearrange("(kc p) t -> p kc t", p=128), BaT, writes=[BaT])
        G = Gemm(S, ph, 16, "wo")
        acc = [ph.psum(f"acco{i}", [128, 512], F32) for i in range(4)]
        Bacc = S.pbufs("Bacco", 4)
        xt = [ph.sbuf(f"xto{i}", [128, 512], F32) for i in range(3)]
        Bxt = S.bufs("Bxto", 3)
        so = [ph.sbuf(f"soo{i}", [128, 512], F32) for i in range(3)]
        Bso = S.bufs("Bsoo", 3)
        slots = {0: G.load(dr["w_out"], 0), 1: G.load(dr["w_out"], 512)}
        na = 0
        for b in range(4):
            if b + 2 < 4:
                slots[b + 2] = G.load(dr["w_out"], (b + 2) * 512)
            sl = slots[b]
            cs = slice(b * 512, (b + 1) * 512)
            for tt in range(T // 128):
                a = na % 4
                q = na % 3
                na += 1
                rows = slice(tt * 128, (tt + 1) * 128)
                S.dma("sp", lambda q=q: xt[q][:], lambda rows=rows, cs=cs: dr["x"]()[rows, cs], Bxt[q], writes=[Bxt[q]])
                mm_group(S, lambda a=a: acc[a][:],
                         [(lambda kc=kc, rows=rows: aT[:, kc, rows]) for kc in range(16)],
                         [(lambda kc=kc, sl=sl: G.wb[sl][:, kc, :]) for kc in range(16)],
                         [G.Bw[sl], BaT], Bacc[a])
                S.op("dve", lambda e, a=a, q=q: e.tensor_tensor(out=so[q][:], in0=acc[a][:], in1=xt[q][:], op=ALU.add),
                     reads=[Bacc[a], Bxt[q]], writes=[Bso[q]])
                S.dma("sp", lambda rows=rows, cs=cs: dr["x1_d"]()[rows, cs], lambda q=q: so[q][:], Bso[q], reads=[Bso[q]])


def p2_ffn_in(S, dr):
    with Phase(S) as ph:
        hT = ph.sbuf("h2T", [128, 16, T], BF16)
        BhT = S.buf("Bh2T")
        ident = ph.sbuf("identf", [128, 128], BF16)
        gm = ph.sbuf("gm2", [128, 16], F32)
        Bident, Bgm = S.buf("Bidentf"), S.buf("Bgm2")
        S.dma("sp", lambda: ident[:], lambda: dr["ident"](), Bident, writes=[Bident])
        S.dma("sp", lambda: gm[:], lambda: dr["gffn"](), Bgm, writes=[Bgm])
        epsc = ph.sbuf("epsc2", [128, 1], F32)
        Bepsc = S.buf("Bepsc2")
        S.op("dve", lambda e: e.memset(epsc[:], EPS), writes=[Bepsc])
        pT = [ph.psum(f"pTf{i}", [128, 8, 128], BF16) for i in range(2)]
        BpT = S.pbufs("BpTf", 2)
        G = Gemm(S, ph, 16, "wf", 4)
        order = []
        for b in range(11):
            order += [("g", b), ("u", b)]
        slots = {}

        def ld(idx):
            kind, b = order[idx]
            slots[idx] = G.load(dr["w_ffn_in"], b * 512 + (DFF if kind == "u" else 0))
        ld(0)
        ld(1)
        norm_to_hT(S, ph, dr["x1_d"], gm, Bgm, ident, Bident, hT, BhT, pT, BpT, "n2", epsc, Bepsc)
        accg = [ph.psum(f"accg{i}", [128, 512], F32) for i in range(3)]
        accu = [ph.psum(f"accu{i}", [128, 512], F32) for i in range(3)]
        Baccg, Baccu = S.pbufs("Baccg", 3), S.pbufs("Baccu", 3)
        sgt = [ph.sbuf(f"sgt{i}", [128, 512], F32) for i in range(3)]
        Bsgt = S.bufs("Bsgt", 3)
        stg = [ph.sbuf(f"stgf{i}", [128, T], BF16) for i in range(2)]
        Bstg = S.bufs("Bstgf", 2)
        na = 0
        nf = 0
        for b in range(11):
            if 2 * b + 2 < 22:
                ld(2 * b + 2)
            if 2 * b + 3 < 22:
                ld(2 * b + 3)
            slg, slu = slots[2 * b], slots[2 * b + 1]
            for sub in range(4):
                fb = b * 4 + sub
                g = nf % 2
                nf += 1
                for tb in range(4):
                    a = na % 3
                    na += 1
                    cols = slice(tb * 512, (tb + 1) * 512)
                    for (accx, Bx, slx) in ((accg, Baccg, slg), (accu, Baccu, slu)):
                        mm_group(S, lambda a=a, accx=accx: accx[a][:],
                                 [(lambda kc=kc, slx=slx, sub=sub: G.wb[slx][:, kc, sub * 128:(sub + 1) * 128]) for kc in range(16)],
                                 [(lambda kc=kc, cols=cols: hT[:, kc, cols]) for kc in range(16)],
                                 [G.Bw[slx], BhT], Bx[a])
                    S.op("act", lambda e, a=a: e.activation(out=sgt[a][:], in_=accg[a][:], func=AF.Silu), reads=[Baccg[a]], writes=[Bsgt[a]])
                    S.op("dve", lambda e, a=a, g=g, cols=cols: e.tensor_tensor(out=stg[g][:, cols], in0=accu[a][:], in1=sgt[a][:], op=ALU.mult),
                         reads=[Baccu[a], Bsgt[a]], writes=[Bstg[g]])
                S.dma("sp", lambda fb=fb: dr["aT_d"]()[fb * 128:(fb + 1) * 128, :], lambda g=g: stg[g][:], Bstg[g], reads=[Bstg[g]])


def p2_ffn_out(S, dr):
    KC = DFF // 128
    for th in range(2):
        with Phase(S) as ph:
            aT = ph.sbuf(f"aTh{th}", [128, KC, 1024], BF16)
            BaT = S.buf("BaTh")
            for q in range(4):
                S.dma("sp", lambda q=q: aT[:, q * 11:(q + 1) * 11, :],
                      lambda q=q: dr["aT_d"]()[q * 11 * 128:(q + 1) * 11 * 128, th * 1024:(th + 1) * 1024].rearrange("(kc p) t -> p kc t", p=128),
                      BaT, writes=[BaT])
            wb = [ph.sbuf(f"wfo{th}_{i}", [128, KC, 256], BF16) for i in range(3)]
            Bw = S.bufs("Bwfo", 3)

            def ld(cb):
                sl = cb % 3
                S.dma("pool", lambda: wb[sl][:], lambda: dr["w_ffn_out"]()[:, cb * 256:(cb + 1) * 256].rearrange("(kc p) n -> p kc n", p=128),
                      Bw[sl], writes=[Bw[sl]])
            ld(0)
            ld(1)
            acc = [ph.psum(f"accf{th}_{i}", [128, 512], F32) for i in range(4)]
            Bacc = S.pbufs("Baccf", 4)
            xt = [ph.sbuf(f"xtf{th}_{i}", [128, 256], F32) for i in range(3)]
            Bxt = S.bufs("Bxtf", 3)
            so = [ph.sbuf(f"sof{th}_{i}", [128, 256], F32) for i in range(3)]
            Bso = S.bufs("Bsof", 3)
            na = 0
            for cb in range(8):
                if cb + 2 < 8:
                    ld(cb + 2)
                sl = cb % 3
                cs = slice(cb * 256, (cb + 1) * 256)
                for tt in range(8):
                    a = na % 4
                    q = na % 3
                    na += 1
                    rows = slice(th * 1024 + tt * 128, th * 1024 + (tt + 1) * 128)
                    lrows = slice(tt * 128, (tt + 1) * 128)
                    S.dma("sp", lambda q=q: xt[q][:], lambda rows=rows, cs=cs: dr["x1_d"]()[rows, cs], Bxt[q], writes=[Bxt[q]])
                    mm_group(S, lambda a=a: acc[a][:, 0:256],
                             [(lambda kc=kc, lrows=lrows: aT[:, kc, lrows]) for kc in range(KC)],
                             [(lambda kc=kc, sl=sl: wb[sl][:, kc, :]) for kc in range(KC)],
                             [Bw[sl], BaT], Bacc[a])
                    S.op("dve", lambda e, a=a, q=q: e.tensor_tensor(out=so[q][:], in0=acc[a][:, 0:256], in1=xt[q][:], op=ALU.add),
                         reads=[Bacc[a], Bxt[q]], writes=[Bso[q]])
                    S.dma("sp", lambda rows=rows, cs=cs: dr["xo"]()[rows, cs], lambda q=q: so[q][:], Bso[q], reads=[Bso[q]])


def gen_p2(S, dr):
    for ph in DBG.get("p2_phases", ("attn", "oattn", "ret", "oret", "wout", "ffn_in", "ffn_out")):
        {"attn": (p2_attention if DBG.get("attn_v1") else p2_attention_v2), "oattn": p2_oattn, "ret": p2_retention, "oret": p2_oret, "wout": p2_wout,
         "ffn_in": p2_ffn_in, "ffn_out": p2_ffn_out}[ph](S, dr)


def p2_consts(j):
    sl = alibi_slopes()
    lg = log_g()
    Bi = [block_of(i, j) for i in range(NT)]
    p = np.arange(128)
    validb = np.zeros((128, 8, 32), np.float32)
    tab2 = np.zeros((128, 8, 8, 32), np.float32)
    wsel = np.zeros((128, 32), np.float32)
    for i in range(NT):
        for n in range(32):
            ok = n < Bi[i]
            validb[:, i, n] = 0.0 if ok else -1e30
            for h in range(8):
                tab2[:, i, h, n] = (-sl[h] * 256.0 * (Bi[i] - n) - 30000.0) if ok else -60000.0
        wsel[:, Bi[i]] = 1.0
    alibk = np.zeros((128, 8, 2), np.float32)
    for h in range(8):
        for c in range(2):
            alibk[:, h, c] = sl[h] * (c * 128 + p)
    aq = np.zeros((64, 8, 256), np.float32)
    q = np.arange(256)
    for h in range(8):
        aq[32, h, :] = -sl[h] * q
    selm = np.zeros((64, 32, 128), np.float32)
    for n in range(32):
        selm[n, n, :] = 1.0
        selm[32, n, :] = 1.0
    cm = np.zeros((128, 8, 2, 256), np.float32)
    for h in range(8):
        for c in range(2):
            kk = (c * 128 + p)[:, None]
            cm[:, h, c, :] = np.where(kk <= q[None, :], -sl[h] * q[None, :], -30000.0)
    dm = np.zeros((128, 4, 2, 256), np.float32)
    for h in range(4):
        for js in range(2):
            jj = (js * 128 + p)[:, None].astype(np.float64)
            diff = q[None, :].astype(np.float64) - jj
            dm[:, h, js, :] = np.where(diff >= 0, np.exp(lg[h] * np.maximum(diff, 0.0)), 0.0)
    cdec = np.tile(np.exp(lg * 256.0)[None, :], (128, 1)).astype(np.float32)
    return {"ident": np.eye(128, dtype=np.float32).astype(NPBF), "ones": np.ones((128, 128), np.float32).astype(NPBF),
            "validb": validb, "tab2": tab2, "alibk": alibk, "aq": aq.astype(NPBF), "selm": selm.astype(NPBF),
            "cm": cm.astype(NPBF), "dm": dm, "wsel": wsel, "cdec": cdec}


def vaug_layout(va, nchunk):
    v = va.reshape(nchunk, 128, 8, 128).transpose(2, 1, 0, 3)
    out = np.ones((8, 128, nchunk, 129), dtype=va.dtype)
    out[..., :128] = v
    return out


def chunk_layout(a):
    nch = a.shape[0] // 256
    return np.ascontiguousarray(a.reshape(nch, 2, 128, a.shape[1]).transpose(0, 2, 1, 3))


def shard_tokens(x):
    out = []
    for c in range(8):
        b, j = divmod(c, 4)
        out.append(np.concatenate([x[b, block_of(i, j) * 256:(block_of(i, j) + 1) * 256] for i in range(NT)], 0))
    return out


def unshard_tokens(xs):
    out = np.empty((2, S_LEN, D), xs[0].dtype)
    for c in range(8):
        b, j = divmod(c, 4)
        for i in range(NT):
            B = block_of(i, j)
            out[b, B * 256:(B + 1) * 256] = xs[c][i * 256:(i + 1) * 256]
    return out


def p2_inputs_from_p1(p1res, xs, wl, consts):
    ins = []
    glob = {}
    for b in range(2):
        kaT = np.empty((1024, S_LEN), NPBF)
        va = np.empty((S_LEN, 1024), NPBF)
        kd = np.empty((S_LEN, 1024), NPBF)
        vr = np.empty((S_LEN, 2048), NPBF)
        for j in range(4):
            r = p1res[4 * b + j]
            for i in range(NT):
                B = block_of(i, j)
                g, l = slice(B * 256, (B + 1) * 256), slice(i * 256, (i + 1) * 256)
                kaT[:, g] = r["kaT"][:, l]
                va[g] = r["va"][l]
                kd[g] = r["kd"][l]
                vr[g] = r["vr"][l]
        glob[b] = {"kaT_all": kaT, "vaug_all": vaug_layout(va, 64), "kd_all": chunk_layout(kd), "vr_all": chunk_layout(vr)}
    for c in range(8):
        b, j = divmod(c, 4)
        r = p1res[c]
        d = dict(glob[b])
        d.update({"x": xs[c], "qaT": r["qaT"], "kaT_loc": r["kaT"], "vaug_loc": vaug_layout(np.asarray(r["va"]), 16),
                  "qrT": r["qrT"], "qdT": r["qdT"], "krT": r["krT"], "vr_loc": chunk_layout(np.asarray(r["vr"])),
                  "sgT": r["sgT"], "gaT": r["gaT"], "ggT": r["ggT"]})
        d.update(wl)
        d.update(consts[j])
        ins.append(d)
    return ins


_PROGS = {}


def _prog(name):
    if name not in _PROGS:
        if name == "p1":
            _PROGS[name] = build_program(gen_p1, P1_INS, P1_OUTS)[0]
        else:
            _PROGS[name] = build_program(gen_p2, P2_INS, P2_OUTS, P2_SCRATCH)[0]
    return _PROGS[name]


def _pk(v):
    return np.ascontiguousarray(np.asarray(v, np.float32).reshape(16, 128).T)


def kernel(x, w_in, w_o_attn, w_o_ret, w_out, q_norm, k_norm, norm_mix, norm_ffn, w_ffn_in, w_ffn_out):
    x = np.asarray(x, np.float32)
    cores = list(range(8))
    xs = shard_tokens(x)
    c1 = p1_consts()
    c2 = [p2_consts(j) for j in range(4)]
    p1, p2 = _prog("p1"), _prog("p2")
    for l in range(DEPTH):
        wl1 = {"w_in": np.ascontiguousarray(w_in[l], dtype=np.float32), "gmix": _pk(norm_mix[l]),
               "qkg": np.ascontiguousarray(np.stack([q_norm[l], k_norm[l]], 1), dtype=np.float32)}
        wl1.update(c1)
        ins1 = [dict(wl1, x=xs[c]) for c in cores]
        r1 = run_bass_kernel_spmd(p1, ins1, core_ids=cores).results
        wl2 = {"w_o_attn": np.ascontiguousarray(w_o_attn[l], dtype=np.float32), "w_o_ret": np.ascontiguousarray(w_o_ret[l], dtype=np.float32),
               "w_out": np.ascontiguousarray(w_out[l], dtype=np.float32), "w_ffn_in": np.ascontiguousarray(w_ffn_in[l], dtype=np.float32),
               "w_ffn_out": np.ascontiguousarray(w_ffn_out[l], dtype=np.float32), "gffn": _pk(norm_ffn[l])}
        ins2 = p2_inputs_from_p1(r1, xs, wl2, c2)
        r2 = run_bass_kernel_spmd(p2, ins2, core_ids=cores).results
        xs = [np.asarray(r2[c]["xo"], np.float32) for c in cores]
    return unshard_tokens(xs)


def p2_attention_v2(S, dr):
    SKEW = 2
    with Phase(S) as ph:
        def cst(name, shape, dt):
            t = ph.sbuf("c_" + name, shape, dt)
            b = S.buf("Bc_" + name)
            S.dma("sp", lambda: t[:], lambda: dr[name](), b, writes=[b])
            return t, b
        ident, Bident = cst("ident", [128, 128], BF16)
        validb, Bvalidb = cst("validb", [128, 8, 32], F32)
        tab2, Btab2 = cst("tab2", [128, 8, 8, 32], F32)
        alibk, Balibk = cst("alibk", [128, 8, 2], F32)
        aq, Baq = cst("aq", [64, 8, 256], BF16)
        selm, Bselm = cst("selm", [64, 32, 128], BF16)
        cm, Bcm = cst("cm", [128, 8, 2, 256], BF16)

        KT = [ph.sbuf(f"KT{i}", [128, S_LEN], BF16) for i in range(2)]
        VA = [ph.sbuf(f"VA{i}", [128, 64, 129], BF16) for i in range(2)]
        QT = [ph.sbuf(f"QT{i}", [128, T], BF16) for i in range(2)]
        KL = [ph.sbuf(f"KL{i}", [128, T], BF16) for i in range(2)]
        VL = [ph.sbuf(f"VL{i}", [128, 16, 129], BF16) for i in range(2)]
        BKT, BVA, BQT, BKL, BVL = (S.bufs(n, 2) for n in ("BKT", "BVA", "BQT", "BKL", "BVL"))
        oaT = ph.sbuf("oaT", [128, 8, T], BF16)
        BoaT = S.bufs("BoaT", 8)
        km = ph.sbuf("km", [128, 32], F32)
        kmhf = ph.sbuf("kmhf", [128, 32], F32)
        kmh = [ph.sbuf(f"kmh{i}", [128, 32], BF16) for i in range(2)]
        kml = [ph.sbuf(f"kml{i}", [128, 32], BF16) for i in range(2)]
        Bkm, Bkmhf = S.buf("Bkm"), S.buf("Bkmhf")
        Bkmh, Bkml = S.bufs("Bkmh", 2), S.bufs("Bkml", 2)
        M = [ph.sbuf(f"M{i}", [64, 256], BF16) for i in range(4)]
        BM = S.bufs("BM", 4)
        g2 = ph.sbuf("g2", [128, 2, 32], F32)
        mx8 = ph.sbuf("mx8", [128, 2, 8], F32)
        msk = ph.sbuf("msk", [128, 2, 32], F32)
        mrow = ph.sbuf("mrow", [128, 2, 32], BF16)
        Bg2, Bmx8, Bmsk, Bmrow = (S.buf(n) for n in ("Bg2", "Bmx8", "Bmsk", "Bmrow"))
        NPT = 6
        PT = [ph.sbuf(f"PT{i}", [128, 256], BF16) for i in range(NPT)]
        BPT = S.bufs("BPT", NPT)
        rden = [ph.sbuf(f"rden{i}", [128, 2], F32) for i in range(2)]
        Brden = S.bufs("Brden", 2)
        on = [ph.sbuf(f"on{i}", [128, 2, 128], BF16) for i in range(2)]
        Bon = S.bufs("Bon", 2)

        sT = [ph.psum(f"sT{i}", [128, 512], F32) for i in range(3)]
        BsT = S.pbufs("BsT", 3)
        Ops = [[ph.psum(f"O{a}{s}", [128, 512], F32) for s in range(2)] for a in range(2)]
        BO = [S.pbufs(f"BO{a}", 2) for a in range(2)]
        misc = ph.psum("misc", [128, 512], F32)
        Bmisc = S.pbufs("Bmisc", 1)[0]
        gp = (lambda: misc[:, 0:64].rearrange("p (s n) -> p s n", s=2))
        miscb = (lambda: misc[:].bitcast(BF16))
        mT = (lambda: miscb()[0:32, 256:512])
        oT = (lambda: miscb()[:, 512:768])

        def load_head(h):
            k = h % 2
            S.dma("sp", lambda: KT[k][:], lambda: dr["kaT_all"]()[h * 128:(h + 1) * 128, :], BKT[k], writes=[BKT[k]])
            S.dma("sp", lambda: VA[k][:], lambda: dr["vaug_all"]()[h], BVA[k], writes=[BVA[k]])
            S.dma("sp", lambda: QT[k][:], lambda: dr["qaT"]()[h * 128:(h + 1) * 128, :], BQT[k], writes=[BQT[k]])
            S.dma("sp", lambda: KL[k][:], lambda: dr["kaT_loc"]()[h * 128:(h + 1) * 128, :], BKL[k], writes=[BKL[k]])
            S.dma("sp", lambda: VL[k][:], lambda: dr["vaug_loc"]()[h], BVL[k], writes=[BVL[k]])

        def head_prologue(h):
            k = h % 2
            S.op("dve", lambda e: e.tensor_reduce(out=km[:], in_=KT[k][:].rearrange("p (n s) -> p n s", s=256),
                                                  axis=AX.X, op=ALU.add), reads=[BKT[k]], writes=[Bkm])
            S.op("dve", lambda e: e.tensor_scalar(out=km[:], in0=km[:], scalar1=1.0 / 256, scalar2=None, op0=ALU.mult),
                 reads=[Bkm], writes=[Bkm])
            S.op("dve", lambda e: e.tensor_copy(out=kmh[k][:], in_=km[:]), reads=[Bkm], writes=[Bkmh[k]])
            S.op("dve", lambda e: e.tensor_copy(out=kmhf[:], in_=kmh[k][:]), reads=[Bkmh[k]], writes=[Bkmhf])
            S.op("dve", lambda e: e.tensor_tensor(out=kml[k][:], in0=km[:], in1=kmhf[:], op=ALU.subtract),
                 reads=[Bkm, Bkmhf], writes=[Bkml[k]])
            for par in range(2):
                S.op("dve", lambda e, par=par: e.tensor_copy(out=M[2 * k + par][32:64, :], in_=aq[32:64, h, :]),
                     reads=[Baq], writes=[BM[2 * k + par]])

        def tile_pro_a(h, i):
            k = h % 2
            for s in range(2):
                qs = slice(i * 256 + s * 128, i * 256 + (s + 1) * 128)
                S.op("pe", lambda e, s=s, qs=qs: e.matmul(gp()[:, s, :], lhsT=QT[k][:, qs], rhs=kmh[k][:], start=True, stop=False),
                     reads=[BQT[k], Bkmh[k]], writes=[Bmisc])
                S.op("pe", lambda e, s=s, qs=qs: e.matmul(gp()[:, s, :], lhsT=QT[k][:, qs], rhs=kml[k][:], start=False, stop=True),
                     reads=[BQT[k], Bkml[k]], writes=[Bmisc])
            S.op("dve", lambda e: e.tensor_tensor(out=g2[:], in0=gp(), in1=validb[:, i:i + 1, :].to_broadcast([128, 2, 32]), op=ALU.add),
                 reads=[Bmisc, Bvalidb], writes=[Bg2])
            for s in range(2):
                S.op("dve", lambda e, s=s: e.max(out=mx8[:, s, :], in_=g2[:, s, :]), reads=[Bg2], writes=[Bmx8])
            for s in range(2):
                S.op("dve", lambda e, s=s: e.tensor_scalar(out=msk[:, s, :], in0=g2[:, s, :], scalar1=mx8[:, s, 2:3], scalar2=30000.0,
                                                          op0=ALU.is_ge, op1=ALU.mult), reads=[Bg2, Bmx8], writes=[Bmsk])
            S.op("dve", lambda e: e.tensor_tensor(out=mrow[:], in0=msk[:], in1=tab2[:, i, h:h + 1, :].to_broadcast([128, 2, 32]), op=ALU.add),
                 reads=[Bmsk, Btab2], writes=[Bmrow])

        def tile_pro_b(h, i):
            k = h % 2
            mi = 2 * k + (i % 2)
            for s in range(2):
                S.op("pe", lambda e, s=s: e.transpose(out=mT()[:, s * 128:(s + 1) * 128], in_=mrow[:, s, :], identity=ident[:]),
                     reads=[Bmrow, Bident], writes=[Bmisc])
            S.op("dve", lambda e: e.tensor_copy(out=M[mi][0:32, :], in_=mT()), reads=[Bmisc], writes=[BM[mi]])

        cnt = {"s": 0, "p": 0, "t": 0, "step": 0}
        pending = []

        def flush(force=False):
            while pending and (force or pending[0][0] <= cnt["step"]):
                pending.pop(0)[1]()

        def later(delay, fn):
            due = cnt["step"] + delay
            if pending and due < pending[-1][0]:
                due = pending[-1][0]
            pending.append((due, fn))

        def s_stage(h, i, n, c, ob, first_flags):
            k = h % 2
            mi = 2 * k + (i % 2)
            qcols = slice(i * 256, (i + 1) * 256)
            r = cnt["s"] % 3
            cnt["s"] += 1
            p = cnt["p"] % NPT
            cnt["p"] += 1
            if n == "own":
                ks = slice(i * 256 + c * 128, i * 256 + (c + 1) * 128)
                S.op("pe", lambda e: e.matmul(sT[r][:, 0:256], lhsT=KL[k][:, ks], rhs=QT[k][:, qcols], start=True, stop=False),
                     reads=[BKL[k], BQT[k]], writes=[BsT[r]])
                S.op("pe", lambda e: e.matmul(sT[r][:, 0:256], lhsT=ident[:], rhs=cm[:, h, c, :], start=False, stop=True),
                     reads=[Bident, Bcm], writes=[BsT[r]])
            else:
                ks = slice((2 * n + c) * 128, (2 * n + c + 1) * 128)
                S.op("pe", lambda e: e.matmul(sT[r][:, 0:256], lhsT=KT[k][:, ks], rhs=QT[k][:, qcols], start=True, stop=False),
                     reads=[BKT[k], BQT[k]], writes=[BsT[r]])
                S.op("pe", lambda e: e.matmul(sT[r][:, 0:256], lhsT=selm[:, n, :], rhs=M[mi][:], start=False, stop=True),
                     reads=[Bselm, BM[mi]], writes=[BsT[r]])
            S.op("act", lambda e: e.activation(out=PT[p][:], in_=sT[r][:, 0:256], func=AF.Exp, bias=alibk[:, h, c:c + 1]),
                 reads=[BsT[r], Balibk], writes=[BPT[p]])

            def pv():
                for s in range(2):
                    if n == "own" and c == 1 and s == 0:
                        continue
                    last = (n == "own") and (c == 1 or (c == 0 and s == 0))
                    if n == "own":
                        rhs_fn, rb = (lambda: VL[k][:, 2 * i + c, :]), BVL[k]
                    else:
                        rhs_fn, rb = (lambda: VA[k][:, 2 * n + c, :]), BVA[k]
                    st = first_flags[s]
                    first_flags[s] = False
                    S.op("pe", lambda e, s=s, st=st, last=last, rhs_fn=rhs_fn: e.matmul(
                        Ops[ob][s][:, 0:129], lhsT=PT[p][:, s * 128:(s + 1) * 128], rhs=rhs_fn(), start=st, stop=last),
                         reads=[BPT[p], rb], writes=[BO[ob][s]])
            later(SKEW, pv)

        def epilogue_a(h, i, ob):
            o = cnt["t"] % 2
            cnt["t"] += 1
            for s in range(2):
                S.op("dve", lambda e, s=s: e.reciprocal(out=rden[o][:, s:s + 1], in_=Ops[ob][s][:, 128:129]), reads=[BO[ob][s]], writes=[Brden[o]])
            for s in range(2):
                S.op("dve", lambda e, s=s: e.tensor_scalar(out=on[o][:, s, :], in0=Ops[ob][s][:, 0:128], scalar1=rden[o][:, s:s + 1], scalar2=None,
                                                          op0=ALU.mult), reads=[BO[ob][s], Brden[o]], writes=[Bon[o]])

            def epi_b():
                for s in range(2):
                    S.op("pe", lambda e, s=s: e.transpose(out=oT()[:, s * 128:(s + 1) * 128], in_=on[o][:, s, :], identity=ident[:]),
                         reads=[Bon[o], Bident], writes=[Bmisc])
                S.op("act", lambda e: e.activation(out=oaT[:, h, i * 256:(i + 1) * 256], in_=oT(), func=AF.Copy),
                     reads=[Bmisc], writes=[BoaT[h]])
                if i == NT - 1:
                    S.dma("sp", lambda: dr["oaT_d"]()[h * 128:(h + 1) * 128, :], lambda: oaT[:, h, :], BoaT[h], reads=[BoaT[h]])
            later(2, epi_b)

        load_head(0)
        head_prologue(0)
        tile_pro_a(0, 0)
        tile_pro_b(0, 0)
        tcount = 0
        for h in range(8):
            for i in range(NT):
                ob = tcount % 2
                tcount += 1
                nblk = 4 * i + 3
                steps = [(n, c) for n in range(nblk) for c in range(2)] + [("own", 0), ("own", 1)]
                first_flags = [True, True]
                nxt = (h, i + 1) if i + 1 < NT else ((h + 1, 0) if h + 1 < 8 else None)
                for idx, (n, c) in enumerate(steps):
                    s_stage(h, i, n, c, ob, first_flags)
                    cnt["step"] += 1
                    flush()
                    if i == 0 and idx == 6 and h + 1 < 8:
                        load_head(h + 1)
                    if nxt is not None:
                        if idx == 0:
                            if nxt[1] == 0:
                                head_prologue(nxt[0])
                            tile_pro_a(*nxt)
                        if idx == 3:
                            tile_pro_b(*nxt)
                later(SKEW, (lambda h=h, i=i, ob=ob: epilogue_a(h, i, ob)))
        flush(force=True)
        flush(force=True)
```

```python
import numpy as np
import ml_dtypes
from contextlib import ExitStack

import concourse.bass as bass
import concourse.mybir as mybir
from concourse.bass_utils import run_bass_kernel_spmd

F32 = mybir.dt.float32
BF16 = mybir.dt.bfloat16
AF = mybir.ActivationFunctionType
ALU = mybir.AluOpType
AX = mybir.AxisListType
NPBF = ml_dtypes.bfloat16

D = 2048
S_LEN = 8192
NB = 32
T = 2048
NT = 8
DEPTH = 4
DFF = 5632
EPS = 1e-6
NEG = -30000.0
DBG = {}
IN_COLS = 13312

ENGS = ("pe", "act", "dve", "pool", "sp")
SEM_EPOCH = 20000


class Ev:
    __slots__ = ("eng", "iid", "sem", "val", "buf")

    def __init__(self, eng, iid):
        self.eng, self.iid, self.sem, self.val, self.buf = eng, iid, None, None, None


class Buf:
    __slots__ = ("name", "w", "r", "dsem", "dcnt", "excl", "dq")

    def __init__(self, name, excl=False):
        self.name, self.w, self.r, self.dsem, self.dcnt, self.excl, self.dq = name, None, [], None, 0, excl, None


class Sched:
    def __init__(self, nc, need):
        self.nc = nc
        self.dry = nc is None
        self.need = need
        self.iid = 0
        self.stack = ExitStack()
        if not self.dry:
            self.eng = {"pe": nc.tensor, "act": nc.scalar, "dve": nc.vector, "pool": nc.gpsimd, "sp": nc.sync}
        self.sig = {e: [None, 0, 0] for e in ENGS}
        self.waited = {e: {} for e in ENGS}
        self.last = {e: None for e in ENGS}
        self.live_bufs = []
        self.sem_pool = {}
        self.nsem = 0

    def buf(self, name, excl=False):
        b = Buf(name, excl)
        self.live_bufs.append(b)
        return b

    def bufs(self, name, n, excl=False):
        return [self.buf(f"{name}{i}", excl) for i in range(n)]

    def pbufs(self, name, n):
        return self.bufs(name, n, True)

    def _new_sem(self, name):
        self.nsem += 1
        if self.dry:
            return ("sem", name, self.nsem)
        return self.stack.enter_context(self.nc.semaphore(f"{name}_{self.nsem}"))

    def _dma_sem(self, b, q):
        if b.dsem is None:
            b.dq = q
            pool = self.sem_pool.setdefault(q, [])
            if pool:
                b.dsem, b.dcnt = pool.pop()
            else:
                b.dsem, b.dcnt = self._new_sem("d" + q), 0
        assert b.dq == q, (b.name, b.dq, q)
        return b.dsem

    def _wait(self, eng, sem, val):
        key = id(sem) if not isinstance(sem, tuple) else sem
        if self.waited[eng].get(key, -1) >= val:
            return
        self.waited[eng][key] = val
        if not self.dry:
            self.eng[eng].wait_ge(sem, val)

    def _dep(self, eng, d, raw):
        if d is None:
            return
        if d.eng == "dma":
            self._wait(eng, d.buf.dsem, 16 * d.buf.dcnt)
            return
        if d.eng == eng and eng in ("pe", "sp"):
            return
        if self.dry:
            self.need.add(d.iid)
            return
        assert d.sem is not None, "dependency on non-signalling instruction"
        self._wait(eng, d.sem, d.val)

    def _collect(self, eng, reads, writes):
        for b in reads:
            self._dep(eng, b.w, True)
            if b.excl:
                for r in b.r:
                    self._dep(eng, r, False)
        for b in writes:
            self._dep(eng, b.w, False)
            for r in b.r:
                self._dep(eng, r, False)

    def _commit(self, ev, reads, writes):
        for b in reads:
            if b.excl:
                b.w = ev
                b.r = []
            else:
                b.r.append(ev)
        for b in writes:
            b.w = ev
            b.r = []

    def op(self, eng, fn, reads=(), writes=()):
        iid = self.iid
        self.iid += 1
        ev = Ev(eng, iid)
        self._collect(eng, reads, writes)
        if not self.dry:
            ins = fn(self.eng[eng])
            if iid in self.need:
                sg = self.sig[eng]
                if sg[0] is None or sg[1] >= SEM_EPOCH:
                    sg[0], sg[1] = self._new_sem(eng), 0
                sg[1] += 1
                ev.sem, ev.val = sg[0], sg[1]
                ins.then_inc(sg[0], 1)
        self.last[eng] = ev
        self._commit(ev, reads, writes)
        return ev

    def dma(self, q, out_fn, in_fn, sb, reads=(), writes=(), **kw):
        iid = self.iid
        self.iid += 1
        ev = Ev("dma", iid)
        ev.buf = sb
        self._collect(q, reads, writes)
        sem = self._dma_sem(sb, q)
        sb.dcnt += 1
        if not self.dry:
            self.eng[q].dma_start(out=out_fn(), in_=in_fn(), **kw).then_inc(sem, 16)
        self._commit(ev, reads, writes)
        return ev

    def barrier(self, final=False):
        lasts = dict(self.last)
        dbufs = [b for b in self.live_bufs if b.dsem is not None]
        for e in (("sp",) if final else ENGS):
            for f in ENGS:
                d = lasts[f]
                if d is None or f == e or f == "sp":
                    continue
                if self.dry:
                    self.need.add(d.iid)
                else:
                    self._wait(e, d.sem, d.val)
            for b in dbufs:
                self._wait(e, b.dsem, 16 * b.dcnt)
        for b in dbufs:
            self.sem_pool[b.dq].append((b.dsem, b.dcnt))
        self.live_bufs = []


class Phase:
    _count = [0]

    def __init__(self, S):
        self.S = S
        self.stack = ExitStack()
        Phase._count[0] += 1
        self.pid = Phase._count[0]

    def __enter__(self):
        return self

    def __exit__(self, *a):
        self.S.barrier()
        self.stack.close()
        return False

    def sbuf(self, name, shape, dt):
        if self.S.dry:
            return None
        return self.stack.enter_context(self.S.nc.sbuf_tensor(f"sb{self.pid}_" + name, list(shape), dt))

    def psum(self, name, shape, dt):
        if self.S.dry:
            return None
        return self.stack.enter_context(self.S.nc.psum_tensor(f"ps{self.pid}_" + name, list(shape), dt))


def block_of(i, j):
    return 4 * i + (j if i % 2 == 0 else 3 - j)


def log_g():
    return np.log(1.0 - np.exp2(-5.0 - np.arange(4, dtype=np.float64)))


def alibi_slopes():
    return np.array([0.5 ** (i + 1) for i in range(8)], dtype=np.float64)


class Gemm:
    def __init__(self, S, ph, KC, name="w", nslots=3):
        self.S, self.KC, self.ns = S, KC, nslots
        self.wb = [ph.sbuf(f"{name}b{i}", [128, KC, 512], BF16) for i in range(nslots)]
        self.Bw = S.bufs(f"B{name}", nslots)
        self.n = 0

    def load(self, w_ap_fn, c0, ncols=512, k0=0):
        S = self.S
        sl = self.n % self.ns
        self.n += 1
        KC = self.KC
        S.dma("pool",
              lambda: self.wb[sl][:, :, 0:ncols],
              lambda: w_ap_fn()[k0 * 128:(k0 + KC) * 128, c0:c0 + ncols].rearrange("(kc p) n -> p kc n", p=128),
              self.Bw[sl], writes=[self.Bw[sl]])
        return sl


def mm_group(S, out_fn, lhs_fns, rhs_fns, reads, Bout):
    n = len(lhs_fns)
    for i in range(n):
        S.op("pe", (lambda e, i=i: e.matmul(out_fn(), lhsT=lhs_fns[i](), rhs=rhs_fns[i](),
                                            start=(i == 0), stop=(i == n - 1))),
             reads=reads, writes=[Bout])


def norm_to_hT(S, ph, x_fn, gm, Bgm, ident, Bident, hT, BhT, pT, BpT, tag, epsc, Bepsc):
    xt = [ph.sbuf(f"{tag}xt{i}", [128, D], F32) for i in range(2)]
    xs = [ph.sbuf(f"{tag}xs{i}", [128, D], BF16) for i in range(2)]
    junk = ph.sbuf(f"{tag}junk", [128, D], BF16)
    ss = [ph.sbuf(f"{tag}ss{i}", [128, 1], F32) for i in range(2)]
    rs = [ph.sbuf(f"{tag}rs{i}", [128, 1], F32) for i in range(2)]
    Bxt, Bxs, Bss, Brs = S.bufs(tag + "Bxt", 2), S.bufs(tag + "Bxs", 2), S.bufs(tag + "Bss", 2), S.bufs(tag + "Brs", 2)
    Bjunk = S.buf(tag + "Bjunk")
    for tt in range(T // 128):
        k = tt % 2
        S.dma("sp", lambda k=k: xt[k][:], lambda tt=tt: x_fn()[tt * 128:(tt + 1) * 128, :], Bxt[k], writes=[Bxt[k]])
        S.op("dve", lambda e, k=k: e.memset(ss[k][:], 0.0), writes=[Bss[k]])
        S.op("act", lambda e, k=k: e.activation(out=junk[:], in_=xt[k][:], func=AF.Square, accum_out=ss[k][:]),
             reads=[Bxt[k], Bss[k]], writes=[Bjunk, Bss[k]])
        S.op("act", lambda e, k=k: e.activation(out=rs[k][:], in_=ss[k][:], func=AF.Sqrt, scale=1.0 / D, bias=epsc[:]),
             reads=[Bss[k], Bepsc], writes=[Brs[k]])
        S.op("dve", lambda e, k=k: e.reciprocal(out=rs[k][:], in_=rs[k][:]), reads=[Brs[k]], writes=[Brs[k]])
        S.op("act", lambda e, k=k: e.activation(out=xs[k][:], in_=xt[k][:], func=AF.Copy, scale=rs[k][:]),
             reads=[Bxt[k], Brs[k]], writes=[Bxs[k]])
        for half in range(2):
            for kk in range(8):
                kc = half * 8 + kk
                S.op("pe", lambda e, k=k, kk=kk, kc=kc, half=half: e.transpose(
                    out=pT[half][:, kk, :], in_=xs[k][:, kc * 128:(kc + 1) * 128], identity=ident[:]),
                     reads=[Bxs[k], Bident], writes=[BpT[half]])
            S.op("dve", lambda e, half=half, tt=tt: e.tensor_tensor(
                out=hT[:, half * 8:(half + 1) * 8, tt * 128:(tt + 1) * 128], in0=pT[half][:],
                in1=gm[:, half * 8:(half + 1) * 8].unsqueeze(2).to_broadcast([128, 8, 128]), op=ALU.mult),
                 reads=[BpT[half], Bgm], writes=[BhT])


def gen_p1(S, dr):
    with Phase(S) as ph:
        hT = ph.sbuf("hT", [128, 16, T], BF16)
        BhT = S.buf("BhT")
        ident = ph.sbuf("ident", [128, 128], BF16)
        ones = ph.sbuf("ones", [128, 128], BF16)
        gm = ph.sbuf("gm", [128, 16], F32)
        qg = ph.sbuf("qg", [128, 2], F32)
        qdec = ph.sbuf("qdec", [128, 4, 512], F32)
        kdec = ph.sbuf("kdec", [128, 2, 1024], F32)
        Bident, Bones, Bgm, Bqg, Bqdec, Bkdec = (S.buf(n) for n in ("Bident", "Bones", "Bgm", "Bqg", "Bqdec", "Bkdec"))
        S.dma("sp", lambda: ident[:], lambda: dr["ident"](), Bident, writes=[Bident])
        S.dma("sp", lambda: ones[:], lambda: dr["ones"](), Bones, writes=[Bones])
        S.dma("sp", lambda: gm[:], lambda: dr["gmix"](), Bgm, writes=[Bgm])
        S.dma("sp", lambda: qg[:], lambda: dr["qkg"](), Bqg, writes=[Bqg])
        S.dma("sp", lambda: qdec[:], lambda: dr["qdec"](), Bqdec, writes=[Bqdec])
        S.dma("sp", lambda: kdec[:], lambda: dr["kdec"](), Bkdec, writes=[Bkdec])
        epsc = ph.sbuf("epsc", [128, 1], F32)
        Bepsc = S.buf("Bepsc")
        S.op("dve", lambda e: e.memset(epsc[:], EPS), writes=[Bepsc])
        S.op("dve", lambda e: e.tensor_scalar(out=qg[:, 0:1], in0=qg[:, 0:1], scalar1=float(128 ** -0.5), scalar2=None,
                                              op0=ALU.mult), reads=[Bqg], writes=[Bqg])

        pT = [ph.psum(f"pT{i}", [128, 8, 128], BF16) for i in range(2)]
        BpT = S.pbufs("BpT", 2)
        acc = [ph.psum(f"acc{i}", [128, 512], F32) for i in range(4)]
        Bacc = S.pbufs("Bacc", 4)
        ssp = [ph.psum(f"ssp{i}", [128, 512], F32) for i in range(2)]
        Bssp = S.pbufs("Bssp", 2)

        G = Gemm(S, ph, 16)
        nblk = IN_COLS // 512
        slots = {}
        for b in range(2):
            slots[b] = G.load(dr["w_in"], b * 512)

        norm_to_hT(S, ph, dr["x"], gm, Bgm, ident, Bident, hT, BhT, pT, BpT, "n1", epsc, Bepsc)

        stg = [ph.sbuf(f"stg{i}", [128, T], BF16) for i in range(3)]
        Bstg = S.bufs("Bstg", 3)
        stt = [ph.sbuf(f"stt{i}", [128, 512], BF16) for i in range(4)]
        Bstt = S.bufs("Bstt", 4)
        sq = [ph.sbuf(f"sq{i}", [128, 512], BF16) for i in range(2)]
        Bsq = S.bufs("Bsq", 2)
        rr = [ph.sbuf(f"rr{i}", [128, 512], F32) for i in range(2)]
        Brr = S.bufs("Brr", 2)
        cnt = {"acc": 0, "stg": 0, "stt": 0, "sq": 0}

        def fm_block(sl, sub, kind, out_rows):
            outs = out_rows if isinstance(out_rows, list) else [out_rows]
            sgs = []
            for _ in outs:
                sgs.append(cnt["stg"] % 3)
                cnt["stg"] += 1
            for tb in range(4):
                a = cnt["acc"] % 4
                cnt["acc"] += 1
                mm_group(S, lambda a=a: acc[a][:],
                         [(lambda kc=kc: G.wb[sl][:, kc, sub * 128:(sub + 1) * 128]) for kc in range(16)],
                         [(lambda kc=kc, tb=tb: hT[:, kc, tb * 512:(tb + 1) * 512]) for kc in range(16)],
                         [G.Bw[sl], BhT], Bacc[a])
                cols = slice(tb * 512, (tb + 1) * 512)
                if kind in ("qn", "kn"):
                    q = cnt["sq"] % 2
                    cnt["sq"] += 1
                    gc = 0 if kind == "qn" else 1
                    S.op("act", lambda e, a=a, q=q: e.activation(out=sq[q][:], in_=acc[a][:], func=AF.Square),
                         reads=[Bacc[a]], writes=[Bsq[q]])
                    S.op("pe", lambda e, q=q: e.matmul(ssp[q][:], lhsT=ones[:], rhs=sq[q][:], start=True, stop=True),
                         reads=[Bones, Bsq[q]], writes=[Bssp[q]])
                    S.op("act", lambda e, q=q: e.activation(out=rr[q][:], in_=ssp[q][:], func=AF.Sqrt, scale=1.0 / 128, bias=epsc[:]),
                         reads=[Bssp[q], Bepsc], writes=[Brr[q]])
                    S.op("dve", lambda e, q=q: e.reciprocal(out=rr[q][:], in_=rr[q][:]), reads=[Brr[q]], writes=[Brr[q]])
                    S.op("dve", lambda e, a=a, q=q, gc=gc, cols=cols, sg=sgs[0]: e.scalar_tensor_tensor(
                        out=stg[sg][:, cols], in0=acc[a][:], scalar=qg[:, gc:gc + 1], in1=rr[q][:],
                        op0=ALU.mult, op1=ALU.mult), reads=[Bacc[a], Brr[q], Bqg], writes=[Bstg[sgs[0]]])
                elif kind.startswith("qr"):
                    h = int(kind[2])
                    S.op("act", lambda e, a=a, cols=cols, sg=sgs[0]: e.activation(out=stg[sg][:, cols], in_=acc[a][:], func=AF.Copy),
                         reads=[Bacc[a]], writes=[Bstg[sgs[0]]])
                    S.op("dve", lambda e, a=a, cols=cols, sg=sgs[1], h=h: e.tensor_tensor(
                        out=stg[sg][:, cols], in0=acc[a][:], in1=qdec[:, h, :], op=ALU.mult),
                         reads=[Bacc[a], Bqdec], writes=[Bstg[sgs[1]]])
                elif kind == "kr":
                    S.op("act", lambda e, a=a, cols=cols, sg=sgs[0]: e.activation(out=stg[sg][:, cols], in_=acc[a][:], func=AF.Copy, scale=1.0 / 16),
                         reads=[Bacc[a]], writes=[Bstg[sgs[0]]])
                elif kind == "silu":
                    S.op("act", lambda e, a=a, cols=cols, sg=sgs[0]: e.activation(out=stg[sg][:, cols], in_=acc[a][:], func=AF.Silu),
                         reads=[Bacc[a]], writes=[Bstg[sgs[0]]])
                elif kind == "sig":
                    S.op("act", lambda e, a=a, cols=cols, sg=sgs[0]: e.activation(out=stg[sg][:, cols], in_=acc[a][:], func=AF.Sigmoid),
                         reads=[Bacc[a]], writes=[Bstg[sgs[0]]])
                else:
                    raise ValueError(kind)
            for sg, o in zip(sgs, outs):
                S.dma("sp", o, lambda sg=sg: stg[sg][:], Bstg[sg], reads=[Bstg[sg]])

        def tm_block(sl, kind, out_fn):
            for tt in range(T // 128):
                a = cnt["acc"] % 4
                cnt["acc"] += 1
                mm_group(S, lambda a=a: acc[a][:],
                         [(lambda kc=kc, tt=tt: hT[:, kc, tt * 128:(tt + 1) * 128]) for kc in range(16)],
                         [(lambda kc=kc: G.wb[sl][:, kc, :]) for kc in range(16)],
                         [G.Bw[sl], BhT], Bacc[a])
                st = cnt["stt"] % 4
                cnt["stt"] += 1
                if kind == "v":
                    S.op("act", lambda e, a=a, st=st: e.activation(out=stt[st][:], in_=acc[a][:], func=AF.Copy),
                         reads=[Bacc[a]], writes=[Bstt[st]])
                else:
                    c0 = kind[1]
                    S.op("dve", lambda e, a=a, st=st, tt=tt, c0=c0: e.tensor_tensor(
                        out=stt[st][:], in0=acc[a][:], in1=kdec[:, tt % 2, c0:c0 + 512], op=ALU.mult),
                         reads=[Bacc[a], Bkdec], writes=[Bstt[st]])
                S.dma("sp", (lambda tt=tt: out_fn(tt)), lambda st=st: stt[st][:], Bstt[st], reads=[Bstt[st]])

        for b in DBG.get('blocks', range(nblk)):
            if b + 2 < nblk and 'blocks' not in DBG:
                slots[b + 2] = G.load(dr["w_in"], (b + 2) * 512)
            if 'blocks' in DBG:
                slots[b] = G.load(dr["w_in"], b * 512)
            sl = slots[b]
            c0 = b * 512
            if c0 < 1024:
                for sub in range(4):
                    hh = (c0 // 128) + sub
                    fm_block(sl, sub, "qn", (lambda hh=hh: dr["qaT"]()[hh * 128:(hh + 1) * 128, :]))
            elif c0 < 2048:
                for sub in range(4):
                    hh = ((c0 - 1024) // 128) + sub
                    fm_block(sl, sub, "kn", (lambda hh=hh: dr["kaT"]()[hh * 128:(hh + 1) * 128, :]))
            elif c0 < 3072:
                cc = c0 - 2048
                tm_block(sl, "v", (lambda tt, cc=cc: dr["va"]()[tt * 128:(tt + 1) * 128, cc:cc + 512]))
            elif c0 < 4096:
                for sub in range(4):
                    f = (c0 - 3072) // 128 + sub
                    fm_block(sl, sub, f"qr{f // 2}", [(lambda f=f: dr["qrT"]()[f * 128:(f + 1) * 128, :]),
                                                       (lambda f=f: dr["qdT"]()[f * 128:(f + 1) * 128, :])])
            elif c0 < 5120:
                for sub in range(4):
                    f = (c0 - 4096) // 128 + sub
                    fm_block(sl, sub, "kr", (lambda f=f: dr["krT"]()[f * 128:(f + 1) * 128, :]))
                cc = c0 - 4096
                tm_block(sl, ("kd", cc), (lambda tt, cc=cc: dr["kd"]()[tt * 128:(tt + 1) * 128, cc:cc + 512]))
            elif c0 < 7168:
                cc = c0 - 5120
                tm_block(sl, "v", (lambda tt, cc=cc: dr["vr"]()[tt * 128:(tt + 1) * 128, cc:cc + 512]))
            elif c0 < 9216:
                for sub in range(4):
                    f = (c0 - 7168) // 128 + sub
                    fm_block(sl, sub, "silu", (lambda f=f: dr["sgT"]()[f * 128:(f + 1) * 128, :]))
            elif c0 < 11264:
                for sub in range(4):
                    f = (c0 - 9216) // 128 + sub
                    fm_block(sl, sub, "sig", (lambda f=f: dr["gaT"]()[f * 128:(f + 1) * 128, :]))
            else:
                for sub in range(4):
                    f = (c0 - 11264) // 128 + sub
                    fm_block(sl, sub, "sig", (lambda f=f: dr["ggT"]()[f * 128:(f + 1) * 128, :]))


P1_OUTS = {"qaT": [1024, T], "kaT": [1024, T], "va": [T, 1024], "qrT": [1024, T], "qdT": [1024, T],
           "krT": [1024, T], "kd": [T, 1024], "vr": [T, 2048], "sgT": [2048, T], "gaT": [2048, T], "ggT": [2048, T]}
P1_INS = {"x": ([T, D], F32), "w_in": ([D, IN_COLS], F32), "ident": ([128, 128], BF16), "ones": ([128, 128], BF16),
          "gmix": ([128, 16], F32), "qkg": ([128, 2], F32), "qdec": ([128, 4, 512], F32), "kdec": ([128, 2, 1024], F32)}


def build_program(gen, ins, outs, scratch=None):
    need = set()
    scratch = scratch or {}
    S0 = Sched(None, need)
    gen(S0, {k: (lambda: None) for k in list(ins) + list(outs) + list(scratch)})
    S0.barrier(final=True)
    nc = bass.Bass("TRN2", target_bir_lowering=False)
    aps = {}
    for k, (shape, dt) in ins.items():
        aps[k] = nc.dram_tensor(k, list(shape), dt, kind="ExternalInput").ap()
    for k, shape in outs.items():
        dt = BF16
        if isinstance(shape, tuple):
            shape, dt = shape
        aps[k] = nc.dram_tensor(k, list(shape), dt, kind="ExternalOutput").ap()
    for k, (shape, dt) in scratch.items():
        aps[k] = nc.dram_tensor(k, list(shape), dt).ap()
    S = Sched(nc, need)
    with S.stack:
        gen(S, {k: (lambda k=k: aps[k]) for k in aps})
        S.barrier(final=True)
    assert S.iid == S0.iid, (S.iid, S0.iid)
    return nc, S


def p1_consts():
    lg = log_g()
    i = np.arange(256, dtype=np.float64)
    qd = np.exp(lg[:, None] * (i[None, :] + 1.0))
    qdec = np.tile(np.concatenate([qd, qd], axis=1)[None], (128, 1, 1)).astype(np.float32)
    p = np.arange(128)
    kdec = np.zeros((128, 2, 1024), np.float32)
    for s in range(2):
        for h in range(4):
            kdec[:, s, h * 256:(h + 1) * 256] = (np.exp(lg[h] * (255.0 - (s * 128 + p))) / 16.0)[:, None]
    return {"ident": np.eye(128, dtype=np.float32).astype(NPBF), "ones": np.ones((128, 128), np.float32).astype(NPBF),
            "qdec": qdec, "kdec": kdec}


P2_INS = {
    "x": ([T, D], F32),
    "qaT": ([1024, T], BF16), "kaT_all": ([1024, S_LEN], BF16), "vaug_all": ([8, 128, 64, 129], BF16),
    "kaT_loc": ([1024, T], BF16), "vaug_loc": ([8, 128, 16, 129], BF16),
    "qrT": ([1024, T], BF16), "qdT": ([1024, T], BF16), "krT": ([1024, T], BF16),
    "kd_all": ([32, 128, 2, 1024], BF16), "vr_all": ([32, 128, 2, 2048], BF16), "vr_loc": ([8, 128, 2, 2048], BF16),
    "sgT": ([2048, T], BF16), "gaT": ([2048, T], BF16), "ggT": ([2048, T], BF16),
    "w_o_attn": ([1024, D], F32), "w_o_ret": ([2048, D], F32), "w_out": ([D, D], F32),
    "w_ffn_in": ([D, 2 * DFF], F32), "w_ffn_out": ([DFF, D], F32), "gffn": ([128, 16], F32),
    "ident": ([128, 128], BF16), "ones": ([128, 128], BF16),
    "validb": ([128, 8, 32], F32), "tab2": ([128, 8, 8, 32], F32), "alibk": ([128, 8, 2], F32),
    "aq": ([64, 8, 256], BF16), "selm": ([64, 32, 128], BF16), "cm": ([128, 8, 2, 256], BF16),
    "dm": ([128, 4, 2, 256], F32), "wsel": ([128, 32], F32), "cdec": ([128, 4], F32),
}
P2_OUTS = {"xo": ([T, D], F32)}
P2_SCRATCH = {"oaT_d": ([1024, T], BF16), "m1T_d": ([2048, T], BF16), "orrT_d": ([2048, T], BF16),
              "mgT_d": ([2048, T], BF16), "x1_d": ([T, D], F32), "aT_d": ([DFF, T], BF16)}


def p2_attention(S, dr):
    with Phase(S) as ph:
        def cst(name, shape, dt):
            t = ph.sbuf("c_" + name, shape, dt)
            b = S.buf("Bc_" + name)
            S.dma("sp", lambda: t[:], lambda: dr[name](), b, writes=[b])
            return t, b
        ident, Bident = cst("ident", [128, 128], BF16)
        validb, Bvalidb = cst("validb", [128, 8, 32], F32)
        tab2, Btab2 = cst("tab2", [128, 8, 8, 32], F32)
        alibk, Balibk = cst("alibk", [128, 8, 2], F32)
        aq, Baq = cst("aq", [64, 8, 256], BF16)
        selm, Bselm = cst("selm", [64, 32, 128], BF16)
        cm, Bcm = cst("cm", [128, 8, 2, 256], BF16)

        KT = [ph.sbuf(f"KT{i}", [128, S_LEN], BF16) for i in range(2)]
        VA = [ph.sbuf(f"VA{i}", [128, 64, 129], BF16) for i in range(2)]
        QT = [ph.sbuf(f"QT{i}", [128, T], BF16) for i in range(2)]
        KL = [ph.sbuf(f"KL{i}", [128, T], BF16) for i in range(2)]
        VL = [ph.sbuf(f"VL{i}", [128, 16, 129], BF16) for i in range(2)]
        BKT, BVA, BQT, BKL, BVL = (S.bufs(n, 2) for n in ("BKT", "BVA", "BQT", "BKL", "BVL"))
        oaT = ph.sbuf("oaT", [128, 8, T], BF16)
        BoaT = S.bufs("BoaT", 8)
        km = ph.sbuf("km", [128, 32], F32)
        kmh = ph.sbuf("kmh", [128, 32], BF16)
        kmhf = ph.sbuf("kmhf", [128, 32], F32)
        kml = ph.sbuf("kml", [128, 32], BF16)
        Bkm, Bkmh, Bkmhf, Bkml = (S.buf(n) for n in ("Bkm", "Bkmh", "Bkmhf", "Bkml"))
        M = [ph.sbuf(f"M{i}", [64, 256], BF16) for i in range(2)]
        BM = S.bufs("BM", 2)
        g2 = ph.sbuf("g2", [128, 2, 32], F32)
        mx8 = ph.sbuf("mx8", [128, 2, 8], F32)
        msk = ph.sbuf("msk", [128, 2, 32], F32)
        mrow = ph.sbuf("mrow", [128, 2, 32], BF16)
        Bg2, Bmx8, Bmsk, Bmrow = (S.buf(n) for n in ("Bg2", "Bmx8", "Bmsk", "Bmrow"))
        PT = [ph.sbuf(f"PT{i}", [128, 256], BF16) for i in range(4)]
        BPT = S.bufs("BPT", 4)
        rden = ph.sbuf("rden", [128, 2], F32)
        Brden = S.buf("Brden")
        on = [ph.sbuf(f"on{i}", [128, 128], BF16) for i in range(2)]
        Bon = S.bufs("Bon", 2)

        sT = [ph.psum(f"sT{i}", [128, 512], F32) for i in range(3)]
        BsT = S.pbufs("BsT", 3)
        Ops = [ph.psum(f"O{i}", [128, 512], F32) for i in range(2)]
        BO = S.pbufs("BO", 2)
        gp = ph.psum("gp", [128, 2, 32], F32)
        Bgp = S.pbufs("Bgp", 1)[0]
        mT = ph.psum("mT", [32, 256], BF16)
        BmT = S.pbufs("BmT", 1)[0]
        oT = ph.psum("oT", [128, 2, 128], BF16)
        BoT = S.pbufs("BoT", 1)[0]

        def load_head(h):
            k = h % 2
            S.dma("sp", lambda: KT[k][:], lambda: dr["kaT_all"]()[h * 128:(h + 1) * 128, :], BKT[k], writes=[BKT[k]])
            S.dma("sp", lambda: VA[k][:], lambda: dr["vaug_all"]()[h], BVA[k], writes=[BVA[k]])
            S.dma("sp", lambda: QT[k][:], lambda: dr["qaT"]()[h * 128:(h + 1) * 128, :], BQT[k], writes=[BQT[k]])
            S.dma("sp", lambda: KL[k][:], lambda: dr["kaT_loc"]()[h * 128:(h + 1) * 128, :], BKL[k], writes=[BKL[k]])
            S.dma("sp", lambda: VL[k][:], lambda: dr["vaug_loc"]()[h], BVL[k], writes=[BVL[k]])

        load_head(0)
        cnt = {"s": 0, "p": 0, "on": 0}
        for h in range(8):
            k = h % 2
            if h + 1 < 8:
                load_head(h + 1)
            S.op("dve", lambda e, k=k: e.tensor_reduce(out=km[:], in_=KT[k][:].rearrange("p (n s) -> p n s", s=256),
                                                       axis=AX.X, op=ALU.add), reads=[BKT[k]], writes=[Bkm])
            S.op("dve", lambda e: e.tensor_scalar(out=km[:], in0=km[:], scalar1=1.0 / 256, scalar2=None, op0=ALU.mult),
                 reads=[Bkm], writes=[Bkm])
            S.op("dve", lambda e: e.tensor_copy(out=kmh[:], in_=km[:]), reads=[Bkm], writes=[Bkmh])
            S.op("dve", lambda e: e.tensor_copy(out=kmhf[:], in_=kmh[:]), reads=[Bkmh], writes=[Bkmhf])
            S.op("dve", lambda e: e.tensor_tensor(out=kml[:], in0=km[:], in1=kmhf[:], op=ALU.subtract),
                 reads=[Bkm, Bkmhf], writes=[Bkml])
            for par in range(2):
                S.op("dve", lambda e, par=par, h=h: e.tensor_copy(out=M[par][32:64, :], in_=aq[32:64, h, :]),
                     reads=[Baq], writes=[BM[par]])
            for i in range(NT):
                par = i % 2
                qcols = slice(i * 256, (i + 1) * 256)
                for s in range(2):
                    qs = slice(i * 256 + s * 128, i * 256 + (s + 1) * 128)
                    S.op("pe", lambda e, s=s, qs=qs, k=k: e.matmul(gp[:, s, :], lhsT=QT[k][:, qs], rhs=kmh[:], start=True, stop=False),
                         reads=[BQT[k], Bkmh], writes=[Bgp])
                    S.op("pe", lambda e, s=s, qs=qs, k=k: e.matmul(gp[:, s, :], lhsT=QT[k][:, qs], rhs=kml[:], start=False, stop=True),
                         reads=[BQT[k], Bkml], writes=[Bgp])
                S.op("dve", lambda e, i=i: e.tensor_tensor(out=g2[:], in0=gp[:], in1=validb[:, i:i + 1, :].to_broadcast([128, 2, 32]),
                                                          op=ALU.add), reads=[Bgp, Bvalidb], writes=[Bg2])
                for s in range(2):
                    S.op("dve", lambda e, s=s: e.max(out=mx8[:, s, :], in_=g2[:, s, :]), reads=[Bg2], writes=[Bmx8])
                for s in range(2):
                    S.op("dve", lambda e, s=s: e.tensor_scalar(out=msk[:, s, :], in0=g2[:, s, :], scalar1=mx8[:, s, 2:3], scalar2=30000.0,
                                                              op0=ALU.is_ge, op1=ALU.mult), reads=[Bg2, Bmx8], writes=[Bmsk])
                S.op("dve", lambda e, i=i, h=h: e.tensor_tensor(out=mrow[:], in0=msk[:], in1=tab2[:, i, h:h + 1, :].to_broadcast([128, 2, 32]),
                                                              op=ALU.add), reads=[Bmsk, Btab2], writes=[Bmrow])
                for s in range(2):
                    S.op("pe", lambda e, s=s: e.transpose(out=mT[:, s * 128:(s + 1) * 128], in_=mrow[:, s, :], identity=ident[:]),
                         reads=[Bmrow, Bident], writes=[BmT])
                S.op("dve", lambda e, par=par: e.tensor_copy(out=M[par][0:32, :], in_=mT[:]), reads=[BmT], writes=[BM[par]])
                nblk = 4 * i + 3
                steps = [(n, c) for n in range(nblk) for c in range(2)] + [("own", 0), ("own", 1)]
                first = [True, True]
                for (n, c) in steps:
                    r = cnt["s"] % 3
                    cnt["s"] += 1
                    p = cnt["p"] % 4
                    cnt["p"] += 1
                    if n == "own":
                        ks = slice(i * 256 + c * 128, i * 256 + (c + 1) * 128)
                        S.op("pe", lambda e, r=r, ks=ks, k=k, qcols=qcols: e.matmul(sT[r][:, 0:256], lhsT=KL[k][:, ks], rhs=QT[k][:, qcols], start=True, stop=False),
                             reads=[BKL[k], BQT[k]], writes=[BsT[r]])
                        S.op("pe", lambda e, r=r, c=c, h=h: e.matmul(sT[r][:, 0:256], lhsT=ident[:], rhs=cm[:, h, c, :], start=False, stop=True),
                             reads=[Bident, Bcm], writes=[BsT[r]])
                    else:
                        ks = slice((2 * n + c) * 128, (2 * n + c + 1) * 128)
                        S.op("pe", lambda e, r=r, ks=ks, k=k, qcols=qcols: e.matmul(sT[r][:, 0:256], lhsT=KT[k][:, ks], rhs=QT[k][:, qcols], start=True, stop=False),
                             reads=[BKT[k], BQT[k]], writes=[BsT[r]])
                        S.op("pe", lambda e, r=r, n=n, par=par: e.matmul(sT[r][:, 0:256], lhsT=selm[:, n, :], rhs=M[par][:], start=False, stop=True),
                             reads=[Bselm, BM[par]], writes=[BsT[r]])
                    S.op("act", lambda e, r=r, p=p, h=h, c=c: e.activation(out=PT[p][:], in_=sT[r][:, 0:256], func=AF.Exp, bias=alibk[:, h, c:c + 1]),
                         reads=[BsT[r], Balibk], writes=[BPT[p]])
                    for s in range(2):
                        if n == "own" and c == 1 and s == 0:
                            continue
                        last = (n == "own") and (c == 1 or (c == 0 and s == 0))
                        if n == "own":
                            rhs_fn = (lambda k=k, i=i, c=c: VL[k][:, 2 * i + c, :])
                            rb = BVL[k]
                        else:
                            rhs_fn = (lambda k=k, n=n, c=c: VA[k][:, 2 * n + c, :])
                            rb = BVA[k]
                        S.op("pe", lambda e, s=s, p=p, rhs_fn=rhs_fn, st=first[s], last=last: e.matmul(
                            Ops[s][:, 0:129], lhsT=PT[p][:, s * 128:(s + 1) * 128], rhs=rhs_fn(), start=st, stop=last),
                             reads=[BPT[p], rb], writes=[BO[s]])
                        first[s] = False
                for s in range(2):
                    o = cnt["on"] % 2
                    cnt["on"] += 1
                    S.op("dve", lambda e, s=s: e.reciprocal(out=rden[:, s:s + 1], in_=Ops[s][:, 128:129]), reads=[BO[s]], writes=[Brden])
                    S.op("dve", lambda e, s=s, o=o: e.tensor_scalar(out=on[o][:], in0=Ops[s][:, 0:128], scalar1=rden[:, s:s + 1], scalar2=None,
                                                                   op0=ALU.mult), reads=[BO[s], Brden], writes=[Bon[o]])
                    S.op("pe", lambda e, s=s, o=o: e.transpose(out=oT[:, s, :], in_=on[o][:], identity=ident[:]),
                         reads=[Bon[o], Bident], writes=[BoT])
                S.op("act", lambda e, h=h, qcols=qcols: e.activation(out=oaT[:, h, qcols], in_=oT[:].rearrange("p s q -> p (s q)"), func=AF.Copy),
                     reads=[BoT], writes=[BoaT[h]])
            S.dma("sp", lambda h=h: dr["oaT_d"]()[h * 128:(h + 1) * 128, :], lambda h=h: oaT[:, h, :], BoaT[h], reads=[BoaT[h]])


def p2_oattn(S, dr):
    with Phase(S) as ph:
        oaT = ph.sbuf("oaT2", [128, 8, T], BF16)
        BoaT = S.buf("BoaT2")
        S.dma("sp", lambda: oaT[:], lambda: dr["oaT_d"]().rearrange("(kc p) t -> p kc t", p=128), BoaT, writes=[BoaT])
        G = Gemm(S, ph, 8, "wa")
        acc = [ph.psum(f"acca{i}", [128, 512], F32) for i in range(4)]
        Bacc = S.pbufs("Bacca", 4)
        gt = [ph.sbuf(f"gta{i}", [128, T], BF16) for i in range(2)]
        Bgt = S.bufs("Bgta", 2)
        stg = [ph.sbuf(f"stga{i}", [128, T], BF16) for i in range(2)]
        Bstg = S.bufs("Bstga", 2)
        slots = {0: G.load(dr["w_o_attn"], 0), 1: G.load(dr["w_o_attn"], 512)}
        na = 0
        for b in range(4):
            if b + 2 < 4:
                slots[b + 2] = G.load(dr["w_o_attn"], (b + 2) * 512)
            sl = slots[b]
            for sub in range(4):
                fc = b * 4 + sub
                g = fc % 2
                S.dma("sp", lambda g=g: gt[g][:], lambda fc=fc: dr["gaT"]()[fc * 128:(fc + 1) * 128, :], Bgt[g], writes=[Bgt[g]])
                for tb in range(4):
                    a = na % 4
                    na += 1
                    cols = slice(tb * 512, (tb + 1) * 512)
                    mm_group(S, lambda a=a: acc[a][:],
                             [(lambda kc=kc, sl=sl, sub=sub: G.wb[sl][:, kc, sub * 128:(sub + 1) * 128]) for kc in range(8)],
                             [(lambda kc=kc, cols=cols: oaT[:, kc, cols]) for kc in range(8)],
                             [G.Bw[sl], BoaT], Bacc[a])
                    S.op("dve", lambda e, a=a, g=g, cols=cols: e.tensor_tensor(out=stg[g][:, cols], in0=acc[a][:], in1=gt[g][:, cols], op=ALU.mult),
                         reads=[Bacc[a], Bgt[g]], writes=[Bstg[g]])
                S.dma("sp", lambda fc=fc: dr["m1T_d"]()[fc * 128:(fc + 1) * 128, :], lambda g=g: stg[g][:], Bstg[g], reads=[Bstg[g]])


def p2_retention(S, dr):
    with Phase(S) as ph:
        def cst(name, shape, dt):
            t = ph.sbuf("c_" + name, shape, dt)
            b = S.buf("Bc_" + name)
            S.dma("sp", lambda: t[:], lambda: dr[name](), b, writes=[b])
            return t, b
        ones, Bones = cst("ones", [128, 128], BF16)
        dm, Bdm = cst("dm", [128, 4, 2, 256], F32)
        wsel, Bwsel = cst("wsel", [128, 32], F32)
        cdec, Bcdec = cst("cdec", [128, 4], F32)
        epsc = ph.sbuf("epsc", [128, 1], F32)
        Bepsc = S.buf("Bepsc")
        S.op("dve", lambda e: e.memset(epsc[:], EPS), writes=[Bepsc])

        St = [ph.sbuf(f"St{i}", [128, 4, 2, 512], F32) for i in range(2)]
        BSt = [[[S.buf(f"BSt{i}_{h}_{dc}") for dc in range(2)] for h in range(4)] for i in range(2)]
        Sb = [ph.sbuf(f"Sb{i}", [128, 4, 2, 512], BF16) for i in range(4)]
        BSb = [[S.buf(f"BSb{i}_{h}") for h in range(4)] for i in range(4)]
        S.op("dve", lambda e: e.memset(St[0][:], 0.0), writes=[b for hh_ in BSt[0] for b in hh_])
        kdc = [ph.sbuf(f"kdc{i}", [128, 2, 1024], BF16) for i in range(2)]
        vrc = [ph.sbuf(f"vrc{i}", [128, 2, 2048], BF16) for i in range(2)]
        Bkdc, Bvrc = S.bufs("Bkdc", 2), S.bufs("Bvrc", 2)
        qT = [ph.sbuf(f"rq{i}", [128, 8, 256], BF16) for i in range(2)]
        qdT = [ph.sbuf(f"rqd{i}", [128, 8, 256], BF16) for i in range(2)]
        kT = [ph.sbuf(f"rk{i}", [128, 8, 256], BF16) for i in range(2)]
        vl = [ph.sbuf(f"rvl{i}", [128, 2, 2048], BF16) for i in range(2)]
        sg = [ph.sbuf(f"rsg{i}", [128, 16, 256], BF16) for i in range(2)]
        BqT, BqdT, BkT, Bvl, Bsg = (S.bufs(n, 2) for n in ("BrqT", "BrqdT", "BrkT", "Brvl", "Brsg"))
        innm = [ph.sbuf(f"innm{i}", [128, 2, 256], BF16) for i in range(2)]
        Binnm = S.bufs("Binnm", 2)
        Y = [ph.sbuf(f"Y{i}", [128, 4, 256], F32) for i in range(2)]
        Yb = [ph.sbuf(f"Yb{i}", [128, 4, 256], BF16) for i in range(2)]
        Ysq = [ph.sbuf(f"Ysq{i}", [128, 4, 256], BF16) for i in range(2)]
        BY, BYb, BYsq = S.bufs("BY", 2), S.bufs("BYb", 2), S.bufs("BYsq", 2)
        mu_ = [ph.sbuf(f"mu{i}", [128, 256], F32) for i in range(2)]
        msq_ = [ph.sbuf(f"msq{i}", [128, 256], F32) for i in range(2)]
        var_ = [ph.sbuf(f"var{i}", [128, 256], F32) for i in range(2)]
        Bmu_, Bmsq_, Bvar_ = S.bufs("Bmu", 2), S.bufs("Bmsq", 2), S.bufs("Bvar", 2)
        t1_ = [ph.sbuf(f"t1{i}", [128, 4, 256], F32) for i in range(2)]
        Bt1_ = S.bufs("Bt1", 2)
        ost = [ph.sbuf(f"ost{i}", [128, 4, 256], BF16) for i in range(2)]
        Bost = S.bufs("Bost", 2)

        inn = [ph.psum(f"inn{i}", [128, 2, 256], F32) for i in range(2)]
        Binn = S.pbufs("Binn", 2)
        yps = [ph.psum(f"yps{i}", [128, 2, 256], F32) for i in range(2)]
        Byps = S.pbufs("Byps", 2)
        stp_ = [ph.psum(f"stp{i}", [128, 2, 256], F32) for i in range(2)]
        Bstp_ = S.pbufs("Bstp", 2)
        kvp = [ph.psum(f"kvp{i}", [128, 512], F32) for i in range(2)]
        Bkvp = S.pbufs("Bkvp", 2)

        def load_chunk(n):
            k = n % 2
            S.dma("sp", lambda: kdc[k][:], lambda: dr["kd_all"]()[n], Bkdc[k], writes=[Bkdc[k]])
            S.dma("sp", lambda: vrc[k][:], lambda: dr["vr_all"]()[n], Bvrc[k], writes=[Bvrc[k]])

        def load_tile(i):
            k = i % 2
            cs = slice(i * 256, (i + 1) * 256)
            for dst, Bd, nm in ((qT, BqT, "qrT"), (qdT, BqdT, "qdT"), (kT, BkT, "krT")):
                S.dma("sp", lambda dst=dst: dst[k][:], lambda nm=nm: dr[nm]()[:, cs].rearrange("(fc p) t -> p fc t", p=128), Bd[k], writes=[Bd[k]])
            S.dma("sp", lambda: vl[k][:], lambda: dr["vr_loc"]()[i], Bvl[k], writes=[Bvl[k]])
            S.dma("sp", lambda: sg[k][:], lambda: dr["sgT"]()[:, cs].rearrange("(fc p) t -> p fc t", p=128), Bsg[k], writes=[Bsg[k]])

        def stage_a(i, h, hh):
            k = i % 2
            for js in range(2):
                for dc in range(2):
                    S.op("pe", lambda e, js=js, dc=dc: e.matmul(
                        inn[hh][:, js, :], lhsT=kT[k][:, h * 2 + dc, js * 128:(js + 1) * 128], rhs=qT[k][:, h * 2 + dc, :],
                        start=(dc == 0), stop=(dc == 1)), reads=[BkT[k], BqT[k]], writes=[Binn[hh]])
            S.op("dve", lambda e: e.tensor_tensor(out=innm[hh][:], in0=inn[hh][:], in1=dm[:, h, :, :], op=ALU.mult),
                 reads=[Binn[hh], Bdm], writes=[Binnm[hh]])

        def stage_b(i, h, hh):
            k = i % 2
            for ec in range(4):
                pb, pe_ = divmod(ec, 2)
                lhs = [(lambda js=js, ec=ec: vl[k][:, js, h * 512 + ec * 128:h * 512 + (ec + 1) * 128]) for js in range(2)] + \
                      [(lambda dc=dc, ec=ec, mm=mm: Sb[mm][:, h, dc, ec * 128:(ec + 1) * 128]) for mm in range(4) for dc in range(2)]
                rhs = [(lambda js=js: innm[hh][:, js, :]) for js in range(2)] + \
                      [(lambda dc=dc: qdT[k][:, h * 2 + dc, :]) for mm in range(4) for dc in range(2)]
                mm_group(S, (lambda pb=pb, pe_=pe_: yps[pb][:, pe_, :]), lhs, rhs,
                         [Bvl[k], Binnm[hh], BqdT[k]] + [BSb[mm][h] for mm in range(4)], Byps[pb])
            for pb in range(2):
                S.op("act", lambda e, pb=pb: e.activation(out=Y[hh][:, pb * 2:(pb + 1) * 2, :], in_=yps[pb][:], func=AF.Copy),
                     reads=[Byps[pb]], writes=[BY[hh]])
            S.op("dve", lambda e: e.tensor_copy(out=Yb[hh][:], in_=Y[hh][:]), reads=[BY[hh]], writes=[BYb[hh]])
            S.op("act", lambda e: e.activation(out=Ysq[hh][:], in_=Y[hh][:], func=AF.Square), reads=[BY[hh]], writes=[BYsq[hh]])

        def stage_c(i, h, hh):
            k = i % 2
            mu, msq, var, t1, stp = mu_[hh], msq_[hh], var_[hh], t1_[hh], stp_[hh]
            Bmu, Bmsq, Bvar, Bt1, Bstp = Bmu_[hh], Bmsq_[hh], Bvar_[hh], Bt1_[hh], Bstp_[hh]
            mm_group(S, lambda: stp[:, 0, :], [(lambda: ones[:])] * 4, [(lambda ec=ec: Yb[hh][:, ec, :]) for ec in range(4)],
                     [Bones, BYb[hh]], Bstp)
            mm_group(S, lambda: stp[:, 1, :], [(lambda: ones[:])] * 4, [(lambda ec=ec: Ysq[hh][:, ec, :]) for ec in range(4)],
                     [Bones, BYsq[hh]], Bstp)
            S.op("dve", lambda e: e.tensor_scalar(out=mu[:], in0=stp[:, 0, :], scalar1=1.0 / 512, scalar2=None, op0=ALU.mult),
                 reads=[Bstp], writes=[Bmu])
            S.op("dve", lambda e: e.tensor_tensor(out=msq[:], in0=mu[:], in1=mu[:], op=ALU.mult), reads=[Bmu], writes=[Bmsq])
            S.op("dve", lambda e: e.scalar_tensor_tensor(out=var[:], in0=stp[:, 1, :], scalar=1.0 / 512, in1=msq[:], op0=ALU.mult, op1=ALU.subtract),
                 reads=[Bstp, Bmsq], writes=[Bvar])
            S.op("act", lambda e: e.activation(out=var[:], in_=var[:], func=AF.Sqrt, bias=epsc[:]), reads=[Bvar, Bepsc], writes=[Bvar])
            S.op("dve", lambda e: e.reciprocal(out=var[:], in_=var[:]), reads=[Bvar], writes=[Bvar])
            S.op("dve", lambda e: e.tensor_tensor(out=t1[:], in0=Y[hh][:], in1=mu[:].unsqueeze(1).to_broadcast([128, 4, 256]), op=ALU.subtract),
                 reads=[BY[hh], Bmu], writes=[Bt1])
            S.op("dve", lambda e: e.tensor_tensor(out=t1[:], in0=t1[:], in1=var[:].unsqueeze(1).to_broadcast([128, 4, 256]), op=ALU.mult),
                 reads=[Bt1, Bvar], writes=[Bt1])
            S.op("dve", lambda e: e.tensor_tensor(out=ost[hh][:], in0=t1[:], in1=sg[k][:, h * 4:(h + 1) * 4, :], op=ALU.mult),
                 reads=[Bt1, Bsg[k]], writes=[Bost[hh]])
            S.dma("sp", lambda: dr["orrT_d"]()[h * 512:(h + 1) * 512, i * 256:(i + 1) * 256].rearrange("(ec p) t -> p ec t", p=128),
                  lambda: ost[hh][:], Bost[hh], reads=[Bost[hh]])

        load_chunk(0)
        load_tile(0)
        cnt = {"hh": 0}
        for n in range(NB):
            i, m = divmod(n, 4)
            pp = n % 2
            if n + 1 <= 30:
                load_chunk(n + 1)
            for h in range(4):
                S.op("act", lambda e, h=h: e.activation(out=Sb[m][:, h], in_=St[pp][:, h], func=AF.Copy, scale=wsel[:, n:n + 1]),
                     reads=BSt[pp][h] + [Bwsel], writes=[BSb[m][h]])
            if m == 3:
                if i + 1 < NT:
                    load_tile(i + 1)
                hhs = []
                for h in range(4):
                    hhs.append(cnt["hh"] % 2)
                    cnt["hh"] += 1
                stage_a(i, 0, hhs[0])
                for h in range(4):
                    stage_b(i, h, hhs[h])
                    if h + 1 < 4:
                        stage_a(i, h + 1, hhs[h + 1])
                    stage_c(i, h, hhs[h])
            if n <= 30:
                kk = n % 2
                for h in range(4):
                    for dc in range(2):
                        mm_group(S, (lambda dc=dc: kvp[dc][:]),
                                 [(lambda js=js, h=h, dc=dc: kdc[kk][:, js, h * 256 + dc * 128:h * 256 + (dc + 1) * 128]) for js in range(2)],
                                 [(lambda js=js, h=h: vrc[kk][:, js, h * 512:(h + 1) * 512]) for js in range(2)],
                                 [Bkdc[kk], Bvrc[kk]], Bkvp[dc])
                        S.op("dve", lambda e, h=h, dc=dc: e.scalar_tensor_tensor(out=St[1 - pp][:, h, dc, :], in0=St[pp][:, h, dc, :], scalar=cdec[:, h:h + 1],
                                                                                in1=kvp[dc][:], op0=ALU.mult, op1=ALU.add),
                             reads=[BSt[pp][h][dc], Bcdec, Bkvp[dc]], writes=[BSt[1 - pp][h][dc]])


def p2_oret(S, dr):
    with Phase(S) as ph:
        aT = ph.sbuf("orrT", [128, 16, T], BF16)
        BaT = S.buf("BorrT")
        S.dma("sp", lambda: aT[:], lambda: dr["orrT_d"]().rearrange("(kc p) t -> p kc t", p=128), BaT, writes=[BaT])
        G = Gemm(S, ph, 16, "wr")
        acc = [ph.psum(f"accr{i}", [128, 512], F32) for i in range(4)]
        Bacc = S.pbufs("Baccr", 4)
        gt = [ph.sbuf(f"gtr{i}", [128, T], BF16) for i in range(2)]
        m1 = [ph.sbuf(f"m1r{i}", [128, T], BF16) for i in range(2)]
        Bgt, Bm1 = S.bufs("Bgtr", 2), S.bufs("Bm1r", 2)
        tmp = [ph.sbuf(f"tmpr{i}", [128, 512], F32) for i in range(2)]
        Btmp = S.bufs("Btmpr", 2)
        stg = [ph.sbuf(f"stgr{i}", [128, T], BF16) for i in range(2)]
        Bstg = S.bufs("Bstgr", 2)
        slots = {0: G.load(dr["w_o_ret"], 0), 1: G.load(dr["w_o_ret"], 512)}
        na = 0
        for b in range(4):
            if b + 2 < 4:
                slots[b + 2] = G.load(dr["w_o_ret"], (b + 2) * 512)
            sl = slots[b]
            for sub in range(4):
                fc = b * 4 + sub
                g = fc % 2
                S.dma("sp", lambda g=g: gt[g][:], lambda fc=fc: dr["ggT"]()[fc * 128:(fc + 1) * 128, :], Bgt[g], writes=[Bgt[g]])
                S.dma("sp", lambda g=g: m1[g][:], lambda fc=fc: dr["m1T_d"]()[fc * 128:(fc + 1) * 128, :], Bm1[g], writes=[Bm1[g]])
                for tb in range(4):
                    a = na % 4
                    tq = na % 2
                    na += 1
                    cols = slice(tb * 512, (tb + 1) * 512)
                    mm_group(S, lambda a=a: acc[a][:],
                             [(lambda kc=kc, sl=sl, sub=sub: G.wb[sl][:, kc, sub * 128:(sub + 1) * 128]) for kc in range(16)],
                             [(lambda kc=kc, cols=cols: aT[:, kc, cols]) for kc in range(16)],
                             [G.Bw[sl], BaT], Bacc[a])
                    S.op("dve", lambda e, a=a, g=g, cols=cols, tq=tq: e.tensor_tensor(out=tmp[tq][:], in0=acc[a][:], in1=gt[g][:, cols], op=ALU.mult),
                         reads=[Bacc[a], Bgt[g]], writes=[Btmp[tq]])
                    S.op("dve", lambda e, g=g, cols=cols, tq=tq: e.tensor_tensor(out=stg[g][:, cols], in0=tmp[tq][:], in1=m1[g][:, cols], op=ALU.add),
                         reads=[Btmp[tq], Bm1[g]], writes=[Bstg[g]])
                S.dma("sp", lambda fc=fc: dr["mgT_d"]()[fc * 128:(fc + 1) * 128, :], lambda g=g: stg[g][:], Bstg[g], reads=[Bstg[g]])


def p2_wout(S, dr):
    with Phase(S) as ph:
        aT = ph.sbuf("mgT", [128, 16, T], BF16)
        BaT = S.buf("BmgT")
        S.dma("sp", lambda: aT[:], lambda: dr["mgT_d"]().rearrange("(kc p) t -> p kc t", p=128), BaT, writes=[BaT])
        G = Gemm(S, ph, 16, "wo")
        acc = [ph.psum(f"acco{i}", [128, 512], F32) for i in range(4)]
        Bacc = S.pbufs("Bacco", 4)
        xt = [ph.sbuf(f"xto{i}", [128, 512], F32) for i in range(3)]
        Bxt = S.bufs("Bxto", 3)
        so = [ph.sbuf(f"soo{i}", [128, 512], F32) for i in range(3)]
        Bso = S.bufs("Bsoo", 3)
        slots = {0: G.load(dr["w_out"], 0), 1: G.load(dr["w_out"], 512)}
        na = 0
        for b in range(4):
            if b + 2 < 4:
                slots[b + 2] = G.load(dr["w_out"], (b + 2) * 512)
            sl = slots[b]
            cs = slice(b * 512, (b + 1) * 512)
            for tt in range(T // 128):
                a = na % 4
                q = na % 3
                na += 1
                rows = slice(tt * 128, (tt + 1) * 128)
                S.dma("sp", lambda q=q: xt[q][:], lambda rows=rows, cs=cs: dr["x"]()[rows, cs], Bxt[q], writes=[Bxt[q]])
                mm_group(S, lambda a=a: acc[a][:],
                         [(lambda kc=kc, rows=rows: aT[:, kc, rows]) for kc in range(16)],
                         [(lambda kc=kc, sl=sl: G.wb[sl][:, kc, :]) for kc in range(16)],
                         [G.Bw[sl], BaT], Bacc[a])
                S.op("dve", lambda e, a=a, q=q: e.tensor_tensor(out=so[q][:], in0=acc[a][:], in1=xt[q][:], op=ALU.add),
                     reads=[Bacc[a], Bxt[q]], writes=[Bso[q]])
                S.dma("sp", lambda rows=rows, cs=cs: dr["x1_d"]()[rows, cs], lambda q=q: so[q][:], Bso[q], reads=[Bso[q]])


def p2_ffn_in(S, dr):
    with Phase(S) as ph:
        hT = ph.sbuf("h2T", [128, 16, T], BF16)
        BhT = S.buf("Bh2T")
        ident = ph.sbuf("identf", [128, 128], BF16)
        gm = ph.sbuf("gm2", [128, 16], F32)
        Bident, Bgm = S.buf("Bidentf"), S.buf("Bgm2")
        S.dma("sp", lambda: ident[:], lambda: dr["ident"](), Bident, writes=[Bident])
        S.dma("sp", lambda: gm[:], lambda: dr["gffn"](), Bgm, writes=[Bgm])
        epsc = ph.sbuf("epsc2", [128, 1], F32)
        Bepsc = S.buf("Bepsc2")
        S.op("dve", lambda e: e.memset(epsc[:], EPS), writes=[Bepsc])
        pT = [ph.psum(f"pTf{i}", [128, 8, 128], BF16) for i in range(2)]
        BpT = S.pbufs("BpTf", 2)
        G = Gemm(S, ph, 16, "wf", 4)
        order = []
        for b in range(11):
            order += [("g", b), ("u", b)]
        slots = {}

        def ld(idx):
            kind, b = order[idx]
            slots[idx] = G.load(dr["w_ffn_in"], b * 512 + (DFF if kind == "u" else 0))
        ld(0)
        ld(1)
        norm_to_hT(S, ph, dr["x1_d"], gm, Bgm, ident, Bident, hT, BhT, pT, BpT, "n2", epsc, Bepsc)
        accg = [ph.psum(f"accg{i}", [128, 512], F32) for i in range(3)]
        accu = [ph.psum(f"accu{i}", [128, 512], F32) for i in range(3)]
        Baccg, Baccu = S.pbufs("Baccg", 3), S.pbufs("Baccu", 3)
        sgt = [ph.sbuf(f"sgt{i}", [128, 512], F32) for i in range(3)]
        Bsgt = S.bufs("Bsgt", 3)
        stg = [ph.sbuf(f"stgf{i}", [128, T], BF16) for i in range(2)]
        Bstg = S.bufs("Bstgf", 2)
        na = 0
        nf = 0
        for b in range(11):
            if 2 * b + 2 < 22:
                ld(2 * b + 2)
            if 2 * b + 3 < 22:
                ld(2 * b + 3)
            slg, slu = slots[2 * b], slots[2 * b + 1]
            for sub in range(4):
                fb = b * 4 + sub
                g = nf % 2
                nf += 1
                for tb in range(4):
                    a = na % 3
                    na += 1
                    cols = slice(tb * 512, (tb + 1) * 512)
                    for (accx, Bx, slx) in ((accg, Baccg, slg), (accu, Baccu, slu)):
                        mm_group(S, lambda a=a, accx=accx: accx[a][:],
                                 [(lambda kc=kc, slx=slx, sub=sub: G.wb[slx][:, kc, sub * 128:(sub + 1) * 128]) for kc in range(16)],
                                 [(lambda kc=kc, cols=cols: hT[:, kc, cols]) for kc in range(16)],
                                 [G.Bw[slx], BhT], Bx[a])
                    S.op("act", lambda e, a=a: e.activation(out=sgt[a][:], in_=accg[a][:], func=AF.Silu), reads=[Baccg[a]], writes=[Bsgt[a]])
                    S.op("dve", lambda e, a=a, g=g, cols=cols: e.tensor_tensor(out=stg[g][:, cols], in0=accu[a][:], in1=sgt[a][:], op=ALU.mult),
                         reads=[Baccu[a], Bsgt[a]], writes=[Bstg[g]])
                S.dma("sp", lambda fb=fb: dr["aT_d"]()[fb * 128:(fb + 1) * 128, :], lambda g=g: stg[g][:], Bstg[g], reads=[Bstg[g]])


def p2_ffn_out(S, dr):
    KC = DFF // 128
    for th in range(2):
        with Phase(S) as ph:
            aT = ph.sbuf(f"aTh{th}", [128, KC, 1024], BF16)
            BaT = S.buf("BaTh")
            for q in range(4):
                S.dma("sp", lambda q=q: aT[:, q * 11:(q + 1) * 11, :],
                      lambda q=q: dr["aT_d"]()[q * 11 * 128:(q + 1) * 11 * 128, th * 1024:(th + 1) * 1024].rearrange("(kc p) t -> p kc t", p=128),
                      BaT, writes=[BaT])
            CW = 512
            wb = [ph.sbuf(f"wfo{th}_{i}", [128, KC, CW], BF16) for i in range(2)]
            Bw = S.bufs("Bwfo", 2)

            def ld(cb):
                sl = cb % 2
                for q in range(2):
                    S.dma("pool", lambda q=q: wb[sl][:, q * 22:(q + 1) * 22, :],
                          lambda q=q: dr["w_ffn_out"]()[q * 22 * 128:(q + 1) * 22 * 128, cb * CW:(cb + 1) * CW].rearrange("(kc p) n -> p kc n", p=128),
                          Bw[sl], writes=[Bw[sl]])
            ld(0)
            acc = [ph.psum(f"accf{th}_{i}", [128, 512], F32) for i in range(4)]
            Bacc = S.pbufs("Baccf", 4)
            xt = [ph.sbuf(f"xtf{th}_{i}", [128, CW], F32) for i in range(3)]
            Bxt = S.bufs("Bxtf", 3)
            so = [ph.sbuf(f"sof{th}_{i}", [128, CW], F32) for i in range(3)]
            Bso = S.bufs("Bsof", 3)
            na = 0
            for cb in range(D // CW):
                if cb + 1 < D // CW:
                    ld(cb + 1)
                sl = cb % 2
                cs = slice(cb * CW, (cb + 1) * CW)
                for tt in range(8):
                    a = na % 4
                    q = na % 3
                    na += 1
                    rows = slice(th * 1024 + tt * 128, th * 1024 + (tt + 1) * 128)
                    lrows = slice(tt * 128, (tt + 1) * 128)
                    S.dma("sp", lambda q=q: xt[q][:], lambda rows=rows, cs=cs: dr["x1_d"]()[rows, cs], Bxt[q], writes=[Bxt[q]])
                    mm_group(S, lambda a=a: acc[a][:, 0:CW],
                             [(lambda kc=kc, lrows=lrows: aT[:, kc, lrows]) for kc in range(KC)],
                             [(lambda kc=kc, sl=sl: wb[sl][:, kc, :]) for kc in range(KC)],
                             [Bw[sl], BaT], Bacc[a])
                    S.op("dve", lambda e, a=a, q=q: e.tensor_tensor(out=so[q][:], in0=acc[a][:, 0:CW], in1=xt[q][:], op=ALU.add),
                         reads=[Bacc[a], Bxt[q]], writes=[Bso[q]])
                    S.dma("sp", lambda rows=rows, cs=cs: dr["xo"]()[rows, cs], lambda q=q: so[q][:], Bso[q], reads=[Bso[q]])


def gen_p2(S, dr):
    for ph in DBG.get("p2_phases", ("attn", "oattn", "ret", "oret", "wout", "ffn_in", "ffn_out")):
        {"attn": (p2_attention_v3 if DBG.get("attn_v3") else p2_attention_v2), "oattn": p2_oattn, "ret": p2_retention, "oret": p2_oret, "wout": p2_wout,
         "ffn_in": p2_ffn_in, "ffn_out": p2_ffn_out}[ph](S, dr)


def p2_consts(j):
    sl = alibi_slopes()
    lg = log_g()
    Bi = [block_of(i, j) for i in range(NT)]
    p = np.arange(128)
    validb = np.zeros((128, 8, 32), np.float32)
    tab2 = np.zeros((128, 8, 8, 32), np.float32)
    wsel = np.zeros((128, 32), np.float32)
    for i in range(NT):
        for n in range(32):
            ok = n < Bi[i]
            validb[:, i, n] = 0.0 if ok else -1e30
            for h in range(8):
                tab2[:, i, h, n] = (-sl[h] * 256.0 * (Bi[i] - n) - 30000.0) if ok else -60000.0
        wsel[:, Bi[i]] = 1.0
    alibk = np.zeros((128, 8, 2), np.float32)
    for h in range(8):
        for c in range(2):
            alibk[:, h, c] = sl[h] * (c * 128 + p)
    aq = np.zeros((64, 8, 256), np.float32)
    q = np.arange(256)
    for h in range(8):
        aq[32, h, :] = -sl[h] * q
    selm = np.zeros((64, 32, 128), np.float32)
    for n in range(32):
        selm[n, n, :] = 1.0
        selm[32, n, :] = 1.0
    cm = np.zeros((128, 8, 2, 256), np.float32)
    for h in range(8):
        for c in range(2):
            kk = (c * 128 + p)[:, None]
            cm[:, h, c, :] = np.where(kk <= q[None, :], -sl[h] * q[None, :], -30000.0)
    dm = np.zeros((128, 4, 2, 256), np.float32)
    for h in range(4):
        for js in range(2):
            jj = (js * 128 + p)[:, None].astype(np.float64)
            diff = q[None, :].astype(np.float64) - jj
            dm[:, h, js, :] = np.where(diff >= 0, np.exp(lg[h] * np.maximum(diff, 0.0)), 0.0)
    cdec = np.tile(np.exp(lg * 256.0)[None, :], (128, 1)).astype(np.float32)
    return {"ident": np.eye(128, dtype=np.float32).astype(NPBF), "ones": np.ones((128, 128), np.float32).astype(NPBF),
            "validb": validb, "tab2": tab2, "alibk": alibk, "aq": aq.astype(NPBF), "selm": selm.astype(NPBF),
            "cm": cm.astype(NPBF), "dm": dm, "wsel": wsel, "cdec": cdec}


def vaug_layout(va, nchunk):
    v = va.reshape(nchunk, 128, 8, 128).transpose(2, 1, 0, 3)
    out = np.ones((8, 128, nchunk, 129), dtype=va.dtype)
    out[..., :128] = v
    return out


def chunk_layout(a):
    nch = a.shape[0] // 256
    return np.ascontiguousarray(a.reshape(nch, 2, 128, a.shape[1]).transpose(0, 2, 1, 3))


def shard_tokens(x):
    out = []
    for c in range(8):
        b, j = divmod(c, 4)
        out.append(np.concatenate([x[b, block_of(i, j) * 256:(block_of(i, j) + 1) * 256] for i in range(NT)], 0))
    return out


def unshard_tokens(xs):
    out = np.empty((2, S_LEN, D), xs[0].dtype)
    for c in range(8):
        b, j = divmod(c, 4)
        for i in range(NT):
            B = block_of(i, j)
            out[b, B * 256:(B + 1) * 256] = xs[c][i * 256:(i + 1) * 256]
    return out


def p2_inputs_from_p1(p1res, xs, wl, consts):
    ins = []
    glob = {}
    for b in range(2):
        kaT = np.empty((1024, S_LEN), NPBF)
        va = np.empty((S_LEN, 1024), NPBF)
        kd = np.empty((S_LEN, 1024), NPBF)
        vr = np.empty((S_LEN, 2048), NPBF)
        for j in range(4):
            r = p1res[4 * b + j]
            for i in range(NT):
                B = block_of(i, j)
                g, l = slice(B * 256, (B + 1) * 256), slice(i * 256, (i + 1) * 256)
                kaT[:, g] = r["kaT"][:, l]
                va[g] = r["va"][l]
                kd[g] = r["kd"][l]
                vr[g] = r["vr"][l]
        glob[b] = {"kaT_all": kaT, "vaug_all": vaug_layout(va, 64), "kd_all": chunk_layout(kd), "vr_all": chunk_layout(vr)}
    for c in range(8):
        b, j = divmod(c, 4)
        r = p1res[c]
        d = dict(glob[b])
        d.update({"x": xs[c], "qaT": r["qaT"], "kaT_loc": r["kaT"], "vaug_loc": vaug_layout(np.asarray(r["va"]), 16),
                  "qrT": r["qrT"], "qdT": r["qdT"], "krT": r["krT"], "vr_loc": chunk_layout(np.asarray(r["vr"])),
                  "sgT": r["sgT"], "gaT": r["gaT"], "ggT": r["ggT"]})
        d.update(wl)
        d.update(consts[j])
        ins.append(d)
    return ins


_PROGS = {}


def _prog(name):
    if name not in _PROGS:
        if name == "p1":
            _PROGS[name] = build_program(gen_p1, P1_INS, P1_OUTS)[0]
        else:
            _PROGS[name] = build_program(gen_p2, P2_INS, P2_OUTS, P2_SCRATCH)[0]
    return _PROGS[name]


def _pk(v):
    return np.ascontiguousarray(np.asarray(v, np.float32).reshape(16, 128).T)


def kernel(x, w_in, w_o_attn, w_o_ret, w_out, q_norm, k_norm, norm_mix, norm_ffn, w_ffn_in, w_ffn_out):
    x = np.asarray(x, np.float32)
    cores = list(range(8))
    xs = shard_tokens(x)
    c1 = p1_consts()
    c2 = [p2_consts(j) for j in range(4)]
    p1, p2 = _prog("p1"), _prog("p2")
    for l in range(DEPTH):
        wl1 = {"w_in": np.ascontiguousarray(w_in[l], dtype=np.float32), "gmix": _pk(norm_mix[l]),
               "qkg": np.ascontiguousarray(np.stack([q_norm[l], k_norm[l]], 1), dtype=np.float32)}
        wl1.update(c1)
        ins1 = [dict(wl1, x=xs[c]) for c in cores]
        r1 = run_bass_kernel_spmd(p1, ins1, core_ids=cores).results
        wl2 = {"w_o_attn": np.ascontiguousarray(w_o_attn[l], dtype=np.float32), "w_o_ret": np.ascontiguousarray(w_o_ret[l], dtype=np.float32),
               "w_out": np.ascontiguousarray(w_out[l], dtype=np.float32), "w_ffn_in": np.ascontiguousarray(w_ffn_in[l], dtype=np.float32),
               "w_ffn_out": np.ascontiguousarray(w_ffn_out[l], dtype=np.float32), "gffn": _pk(norm_ffn[l])}
        ins2 = p2_inputs_from_p1(r1, xs, wl2, c2)
        r2 = run_bass_kernel_spmd(p2, ins2, core_ids=cores).results
        xs = [np.asarray(r2[c]["xo"], np.float32) for c in cores]
    return unshard_tokens(xs)


def p2_attention_v2(S, dr):
    SKEW = 2
    with Phase(S) as ph:
        def cst(name, shape, dt):
            t = ph.sbuf("c_" + name, shape, dt)
            b = S.buf("Bc_" + name)
            S.dma("sp", lambda: t[:], lambda: dr[name](), b, writes=[b])
            return t, b
        ident, Bident = cst("ident", [128, 128], BF16)
        validb, Bvalidb = cst("validb", [128, 8, 32], F32)
        tab2, Btab2 = cst("tab2", [128, 8, 8, 32], F32)
        alibk, Balibk = cst("alibk", [128, 8, 2], F32)
        aq, Baq = cst("aq", [64, 8, 256], BF16)
        selm, Bselm = cst("selm", [64, 32, 128], BF16)
        cm, Bcm = cst("cm", [128, 8, 2, 256], BF16)

        KT = [ph.sbuf(f"KT{i}", [128, S_LEN], BF16) for i in range(2)]
        VA = [ph.sbuf(f"VA{i}", [128, 64, 129], BF16) for i in range(2)]
        QT = [ph.sbuf(f"QT{i}", [128, T], BF16) for i in range(2)]
        KL = [ph.sbuf(f"KL{i}", [128, T], BF16) for i in range(2)]
        VL = [ph.sbuf(f"VL{i}", [128, 16, 129], BF16) for i in range(2)]
        BKT, BVA, BQT, BKL, BVL = (S.bufs(n, 2) for n in ("BKT", "BVA", "BQT", "BKL", "BVL"))
        oaT = ph.sbuf("oaT", [128, 8, T], BF16)
        BoaT = S.bufs("BoaT", 8)
        km = ph.sbuf("km", [128, 32], F32)
        kmhf = ph.sbuf("kmhf", [128, 32], F32)
        kmh = [ph.sbuf(f"kmh{i}", [128, 32], BF16) for i in range(2)]
        kml = [ph.sbuf(f"kml{i}", [128, 32], BF16) for i in range(2)]
        Bkm, Bkmhf = S.buf("Bkm"), S.buf("Bkmhf")
        Bkmh, Bkml = S.bufs("Bkmh", 2), S.bufs("Bkml", 2)
        M = [ph.sbuf(f"M{i}", [64, 256], BF16) for i in range(4)]
        BM = S.bufs("BM", 4)
        g2 = ph.sbuf("g2", [128, 2, 32], F32)
        mx8 = ph.sbuf("mx8", [128, 2, 8], F32)
        msk = ph.sbuf("msk", [128, 2, 32], F32)
        mrow = ph.sbuf("mrow", [128, 2, 32], BF16)
        Bg2, Bmx8, Bmsk, Bmrow = (S.buf(n) for n in ("Bg2", "Bmx8", "Bmsk", "Bmrow"))
        NPT = 6
        PT = [ph.sbuf(f"PT{i}", [128, 256], BF16) for i in range(NPT)]
        BPT = S.bufs("BPT", NPT)
        rden = [ph.sbuf(f"rden{i}", [128, 2], F32) for i in range(2)]
        Brden = S.bufs("Brden", 2)
        on = [ph.sbuf(f"on{i}", [128, 2, 128], BF16) for i in range(2)]
        Bon = S.bufs("Bon", 2)

        sT = [ph.psum(f"sT{i}", [128, 512], F32) for i in range(3)]
        BsT = S.pbufs("BsT", 3)
        Ops = [[ph.psum(f"O{a}{s}", [128, 512], F32) for s in range(2)] for a in range(2)]
        BO = [S.pbufs(f"BO{a}", 2) for a in range(2)]
        misc = ph.psum("misc", [128, 512], F32)
        Bmisc = S.pbufs("Bmisc", 1)[0]
        gp = (lambda: misc[:, 0:64].rearrange("p (s n) -> p s n", s=2))
        miscb = (lambda: misc[:].bitcast(BF16))
        mT = (lambda: miscb()[0:32, 256:512])
        oT = (lambda: miscb()[:, 512:768])

        def load_head(h):
            k = h % 2
            S.dma("sp", lambda: KT[k][:], lambda: dr["kaT_all"]()[h * 128:(h + 1) * 128, :], BKT[k], writes=[BKT[k]])
            S.dma("sp", lambda: VA[k][:], lambda: dr["vaug_all"]()[h], BVA[k], writes=[BVA[k]])
            S.dma("sp", lambda: QT[k][:], lambda: dr["qaT"]()[h * 128:(h + 1) * 128, :], BQT[k], writes=[BQT[k]])
            S.dma("sp", lambda: KL[k][:], lambda: dr["kaT_loc"]()[h * 128:(h + 1) * 128, :], BKL[k], writes=[BKL[k]])
            S.dma("sp", lambda: VL[k][:], lambda: dr["vaug_loc"]()[h], BVL[k], writes=[BVL[k]])

        def head_prologue(h):
            k = h % 2
            S.op("dve", lambda e: e.tensor_reduce(out=km[:], in_=KT[k][:].rearrange("p (n s) -> p n s", s=256),
                                                  axis=AX.X, op=ALU.add), reads=[BKT[k]], writes=[Bkm])
            S.op("dve", lambda e: e.tensor_scalar(out=km[:], in0=km[:], scalar1=1.0 / 256, scalar2=None, op0=ALU.mult),
                 reads=[Bkm], writes=[Bkm])
            S.op("dve", lambda e: e.tensor_copy(out=kmh[k][:], in_=km[:]), reads=[Bkm], writes=[Bkmh[k]])
            S.op("dve", lambda e: e.tensor_copy(out=kmhf[:], in_=kmh[k][:]), reads=[Bkmh[k]], writes=[Bkmhf])
            S.op("dve", lambda e: e.tensor_tensor(out=kml[k][:], in0=km[:], in1=kmhf[:], op=ALU.subtract),
                 reads=[Bkm, Bkmhf], writes=[Bkml[k]])
            for par in range(2):
                S.op("dve", lambda e, par=par: e.tensor_copy(out=M[2 * k + par][32:64, :], in_=aq[32:64, h, :]),
                     reads=[Baq], writes=[BM[2 * k + par]])

        def tile_pro_a(h, i):
            k = h % 2
            for s in range(2):
                qs = slice(i * 256 + s * 128, i * 256 + (s + 1) * 128)
                S.op("pe", lambda e, s=s, qs=qs: e.matmul(gp()[:, s, :], lhsT=QT[k][:, qs], rhs=kmh[k][:], start=True, stop=False),
                     reads=[BQT[k], Bkmh[k]], writes=[Bmisc])
                S.op("pe", lambda e, s=s, qs=qs: e.matmul(gp()[:, s, :], lhsT=QT[k][:, qs], rhs=kml[k][:], start=False, stop=True),
                     reads=[BQT[k], Bkml[k]], writes=[Bmisc])
            S.op("dve", lambda e: e.tensor_tensor(out=g2[:], in0=gp(), in1=validb[:, i:i + 1, :].to_broadcast([128, 2, 32]), op=ALU.add),
                 reads=[Bmisc, Bvalidb], writes=[Bg2])
            for s in range(2):
                S.op("dve", lambda e, s=s: e.max(out=mx8[:, s, :], in_=g2[:, s, :]), reads=[Bg2], writes=[Bmx8])
            for s in range(2):
                S.op("dve", lambda e, s=s: e.tensor_scalar(out=msk[:, s, :], in0=g2[:, s, :], scalar1=mx8[:, s, 2:3], scalar2=30000.0,
                                                          op0=ALU.is_ge, op1=ALU.mult), reads=[Bg2, Bmx8], writes=[Bmsk])
            S.op("dve", lambda e: e.tensor_tensor(out=mrow[:], in0=msk[:], in1=tab2[:, i, h:h + 1, :].to_broadcast([128, 2, 32]), op=ALU.add),
                 reads=[Bmsk, Btab2], writes=[Bmrow])

        def tile_pro_b(h, i):
            k = h % 2
            mi = 2 * k + (i % 2)
            for s in range(2):
                S.op("pe", lambda e, s=s: e.transpose(out=mT()[:, s * 128:(s + 1) * 128], in_=mrow[:, s, :], identity=ident[:]),
                     reads=[Bmrow, Bident], writes=[Bmisc])
            S.op("dve", lambda e: e.tensor_copy(out=M[mi][0:32, :], in_=mT()), reads=[Bmisc], writes=[BM[mi]])

        cnt = {"s": 0, "p": 0, "t": 0, "step": 0}
        pending = []

        def flush(force=False):
            while pending and (force or pending[0][0] <= cnt["step"]):
                pending.pop(0)[1]()

        def later(delay, fn):
            due = cnt["step"] + delay
            if pending and due < pending[-1][0]:
                due = pending[-1][0]
            pending.append((due, fn))

        def s_stage(h, i, n, c, ob, first_flags):
            k = h % 2
            mi = 2 * k + (i % 2)
            qcols = slice(i * 256, (i + 1) * 256)
            r = cnt["s"] % 3
            cnt["s"] += 1
            p = cnt["p"] % NPT
            cnt["p"] += 1
            if n == "own":
                ks = slice(i * 256 + c * 128, i * 256 + (c + 1) * 128)
                S.op("pe", lambda e: e.matmul(sT[r][:, 0:256], lhsT=KL[k][:, ks], rhs=QT[k][:, qcols], start=True, stop=False),
                     reads=[BKL[k], BQT[k]], writes=[BsT[r]])
                S.op("pe", lambda e: e.matmul(sT[r][:, 0:256], lhsT=ident[:], rhs=cm[:, h, c, :], start=False, stop=True),
                     reads=[Bident, Bcm], writes=[BsT[r]])
            else:
                ks = slice((2 * n + c) * 128, (2 * n + c + 1) * 128)
                S.op("pe", lambda e: e.matmul(sT[r][:, 0:256], lhsT=KT[k][:, ks], rhs=QT[k][:, qcols], start=True, stop=False),
                     reads=[BKT[k], BQT[k]], writes=[BsT[r]])
                S.op("pe", lambda e: e.matmul(sT[r][:, 0:256], lhsT=selm[:, n, :], rhs=M[mi][:], start=False, stop=True),
                     reads=[Bselm, BM[mi]], writes=[BsT[r]])
            S.op("act", lambda e: e.activation(out=PT[p][:], in_=sT[r][:, 0:256], func=AF.Exp, bias=alibk[:, h, c:c + 1]),
                 reads=[BsT[r], Balibk], writes=[BPT[p]])

            def pv():
                for s in range(2):
                    if n == "own" and c == 1 and s == 0:
                        continue
                    last = (n == "own") and (c == 1 or (c == 0 and s == 0))
                    if n == "own":
                        rhs_fn, rb = (lambda: VL[k][:, 2 * i + c, :]), BVL[k]
                    else:
                        rhs_fn, rb = (lambda: VA[k][:, 2 * n + c, :]), BVA[k]
                    st = first_flags[s]
                    first_flags[s] = False
                    S.op("pe", lambda e, s=s, st=st, last=last, rhs_fn=rhs_fn: e.matmul(
                        Ops[ob][s][:, 0:129], lhsT=PT[p][:, s * 128:(s + 1) * 128], rhs=rhs_fn(), start=st, stop=last),
                         reads=[BPT[p], rb], writes=[BO[ob][s]])
            later(SKEW, pv)

        def epilogue_a(h, i, ob):
            o = cnt["t"] % 2
            cnt["t"] += 1
            for s in range(2):
                S.op("dve", lambda e, s=s: e.reciprocal(out=rden[o][:, s:s + 1], in_=Ops[ob][s][:, 128:129]), reads=[BO[ob][s]], writes=[Brden[o]])
            for s in range(2):
                S.op("dve", lambda e, s=s: e.tensor_scalar(out=on[o][:, s, :], in0=Ops[ob][s][:, 0:128], scalar1=rden[o][:, s:s + 1], scalar2=None,
                                                          op0=ALU.mult), reads=[BO[ob][s], Brden[o]], writes=[Bon[o]])

            def epi_b():
                for s in range(2):
                    S.op("pe", lambda e, s=s: e.transpose(out=oT()[:, s * 128:(s + 1) * 128], in_=on[o][:, s, :], identity=ident[:]),
                         reads=[Bon[o], Bident], writes=[Bmisc])
                S.op("act", lambda e: e.activation(out=oaT[:, h, i * 256:(i + 1) * 256], in_=oT(), func=AF.Copy),
                     reads=[Bmisc], writes=[BoaT[h]])
                if i == NT - 1:
                    S.dma("sp", lambda: dr["oaT_d"]()[h * 128:(h + 1) * 128, :], lambda: oaT[:, h, :], BoaT[h], reads=[BoaT[h]])
            later(2, epi_b)

        load_head(0)
        head_prologue(0)
        tile_pro_a(0, 0)
        tile_pro_b(0, 0)
        tcount = 0
        for h in range(8):
            for i in range(NT):
                ob = tcount % 2
                tcount += 1
                nblk = 4 * i + 3
                steps = [(n, c) for n in range(nblk) for c in range(2)] + [("own", 0), ("own", 1)]
                first_flags = [True, True]
                nxt = (h, i + 1) if i + 1 < NT else ((h + 1, 0) if h + 1 < 8 else None)
                for idx, (n, c) in enumerate(steps):
                    s_stage(h, i, n, c, ob, first_flags)
                    cnt["step"] += 1
                    flush()
                    if i == 0 and idx == 6 and h + 1 < 8:
                        load_head(h + 1)
                    if nxt is not None:
                        if idx == 0:
                            if nxt[1] == 0:
                                head_prologue(nxt[0])
                            tile_pro_a(*nxt)
                        if idx == 3:
                            tile_pro_b(*nxt)
                later(SKEW, (lambda h=h, i=i, ob=ob: epilogue_a(h, i, ob)))
        flush(force=True)
        flush(force=True)


def p2_attention_v3(S, dr):
    SKEW = 2
    NST = NT // 2
    with Phase(S) as ph:
        def cst(name, shape, dt):
            t = ph.sbuf("c_" + name, shape, dt)
            b = S.buf("Bc_" + name)
            S.dma("sp", lambda: t[:], lambda: dr[name](), b, writes=[b])
            return t, b
        ident, Bident = cst("ident", [128, 128], BF16)
        ones, Bones = cst("ones", [128, 128], BF16)
        onesf, Bonesf = cst("onesf", [1, 128], F32)
        validb, Bvalidb = cst("validb", [128, 8, 32], F32)
        tab2, Btab2 = cst("tab2", [128, 8, 8, 32], F32)
        alibk, Balibk = cst("alibk", [128, 8, 2], F32)
        aq, Baq = cst("aq2", [64, 8, 512], BF16)
        selm, Bselm = cst("selm", [64, 32, 128], BF16)
        cm, Bcm = cst("cm", [128, 8, 2, 256], BF16)

        KT = [ph.sbuf(f"KT{i}", [128, S_LEN], BF16) for i in range(2)]
        VA = [ph.sbuf(f"VA{i}", [128, 64, 129], BF16) for i in range(2)]
        QT = [ph.sbuf(f"QT{i}", [128, T], BF16) for i in range(2)]
        KL = [ph.sbuf(f"KL{i}", [128, T], BF16) for i in range(2)]
        VL = [ph.sbuf(f"VL{i}", [128, 16, 129], BF16) for i in range(2)]
        BKT, BVA, BQT, BKL, BVL = (S.bufs(n, 2) for n in ("BKT", "BVA", "BQT", "BKL", "BVL"))
        oaT = ph.sbuf("oaT", [128, 8, T], BF16)
        BoaT = S.bufs("BoaT", 8)
        km = ph.sbuf("km", [128, 32], F32)
        kmhf = ph.sbuf("kmhf", [128, 32], F32)
        kmh = [ph.sbuf(f"kmh{i}", [128, 32], BF16) for i in range(2)]
        kml = [ph.sbuf(f"kml{i}", [128, 32], BF16) for i in range(2)]
        Bkm, Bkmhf = S.buf("Bkm"), S.buf("Bkmhf")
        Bkmh, Bkml = S.bufs("Bkmh", 2), S.bufs("Bkml", 2)
        M = [ph.sbuf(f"M{i}", [64, 512], BF16) for i in range(4)]
        BM = S.bufs("BM", 4)
        g2 = ph.sbuf("g2", [128, 4, 32], F32)
        mx8 = ph.sbuf("mx8", [128, 4, 8], F32)
        msk = ph.sbuf("msk", [128, 4, 32], F32)
        mrow = ph.sbuf("mrow", [128, 4, 32], BF16)
        Bg2, Bmx8, Bmsk, Bmrow = (S.buf(n) for n in ("Bg2", "Bmx8", "Bmsk", "Bmrow"))
        NPT = 6
        PT = [ph.sbuf(f"PT{i}", [128, 512], BF16) for i in range(NPT)]
        BPT = S.bufs("BPT", NPT)
        rden = ph.sbuf("rden", [1, 512], F32)
        Brden = S.buf("Brden")
        bcs = ph.sbuf("bcs", [128, 512], F32)
        Bbcs = S.buf("Bbcs")

        sT = [ph.psum(f"sT{i}", [128, 512], F32) for i in range(3)]
        BsT = S.pbufs("BsT", 3)
        OT = [ph.psum(f"OT{a}", [128, 512], F32) for a in range(2)]
        BOT = S.pbufs("BOT", 2)
        den = ph.psum("den", [1, 512], F32)
        Bden = S.pbufs("Bden", 1)[0]
        bc = ph.psum("bc", [128, 512], F32)
        Bbc = S.pbufs("Bbc", 1)[0]
        misc = ph.psum("misc", [128, 512], F32)
        Bmisc = S.pbufs("Bmisc", 1)[0]
        gp = (lambda: misc[:, 0:128].rearrange("p (s n) -> p s n", s=4))
        mT = (lambda: misc[:].bitcast(BF16)[0:32, 512:1024])

        def load_head(h):
            k = h % 2
            S.dma("sp", lambda: KT[k][:], lambda: dr["kaT_all"]()[h * 128:(h + 1) * 128, :], BKT[k], writes=[BKT[k]])
            S.dma("sp", lambda: VA[k][:], lambda: dr["vaug_all"]()[h], BVA[k], writes=[BVA[k]])
            S.dma("sp", lambda: QT[k][:], lambda: dr["qaT"]()[h * 128:(h + 1) * 128, :], BQT[k], writes=[BQT[k]])
            S.dma("sp", lambda: KL[k][:], lambda: dr["kaT_loc"]()[h * 128:(h + 1) * 128, :], BKL[k], writes=[BKL[k]])
            S.dma("sp", lambda: VL[k][:], lambda: dr["vaug_loc"]()[h], BVL[k], writes=[BVL[k]])

        def head_prologue(h):
            k = h % 2
            S.op("dve", lambda e: e.tensor_reduce(out=km[:], in_=KT[k][:].rearrange("p (n s) -> p n s", s=256),
                                                  axis=AX.X, op=ALU.add), reads=[BKT[k]], writes=[Bkm])
            S.op("dve", lambda e: e.tensor_scalar(out=km[:], in0=km[:], scalar1=1.0 / 256, scalar2=None, op0=ALU.mult),
                 reads=[Bkm], writes=[Bkm])
            S.op("dve", lambda e: e.tensor_copy(out=kmh[k][:], in_=km[:]), reads=[Bkm], writes=[Bkmh[k]])
            S.op("dve", lambda e: e.tensor_copy(out=kmhf[:], in_=kmh[k][:]), reads=[Bkmh[k]], writes=[Bkmhf])
            S.op("dve", lambda e: e.tensor_tensor(out=kml[k][:], in0=km[:], in1=kmhf[:], op=ALU.subtract),
                 reads=[Bkm, Bkmhf], writes=[Bkml[k]])
            for par in range(2):
                S.op("dve", lambda e, par=par: e.tensor_copy(out=M[2 * k + par][32:64, :], in_=aq[32:64, h, :]),
                     reads=[Baq], writes=[BM[2 * k + par]])

        def tile_pro_a(h, t):
            k = h % 2
            for u in range(4):
                qs = slice(t * 512 + u * 128, t * 512 + (u + 1) * 128)
                S.op("pe", lambda e, u=u, qs=qs: e.matmul(gp()[:, u, :], lhsT=QT[k][:, qs], rhs=kmh[k][:], start=True, stop=False),
                     reads=[BQT[k], Bkmh[k]], writes=[Bmisc])
                S.op("pe", lambda e, u=u, qs=qs: e.matmul(gp()[:, u, :], lhsT=QT[k][:, qs], rhs=kml[k][:], start=False, stop=True),
                     reads=[BQT[k], Bkml[k]], writes=[Bmisc])
            for hf in range(2):
                i = 2 * t + hf
                S.op("dve", lambda e, hf=hf, i=i: e.tensor_tensor(out=g2[:, 2 * hf:2 * hf + 2, :], in0=gp()[:, 2 * hf:2 * hf + 2, :],
                                                                  in1=validb[:, i:i + 1, :].to_broadcast([128, 2, 32]), op=ALU.add),
                     reads=[Bmisc, Bvalidb], writes=[Bg2])
            for u in range(4):
                S.op("dve", lambda e, u=u: e.max(out=mx8[:, u, :], in_=g2[:, u, :]), reads=[Bg2], writes=[Bmx8])
            for u in range(4):
                S.op("dve", lambda e, u=u: e.tensor_scalar(out=msk[:, u, :], in0=g2[:, u, :], scalar1=mx8[:, u, 2:3], scalar2=30000.0,
                                                          op0=ALU.is_ge, op1=ALU.mult), reads=[Bg2, Bmx8], writes=[Bmsk])
            for hf in range(2):
                i = 2 * t + hf
                S.op("dve", lambda e, hf=hf, i=i: e.tensor_tensor(out=mrow[:, 2 * hf:2 * hf + 2, :], in0=msk[:, 2 * hf:2 * hf + 2, :],
                                                                  in1=tab2[:, i, h:h + 1, :].to_broadcast([128, 2, 32]), op=ALU.add),
                     reads=[Bmsk, Btab2], writes=[Bmrow])

        def tile_pro_b(h, t):
            k = h % 2
            mi = 2 * k + (t % 2)
            for u in range(4):
                S.op("pe", lambda e, u=u: e.transpose(out=mT()[:, u * 128:(u + 1) * 128], in_=mrow[:, u, :], identity=ident[:]),
                     reads=[Bmrow, Bident], writes=[Bmisc])
            S.op("dve", lambda e: e.tensor_copy(out=M[mi][0:32, :], in_=mT()), reads=[Bmisc], writes=[BM[mi]])

        cnt = {"s": 0, "p": 0, "step": 0}
        pending = []

        def flush(force=False):
            while pending and (force or pending[0][0] <= cnt["step"]):
                pending.pop(0)[1]()

        def later(delay, fn):
            due = cnt["step"] + delay
            if pending and due < pending[-1][0]:
                due = pending[-1][0]
            pending.append((due, fn))

        def s_stage(h, t, n, c, hf, ob, flags):
            k = h % 2
            mi = 2 * k + (t % 2)
            r = cnt["s"] % 3
            cnt["s"] += 1
            p = cnt["p"] % NPT
            cnt["p"] += 1
            if n == "own":
                i = 2 * t + hf
                W = 256
                qcols = slice(i * 256, (i + 1) * 256)
                ocols = slice(hf * 256, (hf + 1) * 256)
                ks = slice(i * 256 + c * 128, i * 256 + (c + 1) * 128)
                S.op("pe", lambda e: e.matmul(sT[r][:, 0:W], lhsT=KL[k][:, ks], rhs=QT[k][:, qcols], start=True, stop=False),
                     reads=[BKL[k], BQT[k]], writes=[BsT[r]])
                S.op("pe", lambda e: e.matmul(sT[r][:, 0:W], lhsT=ident[:], rhs=cm[:, h, c, :], start=False, stop=True),
                     reads=[Bident, Bcm], writes=[BsT[r]])
                v_fn, vb = (lambda: VL[k][:, 2 * i + c, 0:128]), BVL[k]
            else:
                W = 512
                qcols = slice(t * 512, (t + 1) * 512)
                ocols = slice(0, 512)
                ks = slice((2 * n + c) * 128, (2 * n + c + 1) * 128)
                S.op("pe", lambda e: e.matmul(sT[r][:, 0:W], lhsT=KT[k][:, ks], rhs=QT[k][:, qcols], start=True, stop=False),
                     reads=[BKT[k], BQT[k]], writes=[BsT[r]])
                S.op("pe", lambda e: e.matmul(sT[r][:, 0:W], lhsT=selm[:, n, :], rhs=M[mi][:], start=False, stop=True),
                     reads=[Bselm, BM[mi]], writes=[BsT[r]])
                v_fn, vb = (lambda: VA[k][:, 2 * n + c, 0:128]), BVA[k]
            S.op("act", lambda e: e.activation(out=PT[p][:, 0:W], in_=sT[r][:, 0:W], func=AF.Exp, bias=alibk[:, h, c:c + 1]),
                 reads=[BsT[r], Balibk], writes=[BPT[p]])
            last = (n == "own" and hf == 1 and c == 1)

            def pv():
                st = flags[0]
                flags[0] = False
                S.op("pe", lambda e: e.matmul(OT[ob][:, ocols], lhsT=v_fn(), rhs=PT[p][:, 0:W], start=st, stop=last),
                     reads=[BPT[p], vb], writes=[BOT[ob]])
                S.op("pe", lambda e: e.matmul(den[0:1, ocols], lhsT=ones[:, 0:1], rhs=PT[p][:, 0:W], start=st, stop=last),
                     reads=[BPT[p], Bones], writes=[Bden])
            later(SKEW, pv)

        def epilogue(h, t, ob):
            S.op("dve", lambda e: e.reciprocal(out=rden[:], in_=den[0:1, :]), reads=[Bden], writes=[Brden])
            S.op("pe", lambda e: e.matmul(bc[:], lhsT=onesf[:], rhs=rden[:], start=True, stop=True),
                 reads=[Bonesf, Brden], writes=[Bbc])
            S.op("act", lambda e: e.activation(out=bcs[:], in_=bc[:], func=AF.Copy), reads=[Bbc], writes=[Bbcs])
            S.op("dve", lambda e: e.tensor_tensor(out=oaT[:, h, t * 512:(t + 1) * 512], in0=OT[ob][:], in1=bcs[:], op=ALU.mult),
                 reads=[BOT[ob], Bbcs], writes=[BoaT[h]])
            if t == NST - 1:
                S.dma("sp", lambda: dr["oaT_d"]()[h * 128:(h + 1) * 128, :], lambda: oaT[:, h, :], BoaT[h], reads=[BoaT[h]])

        load_head(0)
        head_prologue(0)
        tile_pro_a(0, 0)
        tile_pro_b(0, 0)
        tcount = 0
        for h in range(8):
            for t in range(NST):
                ob = tcount % 2
                tcount += 1
                nblk = 4 * (2 * t + 1) + 3
                steps = [(n, c, 0) for n in range(nblk) for c in range(2)] + [("own", c, hf) for hf in range(2) for c in range(2)]
                flags = [True]
                nxt = (h, t + 1) if t + 1 < NST else ((h + 1, 0) if h + 1 < 8 else None)
                for idx, (n, c, hf) in enumerate(steps):
                    s_stage(h, t, n, c, hf, ob, flags)
                    cnt["step"] += 1
                    flush()
                    if t == 0 and idx == 6 and h + 1 < 8:
                        load_head(h + 1)
                    if nxt is not None:
                        if idx == 0:
                            if nxt[1] == 0:
                                head_prologue(nxt[0])
                            tile_pro_a(*nxt)
                        if idx == 4:
                            tile_pro_b(*nxt)
                later(SKEW, (lambda h=h, t=t, ob=ob: epilogue(h, t, ob)))
        flush(force=True)
```
